# Optimizing a Trainium2 kernel written in Bass

```python
import jax
import jax.numpy as jnp
from jax import lax
import numpy as np

D_MODEL = 1024
BATCH = 8
SEQ = 2048
DEPTH = 1
DEC_BATCH = 128
DEC_SEQ = 4
PAST_LEN = 16384
PAGE_SIZE = 128

GLA_HEADS = 4
GLA_DK = D_MODEL // 8
GLA_DV = D_MODEL // 4
GLA_KEY_W = GLA_HEADS * GLA_DK
GLA_VAL_W = GLA_HEADS * GLA_DV
GLA_GATE_RANK = 16
GLA_GATE_NORM = 16.0
GLA_CHUNK = 64
POOL_WINDOWS = (2, 4, 8, 16)
POOL_GROUPS = 4
POOL_W = D_MODEL
POOL_G = POOL_W // POOL_GROUPS
POOL_BUF = max(POOL_WINDOWS) - 1
MEM_LEN = 256
X_HEADS = 4
X_HEAD_DIM = D_MODEL // X_HEADS
D_FF = 4 * D_MODEL
EPS = 1e-6
IN_SIZES = (GLA_KEY_W, GLA_KEY_W, GLA_VAL_W, GLA_GATE_RANK, GLA_VAL_W, POOL_W, D_MODEL, D_MODEL)
IN_WIDTH = sum(IN_SIZES)

kernel_name = 'gla_pool_hybrid_decoder'


def rmsnorm(x, g):
    xf = x.astype(jnp.float32)
    y = xf * lax.rsqrt(jnp.mean(xf * xf, axis=-1, keepdims=True) + EPS)
    return (y * g.astype(jnp.float32)).astype(x.dtype)


def split_points():
    pts, acc = [], 0
    for s in IN_SIZES[:-1]:
        acc += s
        pts.append(acc)
    return pts


def gla_chunked(q, k, v, log_a, s0):
    B, T, H, DK = q.shape
    DV = v.shape[-1]
    C = min(GLA_CHUNK, T)
    n = -(-T // C)
    pad = n * C - T

    def prep(a):
        a = jnp.pad(a.astype(jnp.float32), ((0, 0), (0, pad), (0, 0), (0, 0)))
        return a.reshape(B, n, C, H, a.shape[-1]).transpose(1, 0, 2, 3, 4)

    causal = jnp.tril(jnp.ones((C, C), dtype=bool))[None, :, :, None, None]

    def step(S, inp):
        qc, kc, vc, ac = inp
        b = jnp.cumsum(ac, axis=1)
        decay = jnp.exp(jnp.where(causal, b[:, :, None] - b[:, None, :], -jnp.inf))
        attn = jnp.einsum('bthk,btshk,bshk->bhts', qc, decay, kc)
        o = (jnp.einsum('bhts,bshv->bthv', attn, vc)
             + jnp.einsum('bthk,bhkv->bthv', qc * jnp.exp(b), S))
        b_end = b[:, -1]
        k_dec = kc * jnp.exp(b_end[:, None] - b)
        S = jnp.exp(b_end)[..., None] * S + jnp.einsum('bshk,bshv->bhkv', k_dec, vc)
        return S, o

    S, o = lax.scan(step, s0.astype(jnp.float32), (prep(q), prep(k), prep(v), prep(log_a)))
    o = o.transpose(1, 0, 2, 3, 4).reshape(B, n * C, H, DV)[:, :T]
    return o.astype(q.dtype), S.astype(s0.dtype)


def pool_mix(u, buf, pos0, w_mix, scale):
    B, T, P = u.shape
    L = POOL_BUF
    ext = jnp.concatenate([buf.astype(u.dtype), u], axis=1).astype(jnp.float32)
    cs = jnp.concatenate([jnp.zeros((B, 1, P), jnp.float32), jnp.cumsum(ext, axis=1)], axis=1)
    hi = cs[:, L + 1:L + 1 + T]
    pos = pos0 + jnp.arange(T)
    means = []
    for g, w in enumerate(POOL_WINDOWS):
        sl = slice(g * POOL_G, (g + 1) * POOL_G)
        win = hi[..., sl] - cs[:, L + 1 - w:L + 1 - w + T, sl]
        cnt = jnp.minimum(pos + 1, w).astype(jnp.float32)[None, :, None]
        means.append(win / cnt)
    d = (jnp.concatenate(means, axis=-1) - u.astype(jnp.float32)).reshape(B, T, POOL_GROUPS, POOL_G)
    y = jnp.einsum('btgc,gcd->btgd', d, w_mix.astype(jnp.float32)).reshape(B, T, P)
    y = y * scale.astype(jnp.float32)
    return y.astype(u.dtype), ext[:, -L:].astype(buf.dtype)


def token_mix(xn, s0, buf, pos0, p):
    B, T, _ = xn.shape
    proj = xn @ p['w_in']
    q, k, v, gz, og, u, ga, gb = jnp.split(proj, split_points(), axis=-1)
    q = q.reshape(B, T, GLA_HEADS, GLA_DK) * (GLA_DK ** -0.5)
    k = k.reshape(B, T, GLA_HEADS, GLA_DK)
    v = v.reshape(B, T, GLA_HEADS, GLA_DV)
    log_a = jax.nn.log_sigmoid((gz @ p['w_gk_up'] + p['b_gk']).astype(jnp.float32)) / GLA_GATE_NORM
    log_a = log_a.reshape(B, T, GLA_HEADS, GLA_DK)
    o, s_new = gla_chunked(q, k, v, log_a, s0)
    o = rmsnorm(o, p['gla_norm_g']).reshape(B, T, GLA_VAL_W) * jax.nn.silu(og)
    branch_a = o @ p['w_branch_a']
    pooled, buf_new = pool_mix(u, buf, pos0, p['w_pool_mix'], p['pool_scale'])
    branch_b = pooled @ p['w_branch_b']
    merged = jax.nn.sigmoid(ga) * branch_a + jax.nn.sigmoid(gb) * branch_b
    return merged @ p['w_out'], s_new, buf_new


def mem_kv(mem, g, wk, wv):
    B, M, _ = mem.shape
    m = rmsnorm(mem, g)
    return ((m @ wk).reshape(B, M, X_HEADS, X_HEAD_DIM),
            (m @ wv).reshape(B, M, X_HEADS, X_HEAD_DIM))


def cross_attn(hn, mk, mv, wq, wo):
    B, T, _ = hn.shape
    q = (hn @ wq).reshape(B, T, X_HEADS, X_HEAD_DIM).astype(jnp.float32)
    s = jnp.einsum('bthd,bmhd->bhtm', q, mk.astype(jnp.float32)) * (X_HEAD_DIM ** -0.5)
    pr = jax.nn.softmax(s, axis=-1)
    o = jnp.einsum('bhtm,bmhd->bthd', pr, mv.astype(jnp.float32)).astype(hn.dtype)
    return o.reshape(B, T, D_MODEL) @ wo


def sq_relu_mlp(x, w_up, w_down):
    h = jax.nn.relu(x @ w_up)
    return (h * h) @ w_down


def decoder_layer(x, mk, mv, s0, buf, pos0, p):
    mix, s_new, buf_new = token_mix(rmsnorm(x, p['norm_mix_g']), s0, buf, pos0, p)
    h = x + mix
    h = h + cross_attn(rmsnorm(h, p['norm_x_g']), mk, mv, p['w_xq'], p['w_xo'])
    h = h + sq_relu_mlp(rmsnorm(h, p['norm_mlp_g']), p['w_up'], p['w_down'])
    return h, s_new, buf_new


def setup_inputs(seed: int = 0) -> dict:
    key = jax.random.key(seed)
    ks = jax.random.split(key, 32)
    f32 = jnp.float32

    def nrm(k, shape, scale=1.0):
        return jax.random.normal(k, shape, f32) * scale

    L = DEPTH
    return {
        'x_prompt': nrm(ks[0], (BATCH, SEQ, D_MODEL)),
        'x_sample': nrm(ks[1], (DEC_BATCH, DEC_SEQ, D_MODEL)),
        'mem_prompt': nrm(ks[2], (BATCH, MEM_LEN, D_MODEL)),
        'state_gla': nrm(ks[3], (L, DEC_BATCH, GLA_HEADS, GLA_DK, GLA_DV)),
        'state_pool': nrm(ks[4], (L, DEC_BATCH, POOL_BUF, POOL_W)),
        'cache_mem_k': nrm(ks[5], (L, DEC_BATCH, MEM_LEN, X_HEADS, X_HEAD_DIM)),
        'cache_mem_v': nrm(ks[6], (L, DEC_BATCH, MEM_LEN, X_HEADS, X_HEAD_DIM)),
        'norm_mix_g': 1.0 + nrm(ks[7], (L, D_MODEL), 0.02),
        'w_in': nrm(ks[8], (L, D_MODEL, IN_WIDTH), D_MODEL ** -0.5),
        'w_gk_up': nrm(ks[9], (L, GLA_GATE_RANK, GLA_KEY_W), GLA_GATE_RANK ** -0.5),
        'b_gk': nrm(ks[10], (L, GLA_KEY_W), 0.02),
        'gla_norm_g': 1.0 + nrm(ks[11], (L, GLA_DV), 0.02),
        'w_pool_mix': nrm(ks[12], (L, POOL_GROUPS, POOL_G, POOL_G), POOL_G ** -0.5),
        'pool_scale': 1.0 + nrm(ks[13], (L, POOL_W), 0.02),
        'w_branch_a': nrm(ks[14], (L, GLA_VAL_W, D_MODEL), GLA_VAL_W ** -0.5),
        'w_branch_b': nrm(ks[15], (L, POOL_W, D_MODEL), POOL_W ** -0.5),
        'w_out': nrm(ks[16], (L, D_MODEL, D_MODEL), D_MODEL ** -0.5),
        'norm_x_g': 1.0 + nrm(ks[17], (L, D_MODEL), 0.02),
        'norm_mem_g': 1.0 + nrm(ks[18], (L, D_MODEL), 0.02),
        'w_xq': nrm(ks[19], (L, D_MODEL, D_MODEL), D_MODEL ** -0.5),
        'w_xk': nrm(ks[20], (L, D_MODEL, D_MODEL), D_MODEL ** -0.5),
        'w_xv': nrm(ks[21], (L, D_MODEL, D_MODEL), D_MODEL ** -0.5),
        'w_xo': nrm(ks[22], (L, D_MODEL, D_MODEL), D_MODEL ** -0.5),
        'norm_mlp_g': 1.0 + nrm(ks[23], (L, D_MODEL), 0.02),
        'w_up': nrm(ks[24], (L, D_MODEL, D_FF), D_MODEL ** -0.5),
        'w_down': nrm(ks[25], (L, D_FF, D_MODEL), D_FF ** -0.5),
        'norm_final_g': 1.0 + nrm(ks[26], (D_MODEL,), 0.02),
    }


def reference(x_prompt, x_sample, mem_prompt, state_gla, state_pool, cache_mem_k, cache_mem_v,
              norm_mix_g, w_in, w_gk_up, b_gk, gla_norm_g, w_pool_mix, pool_scale,
              w_branch_a, w_branch_b, w_out, norm_x_g, norm_mem_g, w_xq, w_xk, w_xv, w_xo,
              norm_mlp_g, w_up, w_down, norm_final_g):
    yp, ys = x_prompt, x_sample
    mk_p_all, mv_p_all, sg_p_all, sg_s_all, sp_p_all, sp_s_all = [], [], [], [], [], []
    for l in range(DEPTH):
        p = {
            'norm_mix_g': norm_mix_g[l], 'w_in': w_in[l], 'w_gk_up': w_gk_up[l], 'b_gk': b_gk[l],
            'gla_norm_g': gla_norm_g[l], 'w_pool_mix': w_pool_mix[l], 'pool_scale': pool_scale[l],
            'w_branch_a': w_branch_a[l], 'w_branch_b': w_branch_b[l], 'w_out': w_out[l],
            'norm_x_g': norm_x_g[l], 'w_xq': w_xq[l], 'w_xo': w_xo[l],
            'norm_mlp_g': norm_mlp_g[l], 'w_up': w_up[l], 'w_down': w_down[l],
        }
        mk_p, mv_p = mem_kv(mem_prompt, norm_mem_g[l], w_xk[l], w_xv[l])
        s0_p = jnp.zeros((yp.shape[0], GLA_HEADS, GLA_DK, GLA_DV), jnp.float32)
        buf_p = jnp.zeros((yp.shape[0], POOL_BUF, POOL_W), yp.dtype)
        yp, sg_p, sp_p = decoder_layer(yp, mk_p, mv_p, s0_p, buf_p, 0, p)
        ys, sg_s, sp_s = decoder_layer(ys, cache_mem_k[l], cache_mem_v[l], state_gla[l],
                                       state_pool[l], PAST_LEN, p)
        mk_p_all.append(mk_p)
        mv_p_all.append(mv_p)
        sg_p_all.append(sg_p)
        sg_s_all.append(sg_s)
        sp_p_all.append(sp_p)
        sp_s_all.append(sp_s)
    y_prompt = rmsnorm(yp, norm_final_g)
    y_sample = rmsnorm(ys, norm_final_g)
    return (y_prompt, y_sample, jnp.stack(mk_p_all), jnp.stack(mv_p_all),
            jnp.stack(sg_p_all), jnp.stack(sg_s_all), jnp.stack(sp_p_all), jnp.stack(sp_s_all))
```

```python
import contextlib
import math
import numpy as np
import concourse.bass as bass
import concourse.mybir as mybir
from concourse.bass_utils import run_bass_kernel_spmd

F32 = mybir.dt.float32
BF16 = mybir.dt.bfloat16
AF = mybir.ActivationFunctionType
ALU = mybir.AluOpType
AX = mybir.AxisListType

COMPUTE = ("pe", "act", "dve", "pool")

D = 1024
SEQ = 2048
NCORE = 8
SB = 16
ST = 4
MEM = 256
DFF = 4096
INW = 6160
EPS = 1e-6
NSLOT = 5
USE_SCRATCH = True
USE_KVSCR = True
SCR_SPREAD = 3
HIDE_TAIL = True
POOL_SOFTMAX = "pool"


class Res:
    __slots__ = ("name", "last_w", "reads", "dsem", "dcount", "excl")

    def __init__(self, name, excl=False):
        self.name = name
        self.excl = excl
        self.last_w = None
        self.reads = {}
        self.dsem = None
        self.dcount = 0


class Sched:
    def __init__(self, nc, stack):
        self.nc = nc
        self.stack = stack
        self.streams = {e: [] for e in COMPUTE + ("sp",)}
        self.count = {e: 0 for e in COMPUTE}
        self.sem = {e: stack.enter_context(nc.semaphore(f"S_{e}")) for e in COMPUTE}
        self.waited = {e: {} for e in COMPUTE + ("sp",)}
        self.dma_res = []
        self.n_waits = 0
        self.n_ops = 0
        self.sb_bytes = 0

    def sb(self, name, shape, dt):
        n = 1
        for s in shape[1:]:
            n *= s
        self.sb_bytes += n * (2 if dt == BF16 else 4)
        return self.stack.enter_context(self.nc.sbuf_tensor("sb_" + name, list(shape), dt))

    def ps(self, name, shape, dt):
        return self.stack.enter_context(self.nc.psum_tensor("ps_" + name, list(shape), dt))

    def new_sem(self, name):
        return self.stack.enter_context(self.nc.semaphore(name))

    def _deps(self, eng, reads, writes):
        deps = {}

        def add(ev, raw):
            if ev is None:
                return
            sem, val, src = ev
            if (not raw) and src == eng and eng == "pe":
                return
            k = id(sem)
            if k not in deps or deps[k][1] < val:
                deps[k] = ev

        for r in reads:
            add(r.last_w, True)
            if r.excl:
                for ev in r.reads.values():
                    add(ev, False)
        for w in writes:
            add(w.last_w, False)
            for ev in w.reads.values():
                add(ev, False)
        return deps

    def _emit_waits(self, eng, deps):
        wd = self.waited[eng]
        out = []
        for k, (sem, val, src) in deps.items():
            if wd.get(k, 0) >= val:
                continue
            wd[k] = val
            out.append((sem, val))
        return out

    def _record(self, ev, reads, writes):
        k = id(ev[0])
        for r in reads:
            old = r.reads.get(k)
            if old is None or old[1] < ev[1]:
                r.reads[k] = ev
        for w in writes:
            w.last_w = ev
            w.reads = {}

    def op(self, eng, fn, reads=(), writes=(), signal=True):
        waits = self._emit_waits(eng, self._deps(eng, reads, writes))
        self.n_waits += len(waits)
        self.n_ops += 1
        if signal:
            self.count[eng] += 1
            val = self.count[eng]
        else:
            val = self.count[eng] + 1
        sem = self.sem[eng]
        ev = (sem, val, eng)

        def run(e, waits=waits, fn=fn, signal=signal, sem=sem):
            for s, v in waits:
                e.wait_ge(s, v)
            ins = fn(e)
            if signal:
                ins.then_inc(sem, 1)

        self.streams[eng].append(run)
        self._record(ev, reads, writes)
        return ev

    def dma(self, queue, out, in_, reads=(), writes=(), sem_res=None):
        if sem_res is None:
            sem_res = (list(writes) + list(reads))[0]
        qk = "sw" if queue == "pool" else "hw"
        if sem_res.dsem is None:
            sem_res.dsem = {}
        if qk not in sem_res.dsem:
            ent = [self.new_sem(f"D{qk}_{sem_res.name}"), 0]
            sem_res.dsem[qk] = ent
            self.dma_res.append(ent)
        ent = sem_res.dsem[qk]
        waits = self._emit_waits(queue, self._deps("dma", reads, writes))
        self.n_waits += len(waits)
        ent[1] += 16
        sem = ent[0]
        ev = (sem, ent[1], "dma")

        def run(e, waits=waits, out=out, in_=in_, sem=sem):
            for s, v in waits:
                e.wait_ge(s, v)
            e.dma_start(out=out, in_=in_).then_inc(sem, 16)

        self.streams[queue].append(run)
        self._record(ev, reads, writes)
        return ev

    def finish(self):
        waits = [(ent[0], ent[1]) for ent in self.dma_res]
        for e in COMPUTE:
            if self.count[e]:
                waits.append((self.sem[e], self.count[e]))

        def run(e, waits=waits):
            for s, v in waits:
                e.wait_ge(s, v)

        self.streams["sp"].append(run)

    def emit(self):
        st = self.streams
        with self.nc.Block() as block:
            @block.sync
            def _(e):
                for f in st["sp"]:
                    f(e)

            @block.tensor
            def _(e):
                for f in st["pe"]:
                    f(e)

            @block.scalar
            def _(e):
                for f in st["act"]:
                    f(e)

            @block.vector
            def _(e):
                for f in st["dve"]:
                    f(e)

            @block.gpsimd
            def _(e):
                for f in st["pool"]:
                    f(e)


POOL_WINDOWS = (2, 4, 8, 16)


def _bf16_round(a):
    a = np.asarray(a, np.float32)
    u = a.view(np.uint32).astype(np.uint64)
    r = ((u + 0x7FFF + ((u >> 16) & 1)) >> 16) << 16
    return r.astype(np.uint32).view(np.float32)


def make_consts():
    c = {}
    c["ident"] = np.eye(128, dtype=np.float32)
    s = np.arange(128)[:, None]
    t = np.arange(128)[None, :]
    c["maskT"] = (s <= t).astype(np.float32)
    s6 = np.arange(64)[:, None]
    t6 = np.arange(64)[None, :]
    c["smask"] = ((s6 // ST == t6 // ST) & (s6 <= t6)).astype(np.float32)
    r = np.ones((128, 512), np.float32)
    r[:, ::128] = 0.0
    c["rst512"] = r
    r = np.ones((128, 64), np.float32)
    r[:, ::ST] = 0.0
    c["rst64"] = r
    pm = np.zeros((128, 4, 4, 128), np.float32)
    for g, w in enumerate(POOL_WINDOWS):
        cur = ((s <= t) & (s >= t - w + 1)).astype(np.float32) / w - (s == t).astype(np.float32)
        prev = ((s - 128) >= (t - w + 1)).astype(np.float32) / w
        cnt = np.minimum(t + 1, w).astype(np.float32)
        m0 = ((s <= t) & (s >= t - w + 1)).astype(np.float32) / cnt - (s == t).astype(np.float32)
        hi = _bf16_round(m0)
        lo = _bf16_round(m0 - hi)
        pm[:, 0, g], pm[:, 1, g], pm[:, 2, g], pm[:, 3, g] = cur, prev, hi, lo
    c["pmat"] = pm
    mb = np.zeros((120, 2, 4, 64), np.float32)
    mu = np.zeros((64, 4, 64), np.float32)
    for g, w in enumerate(POOL_WINDOWS):
        for b in range(SB):
            for tt in range(ST):
                col = b * ST + tt
                lo_idx = 15 + tt - w + 1
                for j in range(15):
                    if j >= lo_idx:
                        mb[(b % 8) * 15 + j, b // 8, g, col] = 1.0 / w
                for t2 in range(ST):
                    v = 0.0
                    if t2 <= tt and (15 + t2) >= lo_idx:
                        v += 1.0 / w
                    if t2 == tt:
                        v -= 1.0
                    mu[b * ST + t2, g, col] = v
    c["smb"] = mb
    c["smu"] = mu
    bq = np.zeros((128, SB, 64), np.float32)
    for b in range(SB):
        bq[:, b, b * ST:(b + 1) * ST] = 1.0
    c["bmq"] = bq
    bk = np.zeros((64, SB), np.float32)
    for b in range(SB):
        bk[b * ST:(b + 1) * ST, b] = 1.0
    c["bmk"] = bk
    return c


CONST_SHAPES = {k: v.shape for k, v in make_consts().items()}

COLS = {}
_o = 0
for _n, _w in (("g_mix", 8), ("g_x", 8), ("g_mlp", 8), ("g_mem", 8), ("g_gla", 2), ("pscale", 8), ("bgk", 4)):
    COLS[_n] = (_o, _w)
    _o += _w
NCOL = _o


class StopBuild(Exception):
    pass


class Prog:
    def __init__(self, do_sample=True, dbg=(), stop=None):
        self.stop = stop
        self.dbg_names = dbg
        self.do_sample = do_sample
        self.nc = nc = bass.Bass("TRN2", target_bir_lowering=False)
        self.I = {}
        self.O = {}

        def inp(name, shape):
            self.I[name] = nc.dram_tensor(name, list(shape), F32, kind="ExternalInput").ap()

        def outp(name, shape):
            self.O[name] = nc.dram_tensor(name, list(shape), F32, kind="ExternalOutput").ap()

        inp("xp", [SEQ, D]); inp("xs", [SB * ST, D]); inp("mem", [MEM, D])
        inp("sgla", [SB, 4, 128, 256]); inp("spool", [SB, 15, D])
        inp("ck", [SB, MEM, D]); inp("cv", [SB, MEM, D])
        inp("w_in", [D, INW]); inp("w_gk", [16, 512]); inp("w_pm", [4, 256, 256])
        for n in ("w_a", "w_b", "w_o", "w_xq", "w_xk", "w_xv", "w_xo"):
            inp(n, [D, D])
        inp("w_up", [D, DFF]); inp("w_dn", [DFF, D])
        inp("cols", [128, NCOL]); inp("g_fin", [D])
        for k, shp in CONST_SHAPES.items():
            inp("c_" + k, shp)
        outp("yp", [SEQ, D]); outp("ys", [SB * ST, D]); outp("mk", [MEM, D]); outp("mv", [MEM, D])
        outp("sgp", [4, 128, 256]); outp("sgs", [SB, 4, 128, 256]); outp("spp", [15, D]); outp("sps", [SB, 15, D])
        self.dbg_out = {}

        with contextlib.ExitStack() as stack:
            self.K = K = Sched(nc, stack)
            self.alloc()
            for s_ in range(2):
                xa, xr = self.xnext(s_)
                K.dma("sp", xa, self.I["mem"][s_ * 128:(s_ + 1) * 128, :], writes=xr)
            self.plan_units()
            self.load_consts()
            try:
                self.stage("consts")
                for s_ in range(4):
                    K.dma("sp", self.xh[:, s_, :], self.I["xp"][s_ * 128:(s_ + 1) * 128, :], writes=[self.xhr[s_]])
                self.mem_kv()
                self.stage("memkv")
                for ti in range(SEQ // 512):
                    self.kv_active = ti >= 2
                    self.prompt_tile(ti)
                    self.stage(f"tile{ti}")
                self.kv_active = False
                while self.kv_todo:
                    w, b = self.kv_todo.pop(0)
                    K.dma("pool", self.kvscr[w, b], self.I["ck" if w == 0 else "cv"][b], writes=[self.kvscr_res])
                if self.kvscr_res.dsem is not None:
                    ent = self.kvscr_res.dsem["sw"]
                    self.kvscr_res.last_w = (ent[0], ent[1], "dma")
                self.prompt_finish()
                if do_sample:
                    self.sample_pass()
                assert self.wi == len(self.units), (self.wi, len(self.units))
            except StopBuild:
                print("[kernel] build stopped at", self.stop)
            K.finish()
            K.emit()
            print(f"[kernel] ops={K.n_ops} waits={K.n_waits} sbuf_bytes/partition={K.sb_bytes} "
                  f"dma_sems={len(K.dma_res)} counts={K.count}")

    def stage(self, name):
        if self.stop == name:
            raise StopBuild()

    def T_(self, name, shape, dt, nres=1):
        t = self.K.sb(name, shape, dt)
        if nres == 1:
            return t, Res(name)
        return t, [Res(f"{name}{i}") for i in range(nres)]

    def alloc(self):
        K = self.K
        self.pb = [K.ps(f"pb{i}", [128, 512], F32) for i in range(8)]
        self.pbf = [p.bitcast(BF16) for p in self.pb]
        self.pbr = [Res(f"pb{i}", excl=True) for i in range(8)]
        self.bi = 0
        self.held = set()
        self.pending_trans = None
        self.wt = [K.sb(f"wt{i}", [128, 8, 512], BF16) for i in range(NSLOT)]
        self.wr = [Res(f"wt{i}") for i in range(NSLOT)]
        self.xh, self.xhr = self.T_("xh", [128, 4, D], F32, 4)
        self.xn, self.xnr = self.T_("xn", [128, 4, D], BF16, 4)
        self.actT, self.actr = self.T_("actT", [128, 8, 512], BF16, 8)
        self.S, self.Sr = self.T_("S", [128, 4, 256], F32, 4)
        self.Sb, self.Sbr = self.T_("Sb", [128, 4, 4, 256], BF16, 16)
        self.ur, self.urr = self.T_("uring", [128, 5, D], BF16, 5)
        self.KTp, self.KTpr = self.T_("KTp", [128, 8, 256], BF16)
        self.Vp, self.Vpr = self.T_("Vp", [128, 2, D], BF16)
        self.junk, self.junkr = self.T_("junk", [128, 256], BF16)
        self.small, self.smallr = self.T_("small", [128, 96], F32)
        self.sm2r = [Res("sm_a"), Res("sm_b"), Res("sm_c")]
        self.nsr = [Res(f"ns{i}") for i in range(4)]
        self.osr = [Res(f"os{i}") for i in range(4)]
        self.ident, self.identr = self.T_("ident", [128, 128], BF16)
        self.maskT, self.maskTr = self.T_("maskT", [128, 128], F32)
        self.smask, self.smaskr = self.T_("smask", [64, 64], F32)
        self.rst512, self.rst512r = self.T_("rst512", [128, 512], BF16)
        self.rst64, self.rst64r = self.T_("rst64", [128, 64], F32)
        self.pmat, self.pmatr = self.T_("pmat", [128, 4, 4, 128], BF16)
        self.smb, self.smbr = self.T_("smb", [120, 2, 4, 64], BF16)
        self.smu, self.smur = self.T_("smu", [64, 4, 64], BF16)
        self.bmq, self.bmqr = self.T_("bmq", [128, SB, 64], BF16)
        self.bmk, self.bmkr = self.T_("bmk", [64, SB], BF16)
        self.cols, self.colsr = self.T_("cols", [128, NCOL], F32)
        self.ncols, self.ncolsr = self.T_("ncols", [128, 8], F32)
        self.gfin, self.gfinr = self.T_("gfin", [128, D], F32)
        self.wgz, self.wgzr = self.T_("wgz", [128, 8, 16], BF16)
        self.wgk, self.wgkr = self.T_("wgk", [16, 512], BF16)
        self.cres = Res("consts")
        self.R1 = K.sb("R1", [128, 16384], BF16)
        self.R1r = [Res(f"R1_{i}") for i in range(32)]
        self.qi, self.qir = self.T_("qi", [128, 4, 512], BF16, 4)
        self.kd, self.kdr = self.T_("kd", [128, 4, 512], BF16, 4)
        self.kdt, self.kdtr = self.T_("kdt", [128, 4, 512], BF16, 4)
        self.vt, self.vtr = self.T_("vt", [128, 4, D], BF16, 4)
        self.ogs, self.ogsr = self.T_("ogs", [128, 4, D], BF16, 4)
        self.ga, self.gar = self.T_("ga", [128, 8, 512], BF16, 8)
        self.gb, self.gbr = self.T_("gb", [128, 8, 512], BF16, 8)
        self.att, self.attr = self.T_("att", [128, 4, 4, 128], BF16, 4)
        self.tmp, self.tmpr = self.T_("tmp", [128, 4, 512], F32, 4)
        self.tp = 0

    def r1_f32(self, i):
        v = self.R1[:, i * 4096:(i + 1) * 4096].bitcast(F32)
        return v.rearrange("p (h t) -> p h t", t=512), self.R1r[i * 8:(i + 1) * 8]

    def r1_bf(self, i):
        v = self.R1[:, i * 4096:(i + 1) * 4096]
        return v.rearrange("p (c t) -> p c t", t=512), self.R1r[i * 8:(i + 1) * 8]

    def bank(self):
        while True:
            b = self.bi
            self.bi = (self.bi + 1) % 8
            if b not in self.held:
                return b

    def hold(self, n):
        bs = []
        for _ in range(n):
            b = self.bank()
            self.held.add(b)
            bs.append(b)
        return bs

    def release(self, bs):
        for b in bs:
            self.held.discard(b)

    def tmpbuf(self):
        i = self.tp
        self.tp = (self.tp + 1) % 4
        return self.tmp[:, i, :], self.tmpr[i]

    def col(self, name, j=0, n=1, P=128):
        o, w = COLS[name]
        return self.cols[:P, o + j:o + j + n]

    def load_consts(self):
        K = self.K
        I = self.I
        cr = self.cres
        K.dma("sp", self.cols[:], I["cols"][:, :], writes=[self.colsr], sem_res=cr)
        K.dma("sp", self.maskT[:], I["c_maskT"][:, :], writes=[self.maskTr], sem_res=cr)
        K.dma("sp", self.smask[:], I["c_smask"][:, :], writes=[self.smaskr], sem_res=cr)
        K.dma("sp", self.rst64[:], I["c_rst64"][:, :], writes=[self.rst64r], sem_res=cr)
        K.dma("sp", self.gfin[:], I["g_fin"].partition_broadcast(128), writes=[self.gfinr], sem_res=cr)
        K.dma("pool", self.ident[:], I["c_ident"][:, :], writes=[self.identr], sem_res=self.identr)
        self.prefetch(0)
        K.dma("pool", self.rst512[:], I["c_rst512"][:, :], writes=[self.rst512r], sem_res=cr)
        K.dma("pool", self.pmat[:], I["c_pmat"][:, :, :, :], writes=[self.pmatr], sem_res=cr)
        K.dma("pool", self.smb[:], I["c_smb"][:, :, :, :], writes=[self.smbr], sem_res=cr)
        K.dma("pool", self.smu[:], I["c_smu"][:, :, :], writes=[self.smur], sem_res=cr)
        K.dma("pool", self.bmq[:], I["c_bmq"][:, :, :], writes=[self.bmqr], sem_res=cr)
        K.dma("pool", self.bmk[:], I["c_bmk"][:, :], writes=[self.bmkr], sem_res=cr)
        K.dma("pool", self.wgk[:], I["w_gk"][:, :], writes=[self.wgkr], sem_res=cr)
        K.dma("pool", self.wgz[:], I["w_in"][:, 2048:2064].rearrange("(kc p) n -> p kc n", p=128),
              writes=[self.wgzr], sem_res=cr)
        for r in (self.colsr, self.maskTr, self.smaskr, self.rst64r, self.gfinr):
            r.last_w = (cr.dsem["hw"][0], cr.dsem["hw"][1], "dma")
        for r in (self.rst512r, self.pmatr, self.smbr, self.smur, self.bmqr, self.bmkr, self.wgkr, self.wgzr):
            r.last_w = (cr.dsem["sw"][0], cr.dsem["sw"][1], "dma")
        o, w = COLS["bgk"]
        K.op("dve", lambda e: e.tensor_scalar(out=self.ncols[:, 0:4], in0=self.cols[:, o:o + 4], scalar1=-1.0,
                                             scalar2=None, op0=ALU.mult), reads=[self.colsr], writes=[self.ncolsr])
        K.op("dve", lambda e: e.memset(self.ncols[:, 4:5], math.log(128.0 ** -0.5)), writes=[self.ncolsr])
        K.op("dve", lambda e: e.memset(self.ncols[:, 5:6], 1.0), writes=[self.ncolsr])
        K.op("dve", lambda e: e.memset(self.ncols[:, 6:7], EPS), writes=[self.ncolsr])
        for h in range(4):
            K.op("dve", lambda e, h=h: e.memset(self.S[:, h, :], 0.0), writes=[self.Sr[h]])
        K.op("dve", lambda e: e.memset(self.ur[:, 4, :], 0.0), writes=[self.urr[4]])

    def plan_units(self):
        I = self.I
        U = []

        def full(w, r0, c0, name, sidx=None, p=0):
            U.append((name, w[r0:r0 + 1024, c0:c0 + 512].rearrange("(kc p) n -> p kc n", p=128), 512, sidx, mode(sidx, p)))

        def mode(sidx, p):
            if sidx is None or not USE_SCRATCH:
                return 0
            tp = sidx % SCR_SPREAD
            return 0 if p < tp else (1 if p == tp else 2)

        def one_pass(first):
            k = 0
            for nm, c0 in (("v0", 1024), ("v1", 1536), ("og0", 2064), ("og1", 2576), ("u0", 3088), ("u1", 3600),
                           ("ga0", 4112), ("ga1", 4624), ("gb0", 5136), ("gb1", 5648), ("k", 512), ("q", 0)):
                full(I["w_in"], 0, c0, nm, k, first); k += 1
            U.append(("pm", I["w_pm"].rearrange("g (kc p) d -> p (g kc) d", p=128), 256, k, mode(k, first))); k += 1
            for nm, w, c0 in (("b0", "w_b", 0), ("a0", "w_a", 0), ("b1", "w_b", 512), ("a1", "w_a", 512),
                              ("o0", "w_o", 0), ("o1", "w_o", 512),
                              ("xq0", "w_xq", 0), ("xq1", "w_xq", 512), ("xo0", "w_xo", 0), ("xo1", "w_xo", 512)):
                full(I[w], 0, c0, nm, k, first); k += 1
            for i in range(8):
                full(I["w_up"], 0, i * 512, f"up{i}", k, first); k += 1
            for n in range(2):
                for j in range(4):
                    full(I["w_dn"], j * 1024, n * 512, f"dn{j}_{n}", k, first); k += 1
            return k

        for nm, w, c0 in (("xk0", "w_xk", 0), ("xk1", "w_xk", 512), ("xv0", "w_xv", 0), ("xv1", "w_xv", 512)):
            full(I[w], 0, c0, nm)
        npass = SEQ // 512 + (1 if self.do_sample else 0)
        for p in range(npass):
            nper = one_pass(p)
        self.units = U
        self.pend_st = []
        self.wi = 0
        self.wl = 0
        self.scr = self.nc.dram_tensor("wscratch", [nper, 128, 8 * 512], BF16, kind="Internal").ap()
        self.scrr = [Res(f"scr{i}") for i in range(nper)]
        self.kvscr = self.nc.dram_tensor("kvscratch", [2, SB, MEM, D], BF16, kind="Internal").ap()
        self.kvscr_res = Res("kvscr")
        self.kv_todo = [(w, b) for w in range(2) for b in range(SB)] if (self.do_sample and USE_KVSCR) else []
        self.kv_tick = 0
        self.kv_active = False

    def W(self, name):
        i = self.wi
        assert self.units[i][0] == name, (self.units[i][0], name)
        self.prefetch(i)
        self.kv_tick += 1
        if self.kv_active and self.kv_todo and self.kv_tick % 2 == 0:
            w, b = self.kv_todo.pop(0)
            src = self.I["ck" if w == 0 else "cv"][b]
            self.K.dma("pool", self.kvscr[w, b], src, writes=[self.kvscr_res])
        self.wi += 1
        s = i % NSLOT
        return self.wt[s], self.wr[s]

    def flush_one_store(self):
        j, s, sidx, nc_ = self.pend_st.pop(0)
        self.K.dma("pool", self.scr[sidx].rearrange("p (kc n) -> p kc n", n=512)[:, :, 0:nc_], self.wt[s][:, :, 0:nc_],
                   reads=[self.wr[s]], writes=[self.scrr[sidx]], sem_res=self.scrr[sidx])

    def prefetch(self, i):
        K = self.K
        lim = min(len(self.units), i + NSLOT - 1)
        while self.wl < lim:
            j = self.wl
            s = j % NSLOT
            nm, src, nc_, sidx, md = self.units[j]
            if md in (0, 1):
                K.dma("pool", self.wt[s][:, :, 0:nc_], src, writes=[self.wr[s]])
                if md == 1:
                    self.pend_st.append((j, s, sidx, nc_))
            else:
                K.dma("pool", self.wt[s][:, :, 0:nc_], self.scr[sidx].rearrange("p (kc n) -> p kc n", n=512)[:, :, 0:nc_],
                      reads=[self.scrr[sidx]], writes=[self.wr[s]])
            self.wl += 1
            while self.pend_st and self.pend_st[0][0] <= j - 2:
                self.flush_one_store()

    def mm(self, out, pairs, reads, bres, start=True, stop=True, signal=True):
        def fn(e, out=out, pairs=pairs, start=start, stop=stop):
            n = len(pairs)
            ins = None
            for i, (l, r) in enumerate(pairs):
                ins = e.matmul(out, l, r, start=(start and i == 0), stop=(stop and i == n - 1))
            return ins
        return self.K.op("pe", fn, reads=reads, writes=[bres], signal=signal)

    def dbg(self, name, ap, res, shape):
        if name not in self.dbg_names or name in self.dbg_out:
            return
        o = self.nc.dram_tensor("dbg_" + name, list(shape), F32, kind="ExternalOutput").ap()
        self.dbg_out[name] = o
        st = self.K.sb("dbgs_" + name, list(shape), F32)
        sr = Res("dbgs_" + name)
        rl = res if isinstance(res, (list, tuple)) else [res]
        self.K.op("dve", lambda e: e.tensor_copy(st[:], ap), reads=rl, writes=[sr])
        self.K.dma("sp", o, st[:], reads=[sr])

    def rstd_from_ss(self, ss_ap, out_ap, inv_n, P, sr=None):
        K = self.K
        sr = self.smallr if sr is None else sr
        K.op("act", lambda e: e.activation(out=out_ap, in_=ss_ap, func=AF.Ln, scale=inv_n, bias=self.ncols[:P, 6:7]),
             reads=[sr, self.ncolsr], writes=[sr])
        K.op("act", lambda e: e.activation(out=out_ap, in_=out_ap, func=AF.Exp, scale=-0.5), reads=[sr], writes=[sr])

    def xnext(self, s):
        g, gr = (self.ga, self.gar) if s < 2 else (self.gb, self.gbr)
        c0 = (s % 2) * 4
        return g[:, c0:c0 + 4, :].rearrange("p c t -> p (c t)").bitcast(F32), gr[c0:c0 + 4]

    def norm_sub(self, s, sp, src=None, srcr=None):
        K = self.K
        sr = self.nsr[s]
        if src is None:
            src, srcr = self.xh[:sp, s, :], [self.xhr[s]]
        K.op("act", lambda e: e.activation(out=self.xn[:sp, s, :], in_=src, func=AF.Square,
                                           accum_out=self.small[:sp, s:s + 1]),
             reads=list(srcr) + [sr], writes=[self.xnr[s], sr])
        self.rstd_from_ss(self.small[:sp, s:s + 1], self.small[:sp, 4 + s:5 + s], 1.0 / D, sp, sr=sr)
        K.op("dve", lambda e: e.tensor_scalar(out=self.xn[:sp, s, :], in0=src,
                                             scalar1=self.small[:sp, 4 + s:5 + s], scalar2=None, op0=ALU.mult),
             reads=list(srcr) + [sr], writes=[self.xnr[s]])

    def trans_sub(self, src, srcr, dst, dstr, s, sp, gname, gmod=8):
        K = self.K
        b = self.bank()
        for c in range(8):
            K.op("pe", lambda e, b=b, c=c: e.transpose(self.pbf[b][:, c * sp:(c + 1) * sp], src[:sp, s, c * 128:(c + 1) * 128],
                                                      self.ident[:sp, :sp]),
                 reads=[srcr[s], self.identr], writes=[self.pbr[b]], signal=(c == 7))
        o, w = COLS[gname]
        if gmod == 8:
            outv = dst[:, :, s * sp:(s + 1) * sp]
            inv = self.pbf[b][:, :8 * sp].rearrange("p (c t) -> p c t", t=sp)
            gv = self.cols[:, o:o + 8].unsqueeze(2).to_broadcast([128, 8, sp])
        else:
            outv = dst[:, :, s * sp:(s + 1) * sp].rearrange("p (a b) t -> p a b t", b=gmod)
            inv = self.pbf[b][:, :8 * sp].rearrange("p (a b t) -> p a b t", b=gmod, t=sp)
            gv = self.cols[:, o:o + gmod].unsqueeze(1).unsqueeze(3).to_broadcast([128, 8 // gmod, gmod, sp])
        K.op("dve", lambda e: e.tensor_tensor(out=outv, in0=inv, in1=gv, op=ALU.mult),
             reads=[self.pbr[b], self.colsr], writes=list(dstr))

    def norm_to_T(self, gname, nsub, sp, T):
        for s in range(nsub):
            self.norm_sub(s, sp)
        for s in range(nsub):
            self.trans_sub(self.xn, self.xnr, self.actT, self.actr, s, sp, gname)

    def to_T(self, src, srcr, dst, dstr, nsub, sp, T, gname, gmod=8):
        K = self.K
        for c in range(8):
            b = self.bank()
            for s in range(nsub):
                K.op("pe", lambda e, b=b, s=s, c=c: e.transpose(self.pbf[b][:, s * sp:(s + 1) * sp],
                                                                src[:sp, s, c * 128:(c + 1) * 128],
                                                                self.ident[:sp, :sp]),
                     reads=[srcr[s], self.identr], writes=[self.pbr[b]], signal=(s == nsub - 1))
            K.op("act", lambda e, b=b, c=c: e.activation(out=dst[:, c, :T], in_=self.pbf[b][:, :T], func=AF.Copy,
                                                         scale=self.col(gname, c % gmod)),
                 reads=[self.pbr[b], self.colsr], writes=[dstr[c]])

    def proj_fm(self, wt, wres, ms, srcT, srcr, T, evac):
        ms = list(ms)
        if T * len(ms) <= 512 and len(ms) == 4:
            b = self.bank()
            for m in ms:
                pairs = [(wt[:, kc, m * 128:(m + 1) * 128], srcT[:, kc, :T]) for kc in range(8)]
                self.mm(self.pb[b][:, m * T:(m + 1) * T], pairs, [wres] + list(srcr), self.pbr[b], signal=(m == ms[-1]))
            evac(None, b)
            return
        if self.pending_trans is not None and T == 512:
            pend, self.pending_trans = self.pending_trans, None
            TA = T - 128
            head = ms[:3]
            hb = []
            for m in head:
                b = self.bank()
                hb.append(b)
                pairs = [(wt[:, kc, m * 128:(m + 1) * 128], srcT[:, kc, :TA]) for kc in range(8)]
                self.mm(self.pb[b][:, :TA], pairs, [wres] + list(srcr), self.pbr[b])
            pend()
            for m, b in zip(head, hb):
                pairs = [(wt[:, kc, m * 128:(m + 1) * 128], srcT[:, kc, TA:T]) for kc in range(8)]
                self.mm(self.pb[b][:, TA:T], pairs, [wres] + list(srcr), self.pbr[b])
                evac(m, b)
            ms = ms[3:]
        for m in ms:
            b = self.bank()
            pairs = [(wt[:, kc, m * 128:(m + 1) * 128], srcT[:, kc, :T]) for kc in range(8)]
            self.mm(self.pb[b][:, :T], pairs, [wres] + list(srcr), self.pbr[b])
            evac(m, b)

    def proj_tm(self, wt, wres, srcT, srcr, nsub, sp, evac):
        for s in range(nsub):
            b = self.bank()
            pairs = [(srcT[:, kc, s * sp:(s + 1) * sp], wt[:, kc, :]) for kc in range(8)]
            self.mm(self.pb[b][:sp, :], pairs, [wres] + list(srcr), self.pbr[b])
            evac(s, b)

    def sigmoid_to(self, b, T, out_ap, out_res, P=128, ncol=None, fold=None):
        K = self.K
        ncol = T if ncol is None else ncol
        t, tr = self.tmpbuf()
        out_res_l = out_res if isinstance(out_res, (list, tuple)) else [out_res]
        if fold is not None:
            tv = t[:P, :ncol].rearrange("p (m t) -> p m t", t=fold)
            K.op("act", lambda e: e.activation(out=t[:P, :ncol], in_=self.pb[b][:P, :ncol], func=AF.Exp, scale=-1.0),
                 reads=[self.pbr[b]], writes=[tr])
            K.op("act", lambda e: e.activation(out=t[:P, :ncol], in_=t[:P, :ncol], func=AF.Ln, bias=self.ncols[:P, 5:6]),
                 reads=[tr, self.ncolsr], writes=[tr])
            K.op("act", lambda e: e.activation(out=out_ap, in_=tv, func=AF.Exp, scale=-1.0),
                 reads=[tr], writes=list(out_res_l))
            return t, tr
        K.op("act", lambda e: e.activation(out=t[:P, :ncol], in_=self.pb[b][:P, :ncol], func=AF.Exp, scale=-1.0),
             reads=[self.pbr[b]], writes=[tr])
        K.op("act", lambda e: e.activation(out=t[:P, :ncol], in_=t[:P, :ncol], func=AF.Ln, bias=self.ncols[:P, 5:6]),
             reads=[tr, self.ncolsr], writes=[tr])
        K.op("act", lambda e: e.activation(out=out_ap, in_=t[:P, :ncol], func=AF.Exp, scale=-1.0),
             reads=[tr], writes=[out_res])
        return t, tr

    def mem_kv(self):
        K = self.K
        I, O = self.I, self.O
        for s in range(2):
            xa, xr = self.xnext(s)
            self.norm_sub(s, 128, src=xa, srcr=xr)
        for s in range(2):
            self.trans_sub(self.xn, self.xnr, self.actT, self.actr, s, 128, "g_mem")
        self.stage("memkv_norm")
        stg, stgr = self.r1_f32(0)
        stg2, stg2r = self.r1_f32(1)
        for which, (st, str_), outn in (("xk", (stg, stgr), "mk"), ("xv", (stg2, stg2r), "mv")):
            if which == "xv":
                self.stage("memkv_xk")
            for n in range(2):
                wt, wres = self.W(f"{which}{n}")
                for mt in range(2):
                    b = self.bank()
                    pairs = [(self.actT[:, kc, mt * 128:(mt + 1) * 128], wt[:, kc, :]) for kc in range(8)]
                    self.mm(self.pb[b][:, :], pairs, [wres] + self.actr, self.pbr[b])
                    g = mt * 2 + n
                    K.op("act", lambda e, b=b, g=g, st=st: e.activation(out=st[:, g, :], in_=self.pb[b][:, :], func=AF.Copy),
                         reads=[self.pbr[b]], writes=[str_[g * 2], str_[g * 2 + 1]])
                    if which == "xv":
                        K.op("dve", lambda e, b=b, mt=mt, n=n: e.tensor_copy(self.Vp[:, mt, n * 512:(n + 1) * 512], self.pb[b][:, :]),
                             reads=[self.pbr[b]], writes=[self.Vpr])
                    K.dma("sp", O[outn][mt * 128:(mt + 1) * 128, n * 512:(n + 1) * 512], st[:, g, :],
                          reads=[str_[g * 2], str_[g * 2 + 1]])
                if which == "xk":
                    for m in range(4):
                        b = self.bank()
                        pairs = [(wt[:, kc, m * 128:(m + 1) * 128], self.actT[:, kc, :256]) for kc in range(8)]
                        self.mm(self.pb[b][:, :256], pairs, [wres] + self.actr, self.pbr[b])
                        K.op("dve", lambda e, b=b, m=m, n=n: e.tensor_copy(self.KTp[:, n * 4 + m, :], self.pb[b][:, :256]),
                             reads=[self.pbr[b]], writes=[self.KTpr])

    def in_proj(self, nsub, sp, T, uslots, last_u_fp32=None):
        K = self.K
        gzT = self.att[:16, 0, :, :].rearrange("p a b -> p (a b)")
        gzr = self.attr[0]
        b = self.bank()
        pairs = [(self.wgz[:, kc, :], self.actT[:, kc, :T]) for kc in range(8)]
        self.mm(self.pb[b][:16, :T], pairs, [self.wgzr] + self.actr, self.pbr[b])
        K.op("act", lambda e, b=b: e.activation(out=gzT[:, :T], in_=self.pb[b][:16, :T], func=AF.Copy),
             reads=[self.pbr[b]], writes=[gzr])
        T1, T1r = self.r1_f32(0)
        Bc, Bcr = self.r1_f32(1)
        E1, E1r = self.r1_f32(2)
        E2, E2r = self.r1_f32(3)
        CL = 128 if T == 512 else ST
        nch = T // CL

        def decay_chain():
            for h in range(4):
                b = self.bank()
                self.mm(self.pb[b][:, :T], [(self.wgk[:, h * 128:(h + 1) * 128], gzT[:, :T])], [self.wgkr, gzr], self.pbr[b])
                K.op("act", lambda e, b=b, h=h: e.activation(out=T1[:, h, :T], in_=self.pb[b][:, :T], func=AF.Exp,
                                                             scale=-1.0, bias=self.ncols[:, h:h + 1]),
                     reads=[self.pbr[b], self.ncolsr], writes=T1r[2 * h:2 * h + 2])
                K.op("act", lambda e, h=h: e.activation(out=T1[:, h, :T], in_=T1[:, h, :T], func=AF.Ln,
                                                        bias=self.ncols[:, 5:6]),
                     reads=T1r[2 * h:2 * h + 2] + [self.ncolsr], writes=T1r[2 * h:2 * h + 2])
                rst = self.rst512 if T == 512 else self.rst64
                rstr = self.rst512r if T == 512 else self.rst64r
                K.op("dve", lambda e, h=h, rst=rst: e.tensor_tensor_scan(out=Bc[:, h, :T], data0=rst[:, :T], data1=T1[:, h, :T],
                                                                         initial=0.0, op0=ALU.mult, op1=ALU.add),
                     reads=T1r[2 * h:2 * h + 2] + [rstr], writes=Bcr[2 * h:2 * h + 2])
            CL = 128 if T == 512 else ST
            nch = T // CL
            Bv = Bc[:, :, :T].rearrange("p h (c t) -> p h c t", t=CL)
            Dv = T1[:, :, :T].rearrange("p h (c t) -> p h c t", t=CL)
            Bend = Bv[:, :, :, CL - 1:CL]
            K.op("dve", lambda e: e.tensor_tensor(out=Dv, in0=Bv, in1=Bend.to_broadcast([128, 4, nch, CL]), op=ALU.subtract),
                 reads=Bcr, writes=T1r)
            eB = self.eB
            K.op("act", lambda e: e.activation(out=eB[:, :, :nch], in_=Bv[:, :, :, CL - 1], func=AF.Exp, scale=-1.0 / 16),
                 reads=Bcr, writes=[self.eBr])
            K.op("act", lambda e: e.activation(out=E1[:, :, :T], in_=T1[:, :, :T], func=AF.Exp, scale=1.0 / 16),
                 reads=T1r, writes=E1r)
            K.op("act", lambda e: e.activation(out=E2[:, :, :T], in_=T1[:, :, :T], func=AF.Exp, scale=-1.0 / 16,
                                               bias=self.ncols[:, 4:5]),
                 reads=T1r + [self.ncolsr], writes=E2r)
        for n in range(2):
            wt, wres = self.W(f"v{n}")

            def ev(s, b, n=n):
                K.op("act", lambda e: e.activation(out=self.vt[:sp, s, n * 512:(n + 1) * 512], in_=self.pb[b][:sp, :],
                                                   func=AF.Copy), reads=[self.pbr[b]], writes=[self.vtr[s]])
            self.proj_tm(wt, wres, self.actT, self.actr, nsub, sp, ev)
        decay_chain()
        for n in range(2):
            wt, wres = self.W(f"og{n}")

            def ev(s, b, n=n):
                t, tr = self.tmpbuf()
                self.sigmoid_to(b, 512, t[:sp, :], tr, P=sp)
                K.op("dve", lambda e: e.tensor_tensor(out=self.ogs[:sp, s, n * 512:(n + 1) * 512], in0=self.pb[b][:sp, :],
                                                     in1=t[:sp, :], op=ALU.mult),
                     reads=[self.pbr[b], tr], writes=[self.ogsr[s]])
            self.proj_tm(wt, wres, self.actT, self.actr, nsub, sp, ev)
        for n in range(2):
            wt, wres = self.W(f"u{n}")

            def ev(s, b, n=n):
                sl = uslots[s]
                K.op("act", lambda e: e.activation(out=self.ur[:sp, sl, n * 512:(n + 1) * 512], in_=self.pb[b][:sp, :],
                                                   func=AF.Copy), reads=[self.pbr[b]], writes=[self.urr[sl]])
                if last_u_fp32 is not None and s == nsub - 1:
                    st, str_ = last_u_fp32
                    K.op("dve", lambda e: e.tensor_copy(st[:sp, n * 512:(n + 1) * 512], self.pb[b][:sp, :]),
                         reads=[self.pbr[b]], writes=list(str_))
            self.proj_tm(wt, wres, self.actT, self.actr, nsub, sp, ev)
        for gt, gr, nm in ((self.ga, self.gar, "ga"), (self.gb, self.gbr, "gb")):
            for n in range(2):
                wt, wres = self.W(f"{nm}{n}")

                def ev(m, b, n=n, gt=gt, gr=gr):
                    if m is None:
                        self.sigmoid_to(b, T, gt[:, n * 4:n * 4 + 4, :T], gr[n * 4:n * 4 + 4], ncol=4 * T, fold=T)
                        return
                    self.sigmoid_to(b, T, gt[:, n * 4 + m, :T], gr[n * 4 + m])
                self.proj_fm(wt, wres, range(4), self.actT, self.actr, T, ev)
        wt, wres = self.W("k")

        def evk(h, b):
            if h is None:
                K.op("dve", lambda e: e.tensor_tensor(out=self.kd[:, :, :T], in0=self.pb[b][:, :4 * T].rearrange("p (m t) -> p m t", t=T),
                                                     in1=E1[:, :, :T], op=ALU.mult),
                     reads=[self.pbr[b]] + E1r, writes=self.kdr)
                return
            K.op("dve", lambda e: e.tensor_tensor(out=self.kd[:, h, :T], in0=self.pb[b][:, :T], in1=E1[:, h, :T], op=ALU.mult),
                 reads=[self.pbr[b]] + E1r[2 * h:2 * h + 2], writes=[self.kdr[h]])
        self.proj_fm(wt, wres, range(4), self.actT, self.actr, T, evk)
        wt, wres = self.W("q")

        def evq(h, b):
            if h is None:
                K.op("dve", lambda e: e.tensor_tensor(out=self.qi[:, :, :T], in0=self.pb[b][:, :4 * T].rearrange("p (m t) -> p m t", t=T),
                                                     in1=E2[:, :, :T], op=ALU.mult),
                     reads=[self.pbr[b]] + E2r, writes=self.qir)
                return
            K.op("dve", lambda e: e.tensor_tensor(out=self.qi[:, h, :T], in0=self.pb[b][:, :T], in1=E2[:, h, :T], op=ALU.mult),
                 reads=[self.pbr[b]] + E2r[2 * h:2 * h + 2], writes=[self.qir[h]])
        self.proj_fm(wt, wres, range(4), self.actT, self.actr, T, evq)
        for c in range(nch if T == 512 else 1):
            cl = 128 if T == 512 else 64
            b = self.bank()
            for h in range(4):
                K.op("pe", lambda e, b=b, h=h, c=c, cl=cl: e.transpose(self.pbf[b][:cl, h * 128:(h + 1) * 128],
                                                                      self.kd[:, h, c * cl:(c + 1) * cl], self.ident[:, :]),
                     reads=[self.kdr[h], self.identr], writes=[self.pbr[b]], signal=(h == 3))
            K.op("act", lambda e, b=b, c=c, cl=cl: e.activation(out=self.kdt[:cl, c, :], in_=self.pbf[b][:cl, :512], func=AF.Copy),
                 reads=[self.pbr[b]], writes=[self.kdtr[c]])

    def branches_out(self, o2T, o2Tr, pooledT, pooledTr, nsub, sp, T):
        K = self.K
        mT, mTr = self.r1_bf(2)
        wts = {}
        for m in range(8):
            if m % 4 == 0:
                wts["b"] = self.W(f"b{m // 4}")
                wts["a"] = self.W(f"a{m // 4}")
            bb = self.bank()
            wt, wres = wts["b"]
            self.mm(self.pb[bb][:, :T], [(wt[:, kc, (m % 4) * 128:(m % 4 + 1) * 128], pooledT[:, kc, :T]) for kc in range(8)],
                    [wres] + list(pooledTr), self.pbr[bb])
            ba = self.bank()
            wt, wres = wts["a"]
            self.mm(self.pb[ba][:, :T], [(wt[:, kc, (m % 4) * 128:(m % 4 + 1) * 128], o2T[:, kc, :T]) for kc in range(8)],
                    [wres] + list(o2Tr), self.pbr[ba])
            t1, t1r = self.tmpbuf()
            t2, t2r = self.tmpbuf()
            K.op("dve", lambda e, bb=bb, m=m, t1=t1: e.tensor_tensor(out=t1[:, :T], in0=self.pb[bb][:, :T], in1=self.gb[:, m, :T], op=ALU.mult),
                 reads=[self.pbr[bb], self.gbr[m]], writes=[t1r])
            K.op("dve", lambda e, ba=ba, m=m, t2=t2: e.tensor_tensor(out=t2[:, :T], in0=self.pb[ba][:, :T], in1=self.ga[:, m, :T], op=ALU.mult),
                 reads=[self.pbr[ba], self.gar[m]], writes=[t2r])
            K.op("dve", lambda e, m=m, t1=t1, t2=t2: e.tensor_tensor(out=mT[:, m, :T], in0=t1[:, :T], in1=t2[:, :T], op=ALU.add),
                 reads=[t1r, t2r], writes=[mTr[m]])
        self.resid_norm("o", mT, mTr, nsub, sp, "g_x")

    def pool_mix(self, dT, dTr, pT, pTr, T):
        K = self.K
        wt, wres = self.W("pm")
        for g in range(4):
            for dch in range(2):
                b = self.bank()
                pairs = [(wt[:, g * 2 + kc, dch * 128:(dch + 1) * 128], dT[:, 2 * g + kc, :T]) for kc in range(2)]
                self.mm(self.pb[b][:, :T], pairs, [wres, dTr[2 * g], dTr[2 * g + 1]], self.pbr[b])
                K.op("act", lambda e, b=b, g=g, dch=dch: e.activation(out=pT[:, 2 * g + dch, :T], in_=self.pb[b][:, :T], func=AF.Copy,
                                                                    scale=self.col("pscale", 2 * g + dch)),
                     reads=[self.pbr[b], self.colsr], writes=[pTr[2 * g + dch]])

    def resid_norm(self, wname, srcT, srcr, nsub, sp, gname):
        K = self.K
        ws = [self.W(f"{wname}0"), self.W(f"{wname}1")]
        for s in range(nsub):
            for n in range(2):
                wt, wres = ws[n]
                b = self.bank()
                pairs = [(srcT[:, kc, s * sp:(s + 1) * sp], wt[:, kc, :]) for kc in range(8)]
                self.mm(self.pb[b][:sp, :], pairs, [wres] + list(srcr), self.pbr[b])
                K.op("dve", lambda e, s=s, n=n, b=b: e.tensor_tensor(out=self.xh[:sp, s, n * 512:(n + 1) * 512],
                                                                    in0=self.xh[:sp, s, n * 512:(n + 1) * 512],
                                                                    in1=self.pb[b][:sp, :], op=ALU.add),
                     reads=[self.pbr[b], self.xhr[s]], writes=[self.xhr[s]])
            if s >= 1:
                self.trans_sub(self.xn, self.xnr, self.actT, self.actr, s - 1, sp, gname)
            self.norm_sub(s, sp)
        if nsub == 4 and sp == 128 and HIDE_TAIL:
            self.pending_trans = lambda: self.trans_sub(self.xn, self.xnr, self.actT, self.actr, nsub - 1, sp, gname)
        else:
            self.trans_sub(self.xn, self.xnr, self.actT, self.actr, nsub - 1, sp, gname)

    def out_resid(self, wname, srcT, srcr, nsub, sp):
        K = self.K
        for n in range(2):
            wt, wres = self.W(f"{wname}{n}")

            def ev(s, b, n=n):
                K.op("dve", lambda e: e.tensor_tensor(out=self.xh[:sp, s, n * 512:(n + 1) * 512],
                                                     in0=self.xh[:sp, s, n * 512:(n + 1) * 512],
                                                     in1=self.pb[b][:sp, :], op=ALU.add),
                     reads=[self.pbr[b], self.xhr[s]], writes=[self.xhr[s]])
            self.proj_tm(wt, wres, srcT, srcr, nsub, sp, ev)

    def softmax_stages(self, hb, P, Pn, Pnr, par=0, c_eng="dve"):
        K = self.K
        sm = self.sm2r[par]
        cb = (8, 64, 80)[par]
        mx = self.small[:P, cb:cb + 4]
        nmx = self.small[:P, cb + 4:cb + 8]
        ssum = self.small[:P, cb + 8:cb + 12]
        rs = self.small[:P, cb + 12:cb + 16]
        bufs = {}

        def stage_a():
            h = 0
            while h < 4:
                b, co = hb[h]
                if h + 1 < 4 and hb[h + 1] == (b, co + 256):
                    K.op("dve", lambda e, b=b, h=h, co=co: e.reduce_max(
                        out=self.small[:P, cb + 4 + h:cb + 4 + h + 2], in_=self.pb[b][:P, co:co + 512].rearrange("p (h m) -> p h m", m=256),
                        axis=AX.X, negate=True),
                         reads=[self.pbr[b]], writes=[sm])
                    h += 2
                else:
                    K.op("dve", lambda e, b=b, h=h, co=co: e.reduce_max(out=self.small[:P, cb + 4 + h:cb + 4 + h + 1], in_=self.pb[b][:P, co:co + 256],
                                                                        axis=AX.X, negate=True),
                         reads=[self.pbr[b]], writes=[sm])
                    h += 1

        def stage_b():
            bufs["pf"] = [self.tmpbuf(), self.tmpbuf()]
            for h, (b, co) in enumerate(hb):
                pf, pfr = bufs["pf"][h // 2]
                K.op("act", lambda e, b=b, h=h, pf=pf, co=co: e.activation(out=pf[:P, (h % 2) * 256:(h % 2 + 1) * 256],
                                                                           in_=self.pb[b][:P, co:co + 256],
                                                                           func=AF.Exp, scale=1.0, bias=self.small[:P, cb + 4 + h:cb + 5 + h],
                                                                           accum_out=self.small[:P, cb + 8 + h:cb + 9 + h]),
                     reads=[self.pbr[b], sm], writes=[pfr, sm])

        def stage_c():
            K.op("dve", lambda e: e.reciprocal(out=rs, in_=ssum), reads=[sm], writes=[sm])
            for hp, (pf, pfr) in enumerate(bufs["pf"]):
                K.op(c_eng, lambda e, hp=hp, pf=pf: e.tensor_tensor(
                    out=Pn[:P, 2 * hp:2 * hp + 2, :], in0=pf[:P, :].rearrange("p (h m) -> p h m", m=256),
                    in1=self.small[:P, cb + 12 + 2 * hp:cb + 14 + 2 * hp].unsqueeze(2).to_broadcast([P, 2, 256]), op=ALU.mult),
                     reads=[pfr, sm], writes=list(Pnr))
        return stage_a, stage_b, stage_c

    def softmax_rows(self, hb, P, Pn, Pnr, par=0):
        fa, fb, fc = self.softmax_stages(hb, P, Pn, Pnr, par)
        fa()
        fb()
        fc()

    def mlp_final(self, nsub, sp, T, ydst, yres_fn, pro_a=None, pro_b=None):
        K = self.K
        hid = self.R1[:, :].rearrange("p (c t) -> p c t", t=512)
        hidr = self.R1r
        for i in range(8):
            wt, wres = self.W(f"up{i}")

            def ev(m, b, i=i):
                t, tr = self.tmpbuf()
                if m is None:
                    K.op("act", lambda e: e.activation(out=t[:, :4 * T], in_=self.pb[b][:, :4 * T], func=AF.Relu),
                         reads=[self.pbr[b]], writes=[tr])
                    K.op("dve", lambda e: e.scalar_tensor_tensor(out=hid[:, 4 * i:4 * i + 4, :T],
                                                                in0=self.pb[b][:, :4 * T].rearrange("p (m t) -> p m t", t=T), scalar=0.0,
                                                                in1=t[:, :4 * T].rearrange("p (m t) -> p m t", t=T), op0=ALU.max, op1=ALU.mult),
                         reads=[self.pbr[b], tr], writes=hidr[4 * i:4 * i + 4])
                    return
                K.op("act", lambda e: e.activation(out=t[:, :T], in_=self.pb[b][:, :T], func=AF.Relu),
                     reads=[self.pbr[b]], writes=[tr])
                K.op("dve", lambda e: e.scalar_tensor_tensor(out=hid[:, 4 * i + m, :T], in0=self.pb[b][:, :T], scalar=0.0,
                                                            in1=t[:, :T], op0=ALU.max, op1=ALU.mult),
                     reads=[self.pbr[b], tr], writes=[hidr[4 * i + m]])
            self.proj_fm(wt, wres, range(4), self.actT, self.actr, T, ev)
        if pro_a is not None:
            pro_a()
        for n in range(2):
            bs = [self.bank() for _ in range(nsub)]
            for j in range(4):
                wt, wres = self.W(f"dn{j}_{n}")
                for s in range(nsub):
                    pairs = [(hid[:, 8 * j + kc, s * sp:(s + 1) * sp], wt[:, kc, :]) for kc in range(8)]
                    self.mm(self.pb[bs[s]][:sp, :], pairs, [wres] + hidr[8 * j:8 * j + 8], self.pbr[bs[s]],
                            start=(j == 0), stop=(j == 3))
            for s in range(nsub):
                b = bs[s]
                K.op("dve", lambda e, s=s, b=b, n=n: e.tensor_tensor(out=self.xh[:sp, s, n * 512:(n + 1) * 512],
                                                                    in0=self.xh[:sp, s, n * 512:(n + 1) * 512],
                                                                    in1=self.pb[b][:sp, :], op=ALU.add),
                     reads=[self.pbr[b], self.xhr[s]], writes=[self.xhr[s]])
            if n == 0 and pro_b is not None:
                pro_b()
        yst = self.R1[:, 0:8192].bitcast(F32).rearrange("p (s d) -> p s d", d=D)
        ss = self.small[:sp, 56:56 + nsub]
        rs = self.small[:sp, 60:60 + nsub]
        for s in range(nsub):
            K.op("act", lambda e, s=s: e.activation(out=self.junk2[:sp, :], in_=self.xh[:sp, s, :], func=AF.Square,
                                                    accum_out=self.small[:sp, 56 + s:57 + s]),
                 reads=[self.xhr[s], self.smallr], writes=[self.junk2r, self.smallr])
        self.rstd_from_ss(ss, rs, 1.0 / D, sp)
        for s in range(nsub):
            K.op("dve", lambda e, s=s: e.scalar_tensor_tensor(out=yst[:sp, s, :], in0=self.xh[:sp, s, :],
                                                             scalar=self.small[:sp, 60 + s:61 + s], in1=self.gfin[:sp, :],
                                                             op0=ALU.mult, op1=ALU.mult),
                 reads=[self.xhr[s], self.smallr, self.gfinr], writes=self.R1r[4 * s:4 * s + 4])
            K.dma("sp", yres_fn(s), yst[:sp, s, :], reads=self.R1r[4 * s:4 * s + 4])

    def prompt_tile(self, ti):
        K = self.K
        I, O = self.I, self.O
        T, nsub, sp = 512, 4, 128
        if ti == 0:
            self.norm_to_T("g_mix", nsub, sp, T)
        else:
            for s in range(nsub):
                xa, xr = self.xnext(s)
                K.op("dve", lambda e, s=s, xa=xa: e.tensor_copy(self.xh[:, s, :], xa), reads=list(xr), writes=[self.xhr[s]])
        self.dbg("xnT", self.actT[:, :, :], self.actr, [128, 8, 512])
        uslots = [(ti * 4 + s) % 5 for s in range(4)]
        last = None
        if ti == SEQ // 512 - 1:
            last = self.r1_dummy_ufp()
        self.in_proj(nsub, sp, T, uslots, last_u_fp32=last)
        self.dbg("kd", self.kd[:, :, :], self.kdr, [128, 4, 512])
        self.dbg("qi", self.qi[:, :, :], self.qir, [128, 4, 512])
        if last is not None:
            K.dma("sp", O["spp"][:, :], last[0][113:128, :], reads=list(last[1]))
        o2 = self.xn
        o2r = self.xnr
        for c in range(4):
            cs = slice(c * 128, (c + 1) * 128)
            b = self.bank()
            for h in range(4):
                self.mm(self.pb[b][:, h * 128:(h + 1) * 128], [(self.kd[:, h, cs], self.qi[:, h, cs])],
                        [self.kdr[h], self.qir[h]], self.pbr[b], signal=(h == 3))
            K.op("dve", lambda e, b=b, c=c: e.tensor_tensor(
                out=self.att[:, c, :, :], in0=self.pb[b][:, :].rearrange("p (h t) -> p h t", t=128),
                in1=self.maskT[:, :].unsqueeze(1).to_broadcast([128, 4, 128]), op=ALU.mult),
                 reads=[self.pbr[b], self.maskTr], writes=[self.attr[c]])
            for hp in range(2):
                b2 = self.bank()
                for h in (2 * hp, 2 * hp + 1):
                    self.mm(self.pb[b2][:, (h % 2) * 256:(h % 2 + 1) * 256],
                            [(self.kdt[:, c, h * 128:(h + 1) * 128], self.vt[:, c, h * 256:(h + 1) * 256])],
                            [self.kdtr[c], self.vtr[c]], self.pbr[b2], signal=(h % 2 == 1))
                for h in (2 * hp, 2 * hp + 1):
                    K.op("act", lambda e, h=h, c=c: e.activation(out=self.Sb[:, c, h, :], in_=self.S[:, h, :], func=AF.Copy,
                                                                 scale=self.eB[:, h, c:c + 1]),
                         reads=[self.Sr[h], self.eBr], writes=[self.Sbr[c * 4 + h]])
                    K.op("dve", lambda e, h=h, b2=b2, c=c: e.scalar_tensor_tensor(
                        out=self.S[:, h, :], in0=self.S[:, h, :], scalar=self.eB[:, h, c:c + 1],
                        in1=self.pb[b2][:, (h % 2) * 256:(h % 2 + 1) * 256], op0=ALU.mult, op1=ALU.add),
                         reads=[self.Sr[h], self.eBr, self.pbr[b2]], writes=[self.Sr[h]])
        dT, dTr = self.r1_bf(0)
        pT, pTr = self.r1_bf(1)

        def pool_cc(cc):
            g = cc // 2
            b = self.bank()
            for s in range(4):
                cur = uslots[s]
                prev = (cur + 4) % 5
                csl = slice(cc * 128, (cc + 1) * 128)
                if ti == 0 and s == 0:
                    pairs = [(self.ur[:, cur, csl], self.pmat[:, 2, g, :]), (self.ur[:, cur, csl], self.pmat[:, 3, g, :])]
                    rd = [self.urr[cur], self.pmatr]
                else:
                    pairs = [(self.ur[:, prev, csl], self.pmat[:, 1, g, :]), (self.ur[:, cur, csl], self.pmat[:, 0, g, :])]
                    rd = [self.urr[cur], self.urr[prev], self.pmatr]
                self.mm(self.pb[b][:, s * 128:(s + 1) * 128], pairs, rd, self.pbr[b], signal=(s == 3))
            K.op("act", lambda e, b=b, cc=cc: e.activation(out=dT[:, cc, :], in_=self.pb[b][:, :], func=AF.Copy),
                 reads=[self.pbr[b]], writes=[dTr[cc]])

        for cc in range(4):
            pool_cc(cc)
        for c in range(4):
            cs = slice(c * 128, (c + 1) * 128)
            sr = self.osr[c]
            obanks = []
            for hp in range(2):
                b3 = self.bank()
                obanks.append(b3)
                for h in (2 * hp, 2 * hp + 1):
                    self.mm(self.pb[b3][:, (h % 2) * 256:(h % 2 + 1) * 256],
                            [(self.att[:, c, h, :], self.vt[:, c, h * 256:(h + 1) * 256]),
                             (self.qi[:, h, cs], self.Sb[:, c, h, :])],
                            [self.attr[c], self.vtr[c], self.qir[h], self.Sbr[c * 4 + h]], self.pbr[b3], signal=(h % 2 == 1))
                for h in (2 * hp, 2 * hp + 1):
                    K.op("act", lambda e, h=h, b3=b3, c=c: e.activation(out=self.junk[:, :256], in_=self.pb[b3][:, (h % 2) * 256:(h % 2 + 1) * 256],
                                                                        func=AF.Square, accum_out=self.small[:, 24 + c * 4 + h:25 + c * 4 + h]),
                         reads=[self.pbr[b3], sr], writes=[self.junkr, sr])
            self.rstd_from_ss(self.small[:, 24 + c * 4:28 + c * 4], self.small[:, 40 + c * 4:44 + c * 4], 1.0 / 256, 128, sr=sr)
            for h in range(4):
                b3 = obanks[h // 2]
                K.op("dve", lambda e, h=h, b3=b3, c=c: e.scalar_tensor_tensor(
                    out=o2[:, c, h * 256:(h + 1) * 256], in0=self.pb[b3][:, (h % 2) * 256:(h % 2 + 1) * 256],
                    scalar=self.small[:, 40 + c * 4 + h:41 + c * 4 + h], in1=self.ogs[:, c, h * 256:(h + 1) * 256],
                    op0=ALU.mult, op1=ALU.mult),
                     reads=[self.pbr[b3], sr, self.ogsr[c]], writes=[o2r[c]])
        for cc in range(4, 8):
            pool_cc(cc)
        self.pool_mix(dT, dTr, pT, pTr, T)
        for c in range(4):
            self.trans_sub(o2, o2r, self.actT, self.actr, c, sp, "g_gla", gmod=2)
        self.branches_out(self.actT, self.actr, pT, pTr, nsub, sp, T)
        self.dbg("h1", self.xh[:, :, :], self.xhr, [128, 4, 1024])
        qx, qxr = self.r1_bf(0)
        PT, PTr = self.r1_bf(1)
        ox, oxr = self.r1_bf(3)
        for n in range(2):
            wt, wres = self.W(f"xq{n}")

            def ev(m, b, n=n):
                K.op("act", lambda e: e.activation(out=qx[:, n * 4 + m, :], in_=self.pb[b][:, :], func=AF.Copy, scale=1.0 / 16),
                     reads=[self.pbr[b]], writes=[qxr[n * 4 + m]])
            self.proj_fm(wt, wres, range(4), self.actT, self.actr, T, ev)
        Pns = [self.att[:, 2 * par:2 * par + 2, :, :].rearrange("p a h t -> p (a h t)").rearrange("p (h m) -> p h m", m=256)
               for par in range(2)]
        Pnrs = [[self.attr[0], self.attr[1]], [self.attr[2], self.attr[3]]]

        def scores(s):
            ss_ = slice(s * 128, (s + 1) * 128)
            banks = []
            for hp in range(2):
                b = self.bank()
                banks.append(b)
                for h in (2 * hp, 2 * hp + 1):
                    pairs = [(qx[:, 2 * h + dc, ss_], self.KTp[:, 2 * h + dc, :]) for dc in range(2)]
                    self.mm(self.pb[b][:, (h % 2) * 256:(h % 2 + 1) * 256], pairs,
                            [qxr[2 * h], qxr[2 * h + 1], self.KTpr], self.pbr[b], signal=(h % 2 == 1))
            return self.softmax_stages([(banks[h // 2], (h % 2) * 256) for h in range(4)], 128, Pns[s % 2], Pnrs[s % 2], par=s % 3,
                                       c_eng=POOL_SOFTMAX)

        def ptrans(s):
            ss_ = slice(s * 128, (s + 1) * 128)
            Pn, Pnr = Pns[s % 2], Pnrs[s % 2]
            b = self.bank()
            for h in range(4):
                for mt in range(2):
                    j = h * 2 + mt
                    K.op("pe", lambda e, b=b, h=h, mt=mt, j=j: e.transpose(self.pbf[b][:, j * 128:(j + 1) * 128],
                                                                          Pn[:, h, mt * 128:(mt + 1) * 128], self.ident[:, :]),
                         reads=list(Pnr) + [self.identr], writes=[self.pbr[b]], signal=(j == 7))
            K.op("act", lambda e, b=b, ss_=ss_: e.activation(out=PT[:, :, ss_], in_=self.pbf[b][:, :].rearrange("p (j t) -> p j t", t=128),
                                                             func=AF.Copy),
                 reads=[self.pbr[b]], writes=PTr)

        st = {}
        st[0] = scores(0); st[0][0](); st[0][1]()
        st[1] = scores(1); st[1][0]()
        st[2] = scores(2); st[2][0]()
        st[0][2](); st[1][1]()
        ptrans(0)
        st[3] = scores(3); st[3][0]()
        st[1][2](); st[2][1]()
        ptrans(1)
        st[2][2](); st[3][1]()
        ptrans(2)
        st[3][2]()
        ptrans(3)
        for h in range(4):
            for dc in range(2):
                b = self.bank()
                pairs = [(self.Vp[:, mt, h * 256 + dc * 128:h * 256 + (dc + 1) * 128], PT[:, h * 2 + mt, :]) for mt in range(2)]
                self.mm(self.pb[b][:, :], pairs, [self.Vpr, PTr[h * 2], PTr[h * 2 + 1]], self.pbr[b])
                K.op("act", lambda e, b=b, h=h, dc=dc: e.activation(out=ox[:, 2 * h + dc, :], in_=self.pb[b][:, :], func=AF.Copy),
                     reads=[self.pbr[b]], writes=[oxr[2 * h + dc]])
        self.resid_norm("xo", ox, oxr, nsub, sp, "g_mlp")
        self.dbg("h2", self.xh[:, :, :], self.xhr, [128, 4, 1024])
        pro_a = pro_b = None
        if ti + 1 == SEQ // 512 and self.do_sample:
            def pro_a():
                xa, xr = self.xnext(0)
                K.dma("sp", xa[:SB * ST, :], I["xs"][:, :], writes=xr)
                self.norm_sub(0, SB * ST, src=xa[:SB * ST, :], srcr=xr)

            def pro_b():
                self.trans_sub(self.xn, self.xnr, self.actT, self.actr, 0, SB * ST, "g_mix")
        if ti + 1 < SEQ // 512:
            def pro_a():
                for s in range(nsub):
                    xa, xr = self.xnext(s)
                    r0 = (ti + 1) * 512 + s * 128
                    K.dma("sp", xa, I["xp"][r0:r0 + 128, :], writes=xr)
                for s in range(nsub):
                    xa, xr = self.xnext(s)
                    self.norm_sub(s, sp, src=xa, srcr=xr)

            def pro_b():
                for s in range(nsub):
                    self.trans_sub(self.xn, self.xnr, self.actT, self.actr, s, sp, "g_mix")
        self.mlp_final(nsub, sp, T, None, lambda s: O["yp"][ti * 512 + s * 128:ti * 512 + (s + 1) * 128, :],
                       pro_a=pro_a, pro_b=pro_b)

    def r1_dummy_ufp(self):
        return self.ufp, [self.xnr[0], self.xnr[1]]

    def prompt_finish(self):
        K = self.K
        O = self.O
        for h in range(4):
            K.dma("sp", O["sgp"][h, :, :], self.S[:, h, :], reads=[self.Sr[h]])

    def sample_pass(self):
        K = self.K
        I, O = self.I, self.O
        T, nsub, sp = 64, 1, 64
        NB = SB
        xa, xr = self.xnext(0)
        K.op("dve", lambda e: e.tensor_copy(self.xh[:sp, 0, :], xa[:sp, :]), reads=list(xr), writes=[self.xhr[0]])
        ufp, ufpr = self.r1_dummy_ufp()
        self.in_proj(nsub, sp, T, [0], last_u_fp32=(ufp, ufpr))
        qblk = self.Sb[:, :, :, :].rearrange("p a b c -> p (a b c)").rearrange("p (h b t) -> p h b t", h=4, b=NB)
        qblkr = self.Sbr
        K.op("dve", lambda e: e.tensor_tensor(out=qblk, in0=self.qi[:, :, :T].unsqueeze(2).to_broadcast([128, 4, NB, T]),
                                             in1=self.bmq[:, :, :].unsqueeze(1).to_broadcast([128, 4, NB, T]), op=ALU.mult),
             reads=self.qir + [self.bmqr], writes=qblkr)
        kblk = self.R1[:sp, 0:8192].rearrange("p (b d) -> p b d", d=512)
        kblkr = self.R1r[0:16]
        K.op("dve", lambda e: e.tensor_tensor(out=kblk, in0=self.kdt[:sp, 0, :].unsqueeze(1).to_broadcast([sp, NB, 512]),
                                             in1=self.bmk[:sp, :].unsqueeze(2).to_broadcast([sp, NB, 512]), op=ALU.mult),
             reads=[self.kdtr[0], self.bmkr], writes=kblkr)
        b0 = self.bank()
        for h in range(4):
            self.mm(self.pb[b0][:sp, h * 64:(h + 1) * 64], [(self.kd[:, h, :T], self.qi[:, h, :T])],
                    [self.kdr[h], self.qir[h]], self.pbr[b0], signal=(h == 3))
        K.op("dve", lambda e: e.tensor_tensor(out=self.att[:sp, 0, :, :64],
                                             in0=self.pb[b0][:sp, :256].rearrange("p (h t) -> p h t", t=64),
                                             in1=self.smask[:, :].unsqueeze(1).to_broadcast([sp, 4, 64]), op=ALU.mult),
             reads=[self.pbr[b0], self.smaskr], writes=[self.attr[0]])
        ob = self.hold(4)
        for h in range(4):
            self.mm(self.pb[ob[h]][:sp, :256], [(self.att[:sp, 0, h, :64], self.vt[:sp, 0, h * 256:(h + 1) * 256])],
                    [self.attr[0], self.vtr[0]], self.pbr[ob[h]], start=True, stop=False)
        s0in = [self.xh[:, 1, :].rearrange("p (h v) -> p h v", v=256), self.xh[:, 2, :].rearrange("p (h v) -> p h v", v=256),
                self.Vp[:, :, :].rearrange("p a b -> p (a b)").bitcast(F32).rearrange("p (h v) -> p h v", v=256),
                self.S[:, :, :]]
        s0inr = [self.xhr[1], self.xhr[2], self.Vpr, self.Sr]
        NS0 = 4
        s0p = [self.vt[:, 1, :].rearrange("p (h v) -> p h v", v=256), self.vt[:, 2, :].rearrange("p (h v) -> p h v", v=256)]
        s0pr = [self.vtr[1], self.vtr[2]]
        snew = [self.xh[:, 3, :].rearrange("p (h v) -> p h v", v=256),
                self.KTp[:, :, :].rearrange("p a b -> p (a b)").bitcast(F32).rearrange("p (h v) -> p h v", v=256)]
        snewr = [self.xhr[3], self.KTpr]
        def s0w(i):
            r = s0inr[i % NS0]
            return list(r) if isinstance(r, (list, tuple)) else [r]

        for b in range(NS0 - 1):
            K.dma("sp", s0in[b], I["sgla"][b].rearrange("h k v -> k h v"), writes=s0w(b))
        cpr = Res("spscopy")
        K.dma("sp", O["sps"][:, 0:11, :], I["spool"][:, 4:15, :], writes=[cpr])
        for b in range(NB):
            K.dma("sp", O["sps"][b, 11:15, :], ufp[b * ST:(b + 1) * ST, :], reads=list(ufpr))
        for b in range(NB):
            bb = b % 2
            if b + NS0 - 1 < NB:
                K.dma("sp", s0in[(b + NS0 - 1) % NS0], I["sgla"][b + NS0 - 1].rearrange("h k v -> k h v"), writes=s0w(b + NS0 - 1))
            for h in range(4):
                K.op("act", lambda e, h=h, b=b, bb=bb: e.activation(out=s0p[bb][:, h, :], in_=s0in[b % NS0][:, h, :], func=AF.Copy,
                                                                    scale=self.eB[:, h, b:b + 1]),
                     reads=s0w(b) + [self.eBr], writes=[s0pr[bb]])
            for h in range(4):
                self.mm(self.pb[ob[h]][:sp, :256], [(qblk[:, h, b, :], s0p[bb][:, h, :])], qblkr + [s0pr[bb]], self.pbr[ob[h]],
                        start=False, stop=(b == NB - 1))
            for hp in range(2):
                b2 = self.bank()
                for h in (2 * hp, 2 * hp + 1):
                    self.mm(self.pb[b2][:, (h % 2) * 256:(h % 2 + 1) * 256],
                            [(kblk[:, b, h * 128:(h + 1) * 128], self.vt[:sp, 0, h * 256:(h + 1) * 256])],
                            kblkr + [self.vtr[0]], self.pbr[b2], signal=(h % 2 == 1))
                for h in (2 * hp, 2 * hp + 1):
                    K.op("dve", lambda e, h=h, b=b, bb=bb, b2=b2: e.scalar_tensor_tensor(
                        out=snew[bb][:, h, :], in0=s0in[b % NS0][:, h, :], scalar=self.eB[:, h, b:b + 1],
                        in1=self.pb[b2][:, (h % 2) * 256:(h % 2 + 1) * 256], op0=ALU.mult, op1=ALU.add),
                         reads=s0w(b) + [self.eBr, self.pbr[b2]], writes=[snewr[bb]])
            K.dma("pool", O["sgs"][b].rearrange("h k v -> k h v"), snew[bb], reads=[snewr[bb]])
        o2, o2r = self.xn, self.xnr
        for h in range(4):
            K.op("act", lambda e, h=h: e.activation(out=self.junk[:sp, :256], in_=self.pb[ob[h]][:sp, :256], func=AF.Square,
                                                    accum_out=self.small[:sp, 24 + h:25 + h]),
                 reads=[self.pbr[ob[h]], self.smallr], writes=[self.junkr, self.smallr])
        self.rstd_from_ss(self.small[:sp, 24:28], self.small[:sp, 40:44], 1.0 / 256, sp)
        for h in range(4):
            K.op("dve", lambda e, h=h: e.scalar_tensor_tensor(
                out=o2[:sp, 0, h * 256:(h + 1) * 256], in0=self.pb[ob[h]][:sp, :256], scalar=self.small[:sp, 40 + h:41 + h],
                in1=self.ogs[:sp, 0, h * 256:(h + 1) * 256], op0=ALU.mult, op1=ALU.mult),
                 reads=[self.pbr[ob[h]], self.smallr, self.ogsr[0]], writes=[o2r[0]])
        self.release(ob)
        self.trans_sub(o2, o2r, self.actT, self.actr, 0, sp, "g_gla", gmod=2)
        for kt in range(2):
            K.dma("pool", self.ur[:120, 1 + kt, :], I["spool"][kt * 8:(kt + 1) * 8].rearrange("b j d -> (b j) d"),
                  writes=[self.urr[1 + kt]])
        dT, dTr = self.r1_bf(0)
        pT, pTr = self.r1_bf(1)
        for cc in range(8):
            g = cc // 2
            csl = slice(cc * 128, (cc + 1) * 128)
            b = self.bank()
            pairs = [(self.ur[:120, 1, csl], self.smb[:, 0, g, :]), (self.ur[:120, 2, csl], self.smb[:, 1, g, :]),
                     (self.ur[:sp, 0, csl], self.smu[:, g, :])]
            self.mm(self.pb[b][:, :T], pairs, [self.urr[0], self.urr[1], self.urr[2], self.smbr, self.smur], self.pbr[b])
            K.op("act", lambda e, b=b, cc=cc: e.activation(out=dT[:, cc, :T], in_=self.pb[b][:, :T], func=AF.Copy),
                 reads=[self.pbr[b]], writes=[dTr[cc]])
        self.pool_mix(dT, dTr, pT, pTr, T)
        self.branches_out(self.actT, self.actr, pT, pTr, nsub, sp, T)
        qx, qxr = self.r1_bf(0)
        for n in range(2):
            wt, wres = self.W(f"xq{n}")

            def ev(m, b, n=n):
                if m is None:
                    K.op("act", lambda e: e.activation(out=qx[:, n * 4:n * 4 + 4, :T],
                                                       in_=self.pb[b][:, :4 * T].rearrange("p (m t) -> p m t", t=T), func=AF.Copy, scale=1.0 / 16),
                         reads=[self.pbr[b]], writes=qxr[n * 4:n * 4 + 4])
                    return
                K.op("act", lambda e: e.activation(out=qx[:, n * 4 + m, :T], in_=self.pb[b][:, :T], func=AF.Copy, scale=1.0 / 16),
                     reads=[self.pbr[b]], writes=[qxr[n * 4 + m]])
            self.proj_fm(wt, wres, range(4), self.actT, self.actr, T, ev)
        qxb = self.R1[:, 4096:12288].rearrange("p (j b t) -> p j b t", j=8, b=NB)
        qxbr = self.R1r[8:24]
        K.op("dve", lambda e: e.tensor_tensor(out=qxb, in0=qx[:, :, :T].unsqueeze(2).to_broadcast([128, 8, NB, T]),
                                             in1=self.bmq[:, :, :].unsqueeze(1).to_broadcast([128, 8, NB, T]), op=ALU.mult),
             reads=qxr + [self.bmqr], writes=qxbr)
        q3, q3r = self.r1_bf(3)
        PT = q3[:, :, 0:64]
        ox = q3[:, :, 64:128]
        kvb = [self.ogs[:, 1:3, :], self.ur[:, 3:5, :],
               self.xh[:, 1, :].bitcast(BF16).rearrange("p (a d) -> p a d", d=D),
               self.xh[:, 2, :].bitcast(BF16).rearrange("p (a d) -> p a d", d=D)]
        kvbr = [[self.ogsr[1], self.ogsr[2]], [self.urr[3], self.urr[4]], [self.xhr[1]], [self.xhr[2]]]
        NKV = 4
        kbT = [self.Sb[:, 0:2, :, :].rearrange("p a b c -> p (a b c)").rearrange("p (j m) -> p j m", m=256),
               self.Sb[:, 2:4, :, :].rearrange("p a b c -> p (a b c)").rearrange("p (j m) -> p j m", m=256)]
        kbTr = [self.Sbr[0:8], self.Sbr[8:16]]
        sbk = self.hold(4)

        def k_load_trans(b):
            bb = b % 2
            if USE_KVSCR:
                K.dma("pool", kvb[b % NKV], self.kvscr[0, b].rearrange("(mt p) d -> p mt d", p=128),
                      reads=[self.kvscr_res], writes=kvbr[b % NKV], sem_res=kvbr[b % NKV][0])
            else:
                K.dma("pool", kvb[b % NKV], I["ck"][b].rearrange("(mt p) d -> p mt d", p=128), writes=kvbr[b % NKV])
            for half in range(2):
                bt = self.bank()
                for jj in range(4):
                    j = half * 4 + jj
                    for mt in range(2):
                        K.op("pe", lambda e, bt=bt, jj=jj, mt=mt, j=j, b=b: e.transpose(
                            self.pbf[bt][:, jj * 256 + mt * 128:jj * 256 + (mt + 1) * 128],
                            kvb[b % NKV][:, mt, j * 128:(j + 1) * 128], self.ident[:, :]),
                             reads=kvbr[b % NKV] + [self.identr], writes=[self.pbr[bt]], signal=(jj == 3 and mt == 1))
                if half == 0:
                    K.op("act", lambda e, bt=bt, bb=bb: e.activation(out=kbT[bb][:, 0:4, :],
                                                                     in_=self.pbf[bt][:, :].rearrange("p (j m) -> p j m", m=256), func=AF.Copy),
                         reads=[self.pbr[bt]], writes=kbTr[bb])
                else:
                    K.op("dve", lambda e, bt=bt, bb=bb: e.tensor_copy(kbT[bb][:, 4:8, :],
                                                                      self.pbf[bt][:, :].rearrange("p (j m) -> p j m", m=256)),
                         reads=[self.pbr[bt]], writes=kbTr[bb])

        def k_scores(b):
            bb = b % 2
            for h in range(4):
                pairs = [(qxb[:, 2 * h + dc, b, :], kbT[bb][:, 2 * h + dc, :]) for dc in range(2)]
                self.mm(self.pb[sbk[h]][:sp, :256], pairs, qxbr + kbTr[bb], self.pbr[sbk[h]], start=(b == 0), stop=(b == NB - 1))

        k_load_trans(0)
        for b in range(NB):
            if b + 1 < NB:
                k_load_trans(b + 1)
            k_scores(b)
        Pn = self.att[:, 0:2, :, :].rearrange("p a h t -> p (a h t)").rearrange("p (h m) -> p h m", m=256)
        Pnr = [self.attr[0], self.attr[1]]
        self.softmax_rows([(sbk[h], 0) for h in range(4)], sp, Pn, Pnr)
        self.release(sbk)
        bt = self.bank()
        for h in range(4):
            for mt in range(2):
                j = h * 2 + mt
                K.op("pe", lambda e, bt=bt, h=h, mt=mt, j=j: e.transpose(self.pbf[bt][:, j * 64:(j + 1) * 64],
                                                                        Pn[:sp, h, mt * 128:(mt + 1) * 128], self.ident[:sp, :sp]),
                     reads=list(Pnr) + [self.identr], writes=[self.pbr[bt]], signal=(j == 7))
        K.op("act", lambda e, bt=bt: e.activation(out=PT, in_=self.pbf[bt][:, :512].rearrange("p (j t) -> p j t", t=64), func=AF.Copy),
             reads=[self.pbr[bt]], writes=q3r)
        bo = self.hold(1)[0]
        for b in range(NB):
            bb = b % 2
            if USE_KVSCR:
                K.dma("pool", kvb[b % NKV], self.kvscr[1, b].rearrange("(mt p) d -> p mt d", p=128),
                      reads=[self.kvscr_res], writes=kvbr[b % NKV], sem_res=kvbr[b % NKV][0])
            else:
                K.dma("pool", kvb[b % NKV], I["cv"][b].rearrange("(mt p) d -> p mt d", p=128), writes=kvbr[b % NKV])
            for j in range(8):
                h = j // 2
                pairs = [(kvb[b % NKV][:, mt, j * 128:(j + 1) * 128], PT[:, h * 2 + mt, b * ST:(b + 1) * ST]) for mt in range(2)]
                self.mm(self.pb[bo][:, j * 64 + b * ST:j * 64 + (b + 1) * ST], pairs, kvbr[b % NKV] + q3r, self.pbr[bo],
                        signal=(j == 7))
        K.op("act", lambda e: e.activation(out=ox, in_=self.pb[bo][:, :].rearrange("p (j t) -> p j t", t=64), func=AF.Copy),
             reads=[self.pbr[bo]], writes=q3r)
        self.release([bo])
        self.resid_norm("xo", ox, q3r, nsub, sp, "g_mlp")
        self.mlp_final(nsub, sp, T, None, lambda s: O["ys"][:, :])


def _extra_alloc(self):
    K = self.K
    self.eB, self.eBr = self.T_("eB", [128, 4, 16], F32)
    self.junk2 = self.vt[:, 3, :]
    self.junk2r = self.vtr[3]
    self.ufp = self.xn[:, 0:2, :].rearrange("p a d -> p (a d)").bitcast(F32)
    self.ufpr = None


_old_alloc = Prog.alloc


def _alloc(self):
    _old_alloc(self)
    _extra_alloc(self)


Prog.alloc = _alloc


def _cols_table(p):
    def colmaj(v, n):
        return np.ascontiguousarray(np.asarray(v, np.float32).reshape(n, 128).T)
    parts = [colmaj(p["norm_mix_g"][0], 8), colmaj(p["norm_x_g"][0], 8), colmaj(p["norm_mlp_g"][0], 8),
             colmaj(p["norm_mem_g"][0], 8), colmaj(p["gla_norm_g"][0], 2), colmaj(p["pool_scale"][0], 8),
             colmaj(p["b_gk"][0], 4)]
    return np.ascontiguousarray(np.concatenate(parts, axis=1))


def make_in_maps(inputs, cores):
    p = inputs
    consts = make_consts()
    shared = {
        "w_in": np.ascontiguousarray(p["w_in"][0]), "w_gk": np.ascontiguousarray(p["w_gk_up"][0]),
        "w_pm": np.ascontiguousarray(p["w_pool_mix"][0]),
        "w_a": np.ascontiguousarray(p["w_branch_a"][0]), "w_b": np.ascontiguousarray(p["w_branch_b"][0]),
        "w_o": np.ascontiguousarray(p["w_out"][0]), "w_xq": np.ascontiguousarray(p["w_xq"][0]),
        "w_xk": np.ascontiguousarray(p["w_xk"][0]), "w_xv": np.ascontiguousarray(p["w_xv"][0]),
        "w_xo": np.ascontiguousarray(p["w_xo"][0]), "w_up": np.ascontiguousarray(p["w_up"][0]),
        "w_dn": np.ascontiguousarray(p["w_down"][0]),
        "cols": _cols_table(p), "g_fin": np.ascontiguousarray(p["norm_final_g"]),
    }
    for k, v in consts.items():
        shared["c_" + k] = v
    maps = []
    for c in cores:
        m = dict(shared)
        m["xp"] = np.ascontiguousarray(p["x_prompt"][c])
        m["xs"] = np.ascontiguousarray(p["x_sample"][c * SB:(c + 1) * SB].reshape(SB * ST, D))
        m["mem"] = np.ascontiguousarray(p["mem_prompt"][c])
        m["sgla"] = np.ascontiguousarray(p["state_gla"][0, c * SB:(c + 1) * SB])
        m["spool"] = np.ascontiguousarray(p["state_pool"][0, c * SB:(c + 1) * SB])
        m["ck"] = np.ascontiguousarray(p["cache_mem_k"][0, c * SB:(c + 1) * SB].reshape(SB, MEM, D))
        m["cv"] = np.ascontiguousarray(p["cache_mem_v"][0, c * SB:(c + 1) * SB].reshape(SB, MEM, D))
        maps.append(m)
    return maps


_PROG_CACHE = {}


def get_prog(do_sample=True, dbg=()):
    key = (do_sample, tuple(dbg))
    if key not in _PROG_CACHE:
        _PROG_CACHE[key] = Prog(do_sample=do_sample, dbg=dbg)
    return _PROG_CACHE[key]


def kernel(**inputs):
    inputs = {k: np.asarray(v) for k, v in inputs.items()}
    prog = get_prog(True)
    cores = list(range(NCORE))
    maps = make_in_maps(inputs, cores)
    res = run_bass_kernel_spmd(prog.nc, maps, core_ids=cores)
    R = res.results
    yp = np.stack([R[c]["yp"] for c in cores]).astype(np.float32)
    ys = np.concatenate([R[c]["ys"].reshape(SB, ST, D) for c in cores]).astype(np.float32)
    mk = np.stack([R[c]["mk"].reshape(MEM, 4, 256) for c in cores])[None].astype(np.float32)
    mv = np.stack([R[c]["mv"].reshape(MEM, 4, 256) for c in cores])[None].astype(np.float32)
    sgp = np.stack([R[c]["sgp"] for c in cores])[None].astype(np.float32)
    sgs = np.concatenate([R[c]["sgs"] for c in cores])[None].astype(np.float32)
    spp = np.stack([R[c]["spp"] for c in cores])[None].astype(np.float32)
    sps = np.concatenate([R[c]["sps"] for c in cores])[None].astype(np.float32)
    return (yp, ys, mk, mv, sgp, sgs, spp, sps)
```

```python
import contextlib
import math
import numpy as np
import concourse.bass as bass
import concourse.mybir as mybir
from concourse.bass_utils import run_bass_kernel_spmd

F32 = mybir.dt.float32
BF16 = mybir.dt.bfloat16
AF = mybir.ActivationFunctionType
ALU = mybir.AluOpType
AX = mybir.AxisListType

COMPUTE = ("pe", "act", "dve", "pool")

D = 1024
SEQ = 2048
NCORE = 8
SB = 16
ST = 4
MEM = 256
DFF = 4096
INW = 6160
EPS = 1e-6
NSLOT = 5
USE_SCRATCH = True
USE_KVSCR = True
SCR_SPREAD = 3
HIDE_TAIL = True
POOL_SOFTMAX = "pool"


class Res:
    __slots__ = ("name", "last_w", "reads", "dsem", "dcount", "excl")

    def __init__(self, name, excl=False):
        self.name = name
        self.excl = excl
        self.last_w = None
        self.reads = {}
        self.dsem = None
        self.dcount = 0


class Sched:
    def __init__(self, nc, stack):
        self.nc = nc
        self.stack = stack
        self.streams = {e: [] for e in COMPUTE + ("sp",)}
        self.count = {e: 0 for e in COMPUTE}
        self.sem = {e: stack.enter_context(nc.semaphore(f"S_{e}")) for e in COMPUTE}
        self.waited = {e: {} for e in COMPUTE + ("sp",)}
        self.dma_res = []
        self.n_waits = 0
        self.n_ops = 0
        self.sb_bytes = 0

    def sb(self, name, shape, dt):
        n = 1
        for s in shape[1:]:
            n *= s
        self.sb_bytes += n * (2 if dt == BF16 else 4)
        return self.stack.enter_context(self.nc.sbuf_tensor("sb_" + name, list(shape), dt))

    def ps(self, name, shape, dt):
        return self.stack.enter_context(self.nc.psum_tensor("ps_" + name, list(shape), dt))

    def new_sem(self, name):
        return self.stack.enter_context(self.nc.semaphore(name))

    def _deps(self, eng, reads, writes):
        deps = {}

        def add(ev, raw):
            if ev is None:
                return
            sem, val, src = ev
            if (not raw) and src == eng and eng == "pe":
                return
            k = id(sem)
            if k not in deps or deps[k][1] < val:
                deps[k] = ev

        for r in reads:
            add(r.last_w, True)
            if r.excl:
                for ev in r.reads.values():
                    add(ev, False)
        for w in writes:
            add(w.last_w, False)
            for ev in w.reads.values():
                add(ev, False)
        return deps

    def _emit_waits(self, eng, deps):
        wd = self.waited[eng]
        out = []
        for k, (sem, val, src) in deps.items():
            if wd.get(k, 0) >= val:
                continue
            wd[k] = val
            out.append((sem, val))
        return out

    def _record(self, ev, reads, writes):
        k = id(ev[0])
        for r in reads:
            old = r.reads.get(k)
            if old is None or old[1] < ev[1]:
                r.reads[k] = ev
        for w in writes:
            w.last_w = ev
            w.reads = {}

    def op(self, eng, fn, reads=(), writes=(), signal=True):
        waits = self._emit_waits(eng, self._deps(eng, reads, writes))
        self.n_waits += len(waits)
        self.n_ops += 1
        if signal:
            self.count[eng] += 1
            val = self.count[eng]
        else:
            val = self.count[eng] + 1
        sem = self.sem[eng]
        ev = (sem, val, eng)

        def run(e, waits=waits, fn=fn, signal=signal, sem=sem):
            for s, v in waits:
                e.wait_ge(s, v)
            ins = fn(e)
            if signal:
                ins.then_inc(sem, 1)

        self.streams[eng].append(run)
        self._record(ev, reads, writes)
        return ev

    def dma(self, queue, out, in_, reads=(), writes=(), sem_res=None):
        if sem_res is None:
            sem_res = (list(writes) + list(reads))[0]
        qk = "sw" if queue == "pool" else "hw"
        if sem_res.dsem is None:
            sem_res.dsem = {}
        if qk not in sem_res.dsem:
            ent = [self.new_sem(f"D{qk}_{sem_res.name}"), 0]
            sem_res.dsem[qk] = ent
            self.dma_res.append(ent)
        ent = sem_res.dsem[qk]
        waits = self._emit_waits(queue, self._deps("dma", reads, writes))
        self.n_waits += len(waits)
        ent[1] += 16
        sem = ent[0]
        ev = (sem, ent[1], "dma")

        def run(e, waits=waits, out=out, in_=in_, sem=sem):
            for s, v in waits:
                e.wait_ge(s, v)
            e.dma_start(out=out, in_=in_).then_inc(sem, 16)

        self.streams[queue].append(run)
        self._record(ev, reads, writes)
        return ev

    def finish(self):
        waits = [(ent[0], ent[1]) for ent in self.dma_res]
        for e in COMPUTE:
            if self.count[e]:
                waits.append((self.sem[e], self.count[e]))

        def run(e, waits=waits):
            for s, v in waits:
                e.wait_ge(s, v)

        self.streams["sp"].append(run)

    def emit(self):
        st = self.streams
        with self.nc.Block() as block:
            @block.sync
            def _(e):
                for f in st["sp"]:
                    f(e)

            @block.tensor
            def _(e):
                for f in st["pe"]:
                    f(e)

            @block.scalar
            def _(e):
                for f in st["act"]:
                    f(e)

            @block.vector
            def _(e):
                for f in st["dve"]:
                    f(e)

            @block.gpsimd
            def _(e):
                for f in st["pool"]:
                    f(e)


POOL_WINDOWS = (2, 4, 8, 16)


def _bf16_round(a):
    a = np.asarray(a, np.float32)
    u = a.view(np.uint32).astype(np.uint64)
    r = ((u + 0x7FFF + ((u >> 16) & 1)) >> 16) << 16
    return r.astype(np.uint32).view(np.float32)


def make_consts():
    c = {}
    c["ident"] = np.eye(128, dtype=np.float32)
    s = np.arange(128)[:, None]
    t = np.arange(128)[None, :]
    c["maskT"] = (s <= t).astype(np.float32)
    s6 = np.arange(64)[:, None]
    t6 = np.arange(64)[None, :]
    c["smask"] = ((s6 // ST == t6 // ST) & (s6 <= t6)).astype(np.float32)
    r = np.ones((128, 512), np.float32)
    r[:, ::128] = 0.0
    c["rst512"] = r
    r = np.ones((128, 64), np.float32)
    r[:, ::ST] = 0.0
    c["rst64"] = r
    pm = np.zeros((128, 4, 4, 128), np.float32)
    for g, w in enumerate(POOL_WINDOWS):
        cur = ((s <= t) & (s >= t - w + 1)).astype(np.float32) / w - (s == t).astype(np.float32)
        prev = ((s - 128) >= (t - w + 1)).astype(np.float32) / w
        cnt = np.minimum(t + 1, w).astype(np.float32)
        m0 = ((s <= t) & (s >= t - w + 1)).astype(np.float32) / cnt - (s == t).astype(np.float32)
        hi = _bf16_round(m0)
        lo = _bf16_round(m0 - hi)
        pm[:, 0, g], pm[:, 1, g], pm[:, 2, g], pm[:, 3, g] = cur, prev, hi, lo
    c["pmat"] = pm
    mb = np.zeros((120, 2, 4, 64), np.float32)
    mu = np.zeros((64, 4, 64), np.float32)
    for g, w in enumerate(POOL_WINDOWS):
        for b in range(SB):
            for tt in range(ST):
                col = b * ST + tt
                lo_idx = 15 + tt - w + 1
                for j in range(15):
                    if j >= lo_idx:
                        mb[(b % 8) * 15 + j, b // 8, g, col] = 1.0 / w
                for t2 in range(ST):
                    v = 0.0
                    if t2 <= tt and (15 + t2) >= lo_idx:
                        v += 1.0 / w
                    if t2 == tt:
                        v -= 1.0
                    mu[b * ST + t2, g, col] = v
    c["smb"] = mb
    c["smu"] = mu
    bq = np.zeros((128, SB, 64), np.float32)
    for b in range(SB):
        bq[:, b, b * ST:(b + 1) * ST] = 1.0
    c["bmq"] = bq
    bk = np.zeros((64, SB), np.float32)
    for b in range(SB):
        bk[b * ST:(b + 1) * ST, b] = 1.0
    c["bmk"] = bk
    return c


CONST_SHAPES = {k: v.shape for k, v in make_consts().items()}

COLS = {}
_o = 0
for _n, _w in (("g_mix", 8), ("g_x", 8), ("g_mlp", 8), ("g_mem", 8), ("g_gla", 2), ("pscale", 8), ("bgk", 4)):
    COLS[_n] = (_o, _w)
    _o += _w
NCOL = _o


class StopBuild(Exception):
    pass


class Prog:
    def __init__(self, do_sample=True, dbg=(), stop=None):
        self.stop = stop
        self.dbg_names = dbg
        self.do_sample = do_sample
        self.nc = nc = bass.Bass("TRN2", target_bir_lowering=False)
        self.I = {}
        self.O = {}

        def inp(name, shape):
            self.I[name] = nc.dram_tensor(name, list(shape), F32, kind="ExternalInput").ap()

        def outp(name, shape):
            self.O[name] = nc.dram_tensor(name, list(shape), F32, kind="ExternalOutput").ap()

        inp("xp", [SEQ, D]); inp("xs", [SB * ST, D]); inp("mem", [MEM, D])
        inp("sgla", [SB, 4, 128, 256]); inp("spool", [SB, 15, D])
        inp("ck", [SB, MEM, D]); inp("cv", [SB, MEM, D])
        inp("w_in", [D, INW]); inp("w_gk", [16, 512]); inp("w_pm", [4, 256, 256])
        for n in ("w_a", "w_b", "w_o", "w_xq", "w_xk", "w_xv", "w_xo"):
            inp(n, [D, D])
        inp("w_up", [D, DFF]); inp("w_dn", [DFF, D])
        inp("cols", [128, NCOL]); inp("g_fin", [D])
        for k, shp in CONST_SHAPES.items():
            inp("c_" + k, shp)
        outp("yp", [SEQ, D]); outp("ys", [SB * ST, D]); outp("mk", [MEM, D]); outp("mv", [MEM, D])
        outp("sgp", [4, 128, 256]); outp("sgs", [SB, 4, 128, 256]); outp("spp", [15, D]); outp("sps", [SB, 15, D])
        self.dbg_out = {}

        with contextlib.ExitStack() as stack:
            self.K = K = Sched(nc, stack)
            self.alloc()
            for s_ in range(2):
                xa, xr = self.xnext(s_)
                K.dma("sp", xa, self.I["mem"][s_ * 128:(s_ + 1) * 128, :], writes=xr)
            self.plan_units()
            self.load_consts()
            try:
                self.stage("consts")
                for s_ in range(4):
                    K.dma("sp", self.xh[:, s_, :], self.I["xp"][s_ * 128:(s_ + 1) * 128, :], writes=[self.xhr[s_]])
                self.mem_kv()
                self.stage("memkv")
                for ti in range(SEQ // 512):
                    self.kv_active = ti >= 2
                    self.prompt_tile(ti)
                    self.stage(f"tile{ti}")
                self.kv_active = False
                while self.kv_todo:
                    w, b = self.kv_todo.pop(0)
                    K.dma("pool", self.kvscr[w, b], self.I["ck" if w == 0 else "cv"][b], writes=[self.kvscr_res])
                if self.kvscr_res.dsem is not None:
                    ent = self.kvscr_res.dsem["sw"]
                    self.kvscr_res.last_w = (ent[0], ent[1], "dma")
                self.prompt_finish()
                if do_sample:
                    self.sample_pass()
                assert self.wi == len(self.units), (self.wi, len(self.units))
            except StopBuild:
                print("[kernel] build stopped at", self.stop)
            K.finish()
            K.emit()
            print(f"[kernel] ops={K.n_ops} waits={K.n_waits} sbuf_bytes/partition={K.sb_bytes} "
                  f"dma_sems={len(K.dma_res)} counts={K.count}")

    def stage(self, name):
        if self.stop == name:
            raise StopBuild()

    def T_(self, name, shape, dt, nres=1):
        t = self.K.sb(name, shape, dt)
        if nres == 1:
            return t, Res(name)
        return t, [Res(f"{name}{i}") for i in range(nres)]

    def alloc(self):
        K = self.K
        self.pb = [K.ps(f"pb{i}", [128, 512], F32) for i in range(8)]
        self.pbf = [p.bitcast(BF16) for p in self.pb]
        self.pbr = [Res(f"pb{i}", excl=True) for i in range(8)]
        self.bi = 0
        self.held = set()
        self.pending_trans = None
        self.wt = [K.sb(f"wt{i}", [128, 8, 512], BF16) for i in range(NSLOT)]
        self.wr = [Res(f"wt{i}") for i in range(NSLOT)]
        self.xh, self.xhr = self.T_("xh", [128, 4, D], F32, 4)
        self.xn, self.xnr = self.T_("xn", [128, 4, D], BF16, 4)
        self.actT, self.actr = self.T_("actT", [128, 8, 512], BF16, 8)
        self.S, self.Sr = self.T_("S", [128, 4, 256], F32, 4)
        self.Sb, self.Sbr = self.T_("Sb", [128, 4, 4, 256], BF16, 16)
        self.ur, self.urr = self.T_("uring", [128, 5, D], BF16, 5)
        self.KTp, self.KTpr = self.T_("KTp", [128, 8, 256], BF16)
        self.Vp, self.Vpr = self.T_("Vp", [128, 2, D], BF16)
        self.junk, self.junkr = self.T_("junk", [128, 256], BF16)
        self.small, self.smallr = self.T_("small", [128, 96], F32)
        self.sm2r = [Res("sm_a"), Res("sm_b"), Res("sm_c")]
        self.nsr = [Res(f"ns{i}") for i in range(4)]
        self.osr = [Res(f"os{i}") for i in range(4)]
        self.ident, self.identr = self.T_("ident", [128, 128], BF16)
        self.maskT, self.maskTr = self.T_("maskT", [128, 128], F32)
        self.smask, self.smaskr = self.T_("smask", [64, 64], F32)
        self.rst512, self.rst512r = self.T_("rst512", [128, 512], BF16)
        self.rst64, self.rst64r = self.T_("rst64", [128, 64], F32)
        self.pmat, self.pmatr = self.T_("pmat", [128, 4, 4, 128], BF16)
        self.smb, self.smbr = self.T_("smb", [120, 2, 4, 64], BF16)
        self.smu, self.smur = self.T_("smu", [64, 4, 64], BF16)
        self.bmq, self.bmqr = self.T_("bmq", [128, SB, 64], BF16)
        self.bmk, self.bmkr = self.T_("bmk", [64, SB], BF16)
        self.cols, self.colsr = self.T_("cols", [128, NCOL], F32)
        self.ncols, self.ncolsr = self.T_("ncols", [128, 8], F32)
        self.gfin, self.gfinr = self.T_("gfin", [128, D], F32)
        self.wgz, self.wgzr = self.T_("wgz", [128, 8, 16], BF16)
        self.wgk, self.wgkr = self.T_("wgk", [16, 512], BF16)
        self.cres = Res("consts")
        self.R1 = K.sb("R1", [128, 16384], BF16)
        self.R1r = [Res(f"R1_{i}") for i in range(32)]
        self.qi, self.qir = self.T_("qi", [128, 4, 512], BF16, 4)
        self.kd, self.kdr = self.T_("kd", [128, 4, 512], BF16, 4)
        self.kdt, self.kdtr = self.T_("kdt", [128, 4, 512], BF16, 4)
        self.vt, self.vtr = self.T_("vt", [128, 4, D], BF16, 4)
        self.ogs, self.ogsr = self.T_("ogs", [128, 4, D], BF16, 4)
        self.ga, self.gar = self.T_("ga", [128, 8, 512], BF16, 8)
        self.gb, self.gbr = self.T_("gb", [128, 8, 512], BF16, 8)
        self.att, self.attr = self.T_("att", [128, 4, 4, 128], BF16, 4)
        self.tmp, self.tmpr = self.T_("tmp", [128, 4, 512], F32, 4)
        self.tp = 0

    def r1_f32(self, i):
        v = self.R1[:, i * 4096:(i + 1) * 4096].bitcast(F32)
        return v.rearrange("p (h t) -> p h t", t=512), self.R1r[i * 8:(i + 1) * 8]

    def r1_bf(self, i):
        v = self.R1[:, i * 4096:(i + 1) * 4096]
        return v.rearrange("p (c t) -> p c t", t=512), self.R1r[i * 8:(i + 1) * 8]

    def bank(self):
        while True:
            b = self.bi
            self.bi = (self.bi + 1) % 8
            if b not in self.held:
                return b

    def hold(self, n):
        bs = []
        for _ in range(n):
            b = self.bank()
            self.held.add(b)
            bs.append(b)
        return bs

    def release(self, bs):
        for b in bs:
            self.held.discard(b)

    def tmpbuf(self):
        i = self.tp
        self.tp = (self.tp + 1) % 4
        return self.tmp[:, i, :], self.tmpr[i]

    def col(self, name, j=0, n=1, P=128):
        o, w = COLS[name]
        return self.cols[:P, o + j:o + j + n]

    def load_consts(self):
        K = self.K
        I = self.I
        cr = self.cres
        K.dma("sp", self.cols[:], I["cols"][:, :], writes=[self.colsr], sem_res=cr)
        K.dma("sp", self.maskT[:], I["c_maskT"][:, :], writes=[self.maskTr], sem_res=cr)
        K.dma("sp", self.smask[:], I["c_smask"][:, :], writes=[self.smaskr], sem_res=cr)
        K.dma("sp", self.rst64[:], I["c_rst64"][:, :], writes=[self.rst64r], sem_res=cr)
        K.dma("sp", self.gfin[:], I["g_fin"].partition_broadcast(128), writes=[self.gfinr], sem_res=cr)
        K.dma("pool", self.ident[:], I["c_ident"][:, :], writes=[self.identr], sem_res=self.identr)
        self.prefetch(0)
        K.dma("pool", self.rst512[:], I["c_rst512"][:, :], writes=[self.rst512r], sem_res=cr)
        K.dma("pool", self.pmat[:], I["c_pmat"][:, :, :, :], writes=[self.pmatr], sem_res=cr)
        K.dma("pool", self.smb[:], I["c_smb"][:, :, :, :], writes=[self.smbr], sem_res=cr)
        K.dma("pool", self.smu[:], I["c_smu"][:, :, :], writes=[self.smur], sem_res=cr)
        K.dma("pool", self.bmq[:], I["c_bmq"][:, :, :], writes=[self.bmqr], sem_res=cr)
        K.dma("pool", self.bmk[:], I["c_bmk"][:, :], writes=[self.bmkr], sem_res=cr)
        K.dma("pool", self.wgk[:], I["w_gk"][:, :], writes=[self.wgkr], sem_res=cr)
        K.dma("pool", self.wgz[:], I["w_in"][:, 2048:2064].rearrange("(kc p) n -> p kc n", p=128),
              writes=[self.wgzr], sem_res=cr)
        for r in (self.colsr, self.maskTr, self.smaskr, self.rst64r, self.gfinr):
            r.last_w = (cr.dsem["hw"][0], cr.dsem["hw"][1], "dma")
        for r in (self.rst512r, self.pmatr, self.smbr, self.smur, self.bmqr, self.bmkr, self.wgkr, self.wgzr):
            r.last_w = (cr.dsem["sw"][0], cr.dsem["sw"][1], "dma")
        o, w = COLS["bgk"]
        K.op("dve", lambda e: e.tensor_scalar(out=self.ncols[:, 0:4], in0=self.cols[:, o:o + 4], scalar1=-1.0,
                                             scalar2=None, op0=ALU.mult), reads=[self.colsr], writes=[self.ncolsr])
        K.op("dve", lambda e: e.memset(self.ncols[:, 4:5], math.log(128.0 ** -0.5)), writes=[self.ncolsr])
        K.op("dve", lambda e: e.memset(self.ncols[:, 5:6], 1.0), writes=[self.ncolsr])
        K.op("dve", lambda e: e.memset(self.ncols[:, 6:7], EPS), writes=[self.ncolsr])
        for h in range(4):
            K.op("dve", lambda e, h=h: e.memset(self.S[:, h, :], 0.0), writes=[self.Sr[h]])
        K.op("dve", lambda e: e.memset(self.ur[:, 4, :], 0.0), writes=[self.urr[4]])

    def plan_units(self):
        I = self.I
        U = []

        def full(w, r0, c0, name, sidx=None, p=0):
            U.append((name, w[r0:r0 + 1024, c0:c0 + 512].rearrange("(kc p) n -> p kc n", p=128), 512, sidx, mode(sidx, p)))

        def mode(sidx, p):
            if sidx is None or not USE_SCRATCH:
                return 0
            tp = sidx % SCR_SPREAD
            return 0 if p < tp else (1 if p == tp else 2)

        def one_pass(first):
            k = 0
            for nm, c0 in (("v0", 1024), ("v1", 1536), ("og0", 2064), ("og1", 2576), ("u0", 3088), ("u1", 3600),
                           ("ga0", 4112), ("ga1", 4624), ("gb0", 5136), ("gb1", 5648), ("k", 512), ("q", 0)):
                full(I["w_in"], 0, c0, nm, k, first); k += 1
            U.append(("pm", I["w_pm"].rearrange("g (kc p) d -> p (g kc) d", p=128), 256, k, mode(k, first))); k += 1
            for nm, w, c0 in (("b0", "w_b", 0), ("a0", "w_a", 0), ("b1", "w_b", 512), ("a1", "w_a", 512),
                              ("o0", "w_o", 0), ("o1", "w_o", 512),
                              ("xq0", "w_xq", 0), ("xq1", "w_xq", 512), ("xo0", "w_xo", 0), ("xo1", "w_xo", 512)):
                full(I[w], 0, c0, nm, k, first); k += 1
            for i in range(8):
                full(I["w_up"], 0, i * 512, f"up{i}", k, first); k += 1
            for n in range(2):
                for j in range(4):
                    full(I["w_dn"], j * 1024, n * 512, f"dn{j}_{n}", k, first); k += 1
            return k

        for nm, w, c0 in (("xk0", "w_xk", 0), ("xk1", "w_xk", 512), ("xv0", "w_xv", 0), ("xv1", "w_xv", 512)):
            full(I[w], 0, c0, nm)
        npass = SEQ // 512 + (1 if self.do_sample else 0)
        for p in range(npass):
            nper = one_pass(p)
        self.units = U
        self.pend_st = []
        self.wi = 0
        self.wl = 0
        self.scr = self.nc.dram_tensor("wscratch", [nper, 128, 8 * 512], BF16, kind="Internal").ap()
        self.scrr = [Res(f"scr{i}") for i in range(nper)]
        self.kvscr = self.nc.dram_tensor("kvscratch", [2, SB, MEM, D], BF16, kind="Internal").ap()
        self.kvscr_res = Res("kvscr")
        self.kv_todo = [(w, b) for w in range(2) for b in range(SB)] if (self.do_sample and USE_KVSCR) else []
        self.kv_tick = 0
        self.kv_active = False

    def W(self, name):
        i = self.wi
        assert self.units[i][0] == name, (self.units[i][0], name)
        self.prefetch(i)
        self.kv_tick += 1
        if self.kv_active and self.kv_todo and self.kv_tick % 2 == 0:
            w, b = self.kv_todo.pop(0)
            src = self.I["ck" if w == 0 else "cv"][b]
            self.K.dma("pool", self.kvscr[w, b], src, writes=[self.kvscr_res])
        self.wi += 1
        s = i % NSLOT
        return self.wt[s], self.wr[s]

    def flush_one_store(self):
        j, s, sidx, nc_ = self.pend_st.pop(0)
        self.K.dma("pool", self.scr[sidx].rearrange("p (kc n) -> p kc n", n=512)[:, :, 0:nc_], self.wt[s][:, :, 0:nc_],
                   reads=[self.wr[s]], writes=[self.scrr[sidx]], sem_res=self.scrr[sidx])

    def prefetch(self, i):
        K = self.K
        lim = min(len(self.units), i + NSLOT - 1)
        while self.wl < lim:
            j = self.wl
            s = j % NSLOT
            nm, src, nc_, sidx, md = self.units[j]
            if md in (0, 1):
                K.dma("pool", self.wt[s][:, :, 0:nc_], src, writes=[self.wr[s]])
                if md == 1:
                    self.pend_st.append((j, s, sidx, nc_))
            else:
                K.dma("pool", self.wt[s][:, :, 0:nc_], self.scr[sidx].rearrange("p (kc n) -> p kc n", n=512)[:, :, 0:nc_],
                      reads=[self.scrr[sidx]], writes=[self.wr[s]])
            self.wl += 1
            while self.pend_st and self.pend_st[0][0] <= j - 2:
                self.flush_one_store()

    def mm(self, out, pairs, reads, bres, start=True, stop=True, signal=True):
        def fn(e, out=out, pairs=pairs, start=start, stop=stop):
            n = len(pairs)
            ins = None
            for i, (l, r) in enumerate(pairs):
                ins = e.matmul(out, l, r, start=(start and i == 0), stop=(stop and i == n - 1))
            return ins
        return self.K.op("pe", fn, reads=reads, writes=[bres], signal=signal)

    def dbg(self, name, ap, res, shape):
        if name not in self.dbg_names or name in self.dbg_out:
            return
        o = self.nc.dram_tensor("dbg_" + name, list(shape), F32, kind="ExternalOutput").ap()
        self.dbg_out[name] = o
        st = self.K.sb("dbgs_" + name, list(shape), F32)
        sr = Res("dbgs_" + name)
        rl = res if isinstance(res, (list, tuple)) else [res]
        self.K.op("dve", lambda e: e.tensor_copy(st[:], ap), reads=rl, writes=[sr])
        self.K.dma("sp", o, st[:], reads=[sr])

    def rstd_from_ss(self, ss_ap, out_ap, inv_n, P, sr=None):
        K = self.K
        sr = self.smallr if sr is None else sr
        K.op("act", lambda e: e.activation(out=out_ap, in_=ss_ap, func=AF.Ln, scale=inv_n, bias=self.ncols[:P, 6:7]),
             reads=[sr, self.ncolsr], writes=[sr])
        K.op("act", lambda e: e.activation(out=out_ap, in_=out_ap, func=AF.Exp, scale=-0.5), reads=[sr], writes=[sr])

    def xnext(self, s):
        g, gr = (self.ga, self.gar) if s < 2 else (self.gb, self.gbr)
        c0 = (s % 2) * 4
        return g[:, c0:c0 + 4, :].rearrange("p c t -> p (c t)").bitcast(F32), gr[c0:c0 + 4]

    def norm_sub(self, s, sp, src=None, srcr=None):
        K = self.K
        sr = self.nsr[s]
        if src is None:
            src, srcr = self.xh[:sp, s, :], [self.xhr[s]]
        K.op("act", lambda e: e.activation(out=self.xn[:sp, s, :], in_=src, func=AF.Square,
                                           accum_out=self.small[:sp, s:s + 1]),
             reads=list(srcr) + [sr], writes=[self.xnr[s], sr])
        self.rstd_from_ss(self.small[:sp, s:s + 1], self.small[:sp, 4 + s:5 + s], 1.0 / D, sp, sr=sr)
        K.op("dve", lambda e: e.tensor_scalar(out=self.xn[:sp, s, :], in0=src,
                                             scalar1=self.small[:sp, 4 + s:5 + s], scalar2=None, op0=ALU.mult),
             reads=list(srcr) + [sr], writes=[self.xnr[s]])

    def trans_sub(self, src, srcr, dst, dstr, s, sp, gname, gmod=8):
        K = self.K
        b = self.bank()
        for c in range(8):
            K.op("pe", lambda e, b=b, c=c: e.transpose(self.pbf[b][:, c * sp:(c + 1) * sp], src[:sp, s, c * 128:(c + 1) * 128],
                                                      self.ident[:sp, :sp]),
                 reads=[srcr[s], self.identr], writes=[self.pbr[b]], signal=(c == 7))
        o, w = COLS[gname]
        if gmod == 8:
            outv = dst[:, :, s * sp:(s + 1) * sp]
            inv = self.pbf[b][:, :8 * sp].rearrange("p (c t) -> p c t", t=sp)
            gv = self.cols[:, o:o + 8].unsqueeze(2).to_broadcast([128, 8, sp])
        else:
            outv = dst[:, :, s * sp:(s + 1) * sp].rearrange("p (a b) t -> p a b t", b=gmod)
            inv = self.pbf[b][:, :8 * sp].rearrange("p (a b t) -> p a b t", b=gmod, t=sp)
            gv = self.cols[:, o:o + gmod].unsqueeze(1).unsqueeze(3).to_broadcast([128, 8 // gmod, gmod, sp])
        K.op("dve", lambda e: e.tensor_tensor(out=outv, in0=inv, in1=gv, op=ALU.mult),
             reads=[self.pbr[b], self.colsr], writes=list(dstr))

    def norm_to_T(self, gname, nsub, sp, T):
        for s in range(nsub):
            self.norm_sub(s, sp)
        for s in range(nsub):
            self.trans_sub(self.xn, self.xnr, self.actT, self.actr, s, sp, gname)

    def to_T(self, src, srcr, dst, dstr, nsub, sp, T, gname, gmod=8):
        K = self.K
        for c in range(8):
            b = self.bank()
            for s in range(nsub):
                K.op("pe", lambda e, b=b, s=s, c=c: e.transpose(self.pbf[b][:, s * sp:(s + 1) * sp],
                                                                src[:sp, s, c * 128:(c + 1) * 128],
                                                                self.ident[:sp, :sp]),
                     reads=[srcr[s], self.identr], writes=[self.pbr[b]], signal=(s == nsub - 1))
            K.op("act", lambda e, b=b, c=c: e.activation(out=dst[:, c, :T], in_=self.pbf[b][:, :T], func=AF.Copy,
                                                         scale=self.col(gname, c % gmod)),
                 reads=[self.pbr[b], self.colsr], writes=[dstr[c]])

    def proj_fm(self, wt, wres, ms, srcT, srcr, T, evac):
        ms = list(ms)
        if T * len(ms) <= 512 and len(ms) == 4:
            b = self.bank()
            for m in ms:
                pairs = [(wt[:, kc, m * 128:(m + 1) * 128], srcT[:, kc, :T]) for kc in range(8)]
                self.mm(self.pb[b][:, m * T:(m + 1) * T], pairs, [wres] + list(srcr), self.pbr[b], signal=(m == ms[-1]))
            evac(None, b)
            return
        if self.pending_trans is not None and T == 512:
            pend, self.pending_trans = self.pending_trans, None
            TA = T - 128
            head = ms[:3]
            hb = []
            for m in head:
                b = self.bank()
                hb.append(b)
                pairs = [(wt[:, kc, m * 128:(m + 1) * 128], srcT[:, kc, :TA]) for kc in range(8)]
                self.mm(self.pb[b][:, :TA], pairs, [wres] + list(srcr), self.pbr[b])
            pend()
            for m, b in zip(head, hb):
                pairs = [(wt[:, kc, m * 128:(m + 1) * 128], srcT[:, kc, TA:T]) for kc in range(8)]
                self.mm(self.pb[b][:, TA:T], pairs, [wres] + list(srcr), self.pbr[b])
                evac(m, b)
            ms = ms[3:]
        for m in ms:
            b = self.bank()
            pairs = [(wt[:, kc, m * 128:(m + 1) * 128], srcT[:, kc, :T]) for kc in range(8)]
            self.mm(self.pb[b][:, :T], pairs, [wres] + list(srcr), self.pbr[b])
            evac(m, b)

    def proj_tm(self, wt, wres, srcT, srcr, nsub, sp, evac):
        for s in range(nsub):
            b = self.bank()
            pairs = [(srcT[:, kc, s * sp:(s + 1) * sp], wt[:, kc, :]) for kc in range(8)]
            self.mm(self.pb[b][:sp, :], pairs, [wres] + list(srcr), self.pbr[b])
            evac(s, b)

    def sigmoid_to(self, b, T, out_ap, out_res, P=128, ncol=None, fold=None):
        K = self.K
        ncol = T if ncol is None else ncol
        t, tr = self.tmpbuf()
        out_res_l = out_res if isinstance(out_res, (list, tuple)) else [out_res]
        if fold is not None:
            tv = t[:P, :ncol].rearrange("p (m t) -> p m t", t=fold)
            K.op("act", lambda e: e.activation(out=t[:P, :ncol], in_=self.pb[b][:P, :ncol], func=AF.Exp, scale=-1.0),
                 reads=[self.pbr[b]], writes=[tr])
            K.op("act", lambda e: e.activation(out=t[:P, :ncol], in_=t[:P, :ncol], func=AF.Ln, bias=self.ncols[:P, 5:6]),
                 reads=[tr, self.ncolsr], writes=[tr])
            K.op("act", lambda e: e.activation(out=out_ap, in_=tv, func=AF.Exp, scale=-1.0),
                 reads=[tr], writes=list(out_res_l))
            return t, tr
        K.op("act", lambda e: e.activation(out=t[:P, :ncol], in_=self.pb[b][:P, :ncol], func=AF.Exp, scale=-1.0),
             reads=[self.pbr[b]], writes=[tr])
        K.op("act", lambda e: e.activation(out=t[:P, :ncol], in_=t[:P, :ncol], func=AF.Ln, bias=self.ncols[:P, 5:6]),
             reads=[tr, self.ncolsr], writes=[tr])
        K.op("act", lambda e: e.activation(out=out_ap, in_=t[:P, :ncol], func=AF.Exp, scale=-1.0),
             reads=[tr], writes=[out_res])
        return t, tr

    def mem_kv(self):
        K = self.K
        I, O = self.I, self.O
        for s in range(2):
            xa, xr = self.xnext(s)
            self.norm_sub(s, 128, src=xa, srcr=xr)
        for s in range(2):
            self.trans_sub(self.xn, self.xnr, self.actT, self.actr, s, 128, "g_mem")
        self.stage("memkv_norm")
        stg, stgr = self.r1_f32(0)
        stg2, stg2r = self.r1_f32(1)
        for which, (st, str_), outn in (("xk", (stg, stgr), "mk"), ("xv", (stg2, stg2r), "mv")):
            if which == "xv":
                self.stage("memkv_xk")
            for n in range(2):
                wt, wres = self.W(f"{which}{n}")
                for mt in range(2):
                    b = self.bank()
                    pairs = [(self.actT[:, kc, mt * 128:(mt + 1) * 128], wt[:, kc, :]) for kc in range(8)]
                    self.mm(self.pb[b][:, :], pairs, [wres] + self.actr, self.pbr[b])
                    g = mt * 2 + n
                    K.op("act", lambda e, b=b, g=g, st=st: e.activation(out=st[:, g, :], in_=self.pb[b][:, :], func=AF.Copy),
                         reads=[self.pbr[b]], writes=[str_[g * 2], str_[g * 2 + 1]])
                    if which == "xv":
                        K.op("dve", lambda e, b=b, mt=mt, n=n: e.tensor_copy(self.Vp[:, mt, n * 512:(n + 1) * 512], self.pb[b][:, :]),
                             reads=[self.pbr[b]], writes=[self.Vpr])
                    K.dma("sp", O[outn][mt * 128:(mt + 1) * 128, n * 512:(n + 1) * 512], st[:, g, :],
                          reads=[str_[g * 2], str_[g * 2 + 1]])
                if which == "xk":
                    for m in range(4):
                        b = self.bank()
                        pairs = [(wt[:, kc, m * 128:(m + 1) * 128], self.actT[:, kc, :256]) for kc in range(8)]
                        self.mm(self.pb[b][:, :256], pairs, [wres] + self.actr, self.pbr[b])
                        K.op("dve", lambda e, b=b, m=m, n=n: e.tensor_copy(self.KTp[:, n * 4 + m, :], self.pb[b][:, :256]),
                             reads=[self.pbr[b]], writes=[self.KTpr])

    def in_proj(self, nsub, sp, T, uslots, last_u_fp32=None):
        K = self.K
        gzT = self.att[:16, 0, :, :].rearrange("p a b -> p (a b)")
        gzr = self.attr[0]
        b = self.bank()
        pairs = [(self.wgz[:, kc, :], self.actT[:, kc, :T]) for kc in range(8)]
        self.mm(self.pb[b][:16, :T], pairs, [self.wgzr] + self.actr, self.pbr[b])
        K.op("act", lambda e, b=b: e.activation(out=gzT[:, :T], in_=self.pb[b][:16, :T], func=AF.Copy),
             reads=[self.pbr[b]], writes=[gzr])
        T1, T1r = self.r1_f32(0)
        Bc, Bcr = self.r1_f32(1)
        E1, E1r = self.r1_f32(2)
        E2, E2r = self.r1_f32(3)
        CL = 128 if T == 512 else ST
        nch = T // CL

        def decay_chain():
            for h in range(4):
                b = self.bank()
                self.mm(self.pb[b][:, :T], [(self.wgk[:, h * 128:(h + 1) * 128], gzT[:, :T])], [self.wgkr, gzr], self.pbr[b])
                K.op("act", lambda e, b=b, h=h: e.activation(out=T1[:, h, :T], in_=self.pb[b][:, :T], func=AF.Exp,
                                                             scale=-1.0, bias=self.ncols[:, h:h + 1]),
                     reads=[self.pbr[b], self.ncolsr], writes=T1r[2 * h:2 * h + 2])
                K.op("act", lambda e, h=h: e.activation(out=T1[:, h, :T], in_=T1[:, h, :T], func=AF.Ln,
                                                        bias=self.ncols[:, 5:6]),
                     reads=T1r[2 * h:2 * h + 2] + [self.ncolsr], writes=T1r[2 * h:2 * h + 2])
                rst = self.rst512 if T == 512 else self.rst64
                rstr = self.rst512r if T == 512 else self.rst64r
                K.op("dve", lambda e, h=h, rst=rst: e.tensor_tensor_scan(out=Bc[:, h, :T], data0=rst[:, :T], data1=T1[:, h, :T],
                                                                         initial=0.0, op0=ALU.mult, op1=ALU.add),
                     reads=T1r[2 * h:2 * h + 2] + [rstr], writes=Bcr[2 * h:2 * h + 2])
            CL = 128 if T == 512 else ST
            nch = T // CL
            Bv = Bc[:, :, :T].rearrange("p h (c t) -> p h c t", t=CL)
            Dv = T1[:, :, :T].rearrange("p h (c t) -> p h c t", t=CL)
            Bend = Bv[:, :, :, CL - 1:CL]
            K.op("dve", lambda e: e.tensor_tensor(out=Dv, in0=Bv, in1=Bend.to_broadcast([128, 4, nch, CL]), op=ALU.subtract),
                 reads=Bcr, writes=T1r)
            eB = self.eB
            K.op("act", lambda e: e.activation(out=eB[:, :, :nch], in_=Bv[:, :, :, CL - 1], func=AF.Exp, scale=-1.0 / 16),
                 reads=Bcr, writes=[self.eBr])
            K.op("act", lambda e: e.activation(out=E1[:, :, :T], in_=T1[:, :, :T], func=AF.Exp, scale=1.0 / 16),
                 reads=T1r, writes=E1r)
            K.op("act", lambda e: e.activation(out=E2[:, :, :T], in_=T1[:, :, :T], func=AF.Exp, scale=-1.0 / 16,
                                               bias=self.ncols[:, 4:5]),
                 reads=T1r + [self.ncolsr], writes=E2r)
        for n in range(2):
            wt, wres = self.W(f"v{n}")

            def ev(s, b, n=n):
                K.op("act", lambda e: e.activation(out=self.vt[:sp, s, n * 512:(n + 1) * 512], in_=self.pb[b][:sp, :],
                                                   func=AF.Copy), reads=[self.pbr[b]], writes=[self.vtr[s]])
            self.proj_tm(wt, wres, self.actT, self.actr, nsub, sp, ev)
        decay_chain()
        for n in range(2):
            wt, wres = self.W(f"og{n}")

            def ev(s, b, n=n):
                t, tr = self.tmpbuf()
                self.sigmoid_to(b, 512, t[:sp, :], tr, P=sp)
                K.op("dve", lambda e: e.tensor_tensor(out=self.ogs[:sp, s, n * 512:(n + 1) * 512], in0=self.pb[b][:sp, :],
                                                     in1=t[:sp, :], op=ALU.mult),
                     reads=[self.pbr[b], tr], writes=[self.ogsr[s]])
            self.proj_tm(wt, wres, self.actT, self.actr, nsub, sp, ev)
        for n in range(2):
            wt, wres = self.W(f"u{n}")

            def ev(s, b, n=n):
                sl = uslots[s]
                K.op("act", lambda e: e.activation(out=self.ur[:sp, sl, n * 512:(n + 1) * 512], in_=self.pb[b][:sp, :],
                                                   func=AF.Copy), reads=[self.pbr[b]], writes=[self.urr[sl]])
                if last_u_fp32 is not None and s == nsub - 1:
                    st, str_ = last_u_fp32
                    K.op("dve", lambda e: e.tensor_copy(st[:sp, n * 512:(n + 1) * 512], self.pb[b][:sp, :]),
                         reads=[self.pbr[b]], writes=list(str_))
            self.proj_tm(wt, wres, self.actT, self.actr, nsub, sp, ev)
        for gt, gr, nm in ((self.ga, self.gar, "ga"), (self.gb, self.gbr, "gb")):
            for n in range(2):
                wt, wres = self.W(f"{nm}{n}")

                def ev(m, b, n=n, gt=gt, gr=gr):
                    if m is None:
                        self.sigmoid_to(b, T, gt[:, n * 4:n * 4 + 4, :T], gr[n * 4:n * 4 + 4], ncol=4 * T, fold=T)
                        return
                    self.sigmoid_to(b, T, gt[:, n * 4 + m, :T], gr[n * 4 + m])
                self.proj_fm(wt, wres, range(4), self.actT, self.actr, T, ev)
        wt, wres = self.W("k")

        def evk(h, b):
            if h is None:
                K.op("dve", lambda e: e.tensor_tensor(out=self.kd[:, :, :T], in0=self.pb[b][:, :4 * T].rearrange("p (m t) -> p m t", t=T),
                                                     in1=E1[:, :, :T], op=ALU.mult),
                     reads=[self.pbr[b]] + E1r, writes=self.kdr)
                return
            K.op("dve", lambda e: e.tensor_tensor(out=self.kd[:, h, :T], in0=self.pb[b][:, :T], in1=E1[:, h, :T], op=ALU.mult),
                 reads=[self.pbr[b]] + E1r[2 * h:2 * h + 2], writes=[self.kdr[h]])
        self.proj_fm(wt, wres, range(4), self.actT, self.actr, T, evk)
        wt, wres = self.W("q")

        def evq(h, b):
            if h is None:
                K.op("dve", lambda e: e.tensor_tensor(out=self.qi[:, :, :T], in0=self.pb[b][:, :4 * T].rearrange("p (m t) -> p m t", t=T),
                                                     in1=E2[:, :, :T], op=ALU.mult),
                     reads=[self.pbr[b]] + E2r, writes=self.qir)
                return
            K.op("dve", lambda e: e.tensor_tensor(out=self.qi[:, h, :T], in0=self.pb[b][:, :T], in1=E2[:, h, :T], op=ALU.mult),
                 reads=[self.pbr[b]] + E2r[2 * h:2 * h + 2], writes=[self.qir[h]])
        self.proj_fm(wt, wres, range(4), self.actT, self.actr, T, evq)
        for c in range(nch if T == 512 else 1):
            cl = 128 if T == 512 else 64
            b = self.bank()
            for h in range(4):
                K.op("pe", lambda e, b=b, h=h, c=c, cl=cl: e.transpose(self.pbf[b][:cl, h * 128:(h + 1) * 128],
                                                                      self.kd[:, h, c * cl:(c + 1) * cl], self.ident[:, :]),
                     reads=[self.kdr[h], self.identr], writes=[self.pbr[b]], signal=(h == 3))
            K.op("act", lambda e, b=b, c=c, cl=cl: e.activation(out=self.kdt[:cl, c, :], in_=self.pbf[b][:cl, :512], func=AF.Copy),
                 reads=[self.pbr[b]], writes=[self.kdtr[c]])

    def branches_out(self, o2T, o2Tr, pooledT, pooledTr, nsub, sp, T):
        K = self.K
        mT, mTr = self.r1_bf(2)
        wts = {}
        for m in range(8):
            if m % 4 == 0:
                wts["b"] = self.W(f"b{m // 4}")
                wts["a"] = self.W(f"a{m // 4}")
            bb = self.bank()
            wt, wres = wts["b"]
            self.mm(self.pb[bb][:, :T], [(wt[:, kc, (m % 4) * 128:(m % 4 + 1) * 128], pooledT[:, kc, :T]) for kc in range(8)],
                    [wres] + list(pooledTr), self.pbr[bb])
            ba = self.bank()
            wt, wres = wts["a"]
            self.mm(self.pb[ba][:, :T], [(wt[:, kc, (m % 4) * 128:(m % 4 + 1) * 128], o2T[:, kc, :T]) for kc in range(8)],
                    [wres] + list(o2Tr), self.pbr[ba])
            t1, t1r = self.tmpbuf()
            t2, t2r = self.tmpbuf()
            K.op("dve", lambda e, bb=bb, m=m, t1=t1: e.tensor_tensor(out=t1[:, :T], in0=self.pb[bb][:, :T], in1=self.gb[:, m, :T], op=ALU.mult),
                 reads=[self.pbr[bb], self.gbr[m]], writes=[t1r])
            K.op("dve", lambda e, ba=ba, m=m, t2=t2: e.tensor_tensor(out=t2[:, :T], in0=self.pb[ba][:, :T], in1=self.ga[:, m, :T], op=ALU.mult),
                 reads=[self.pbr[ba], self.gar[m]], writes=[t2r])
            K.op("dve", lambda e, m=m, t1=t1, t2=t2: e.tensor_tensor(out=mT[:, m, :T], in0=t1[:, :T], in1=t2[:, :T], op=ALU.add),
                 reads=[t1r, t2r], writes=[mTr[m]])
        self.resid_norm("o", mT, mTr, nsub, sp, "g_x")

    def pool_mix(self, dT, dTr, pT, pTr, T):
        K = self.K
        wt, wres = self.W("pm")
        for g in range(4):
            for dch in range(2):
                b = self.bank()
                pairs = [(wt[:, g * 2 + kc, dch * 128:(dch + 1) * 128], dT[:, 2 * g + kc, :T]) for kc in range(2)]
                self.mm(self.pb[b][:, :T], pairs, [wres, dTr[2 * g], dTr[2 * g + 1]], self.pbr[b])
                K.op("act", lambda e, b=b, g=g, dch=dch: e.activation(out=pT[:, 2 * g + dch, :T], in_=self.pb[b][:, :T], func=AF.Copy,
                                                                    scale=self.col("pscale", 2 * g + dch)),
                     reads=[self.pbr[b], self.colsr], writes=[pTr[2 * g + dch]])

    def resid_norm(self, wname, srcT, srcr, nsub, sp, gname):
        K = self.K
        ws = [self.W(f"{wname}0"), self.W(f"{wname}1")]
        for s in range(nsub):
            for n in range(2):
                wt, wres = ws[n]
                b = self.bank()
                pairs = [(srcT[:, kc, s * sp:(s + 1) * sp], wt[:, kc, :]) for kc in range(8)]
                self.mm(self.pb[b][:sp, :], pairs, [wres] + list(srcr), self.pbr[b])
                K.op("dve", lambda e, s=s, n=n, b=b: e.tensor_tensor(out=self.xh[:sp, s, n * 512:(n + 1) * 512],
                                                                    in0=self.xh[:sp, s, n * 512:(n + 1) * 512],
                                                                    in1=self.pb[b][:sp, :], op=ALU.add),
                     reads=[self.pbr[b], self.xhr[s]], writes=[self.xhr[s]])
            if s >= 1:
                self.trans_sub(self.xn, self.xnr, self.actT, self.actr, s - 1, sp, gname)
            self.norm_sub(s, sp)
        if nsub == 4 and sp == 128 and HIDE_TAIL:
            self.pending_trans = lambda: self.trans_sub(self.xn, self.xnr, self.actT, self.actr, nsub - 1, sp, gname)
        else:
            self.trans_sub(self.xn, self.xnr, self.actT, self.actr, nsub - 1, sp, gname)

    def out_resid(self, wname, srcT, srcr, nsub, sp):
        K = self.K
        for n in range(2):
            wt, wres = self.W(f"{wname}{n}")

            def ev(s, b, n=n):
                K.op("dve", lambda e: e.tensor_tensor(out=self.xh[:sp, s, n * 512:(n + 1) * 512],
                                                     in0=self.xh[:sp, s, n * 512:(n + 1) * 512],
                                                     in1=self.pb[b][:sp, :], op=ALU.add),
                     reads=[self.pbr[b], self.xhr[s]], writes=[self.xhr[s]])
            self.proj_tm(wt, wres, srcT, srcr, nsub, sp, ev)

    def softmax_stages(self, hb, P, Pn, Pnr, par=0, c_eng="dve"):
        K = self.K
        sm = self.sm2r[par]
        cb = (8, 64, 80)[par]
        mx = self.small[:P, cb:cb + 4]
        nmx = self.small[:P, cb + 4:cb + 8]
        ssum = self.small[:P, cb + 8:cb + 12]
        rs = self.small[:P, cb + 12:cb + 16]
        bufs = {}

        def stage_a():
            h = 0
            while h < 4:
                b, co = hb[h]
                if h + 1 < 4 and hb[h + 1] == (b, co + 256):
                    K.op("dve", lambda e, b=b, h=h, co=co: e.reduce_max(
                        out=self.small[:P, cb + 4 + h:cb + 4 + h + 2], in_=self.pb[b][:P, co:co + 512].rearrange("p (h m) -> p h m", m=256),
                        axis=AX.X, negate=True),
                         reads=[self.pbr[b]], writes=[sm])
                    h += 2
                else:
                    K.op("dve", lambda e, b=b, h=h, co=co: e.reduce_max(out=self.small[:P, cb + 4 + h:cb + 4 + h + 1], in_=self.pb[b][:P, co:co + 256],
                                                                        axis=AX.X, negate=True),
                         reads=[self.pbr[b]], writes=[sm])
                    h += 1

        def stage_b():
            bufs["pf"] = [self.tmpbuf(), self.tmpbuf()]
            for h, (b, co) in enumerate(hb):
                pf, pfr = bufs["pf"][h // 2]
                K.op("act", lambda e, b=b, h=h, pf=pf, co=co: e.activation(out=pf[:P, (h % 2) * 256:(h % 2 + 1) * 256],
                                                                           in_=self.pb[b][:P, co:co + 256],
                                                                           func=AF.Exp, scale=1.0, bias=self.small[:P, cb + 4 + h:cb + 5 + h],
                                                                           accum_out=self.small[:P, cb + 8 + h:cb + 9 + h]),
                     reads=[self.pbr[b], sm], writes=[pfr, sm])

        def stage_c():
            K.op("dve", lambda e: e.reciprocal(out=rs, in_=ssum), reads=[sm], writes=[sm])
            for hp, (pf, pfr) in enumerate(bufs["pf"]):
                K.op(c_eng, lambda e, hp=hp, pf=pf: e.tensor_tensor(
                    out=Pn[:P, 2 * hp:2 * hp + 2, :], in0=pf[:P, :].rearrange("p (h m) -> p h m", m=256),
                    in1=self.small[:P, cb + 12 + 2 * hp:cb + 14 + 2 * hp].unsqueeze(2).to_broadcast([P, 2, 256]), op=ALU.mult),
                     reads=[pfr, sm], writes=list(Pnr))
        return stage_a, stage_b, stage_c

    def softmax_rows(self, hb, P, Pn, Pnr, par=0):
        fa, fb, fc = self.softmax_stages(hb, P, Pn, Pnr, par)
        fa()
        fb()
        fc()

    def mlp_final(self, nsub, sp, T, ydst, yres_fn, pro_a=None, pro_b=None):
        K = self.K
        hid = self.R1[:, :].rearrange("p (c t) -> p c t", t=512)
        hidr = self.R1r
        for i in range(8):
            wt, wres = self.W(f"up{i}")

            def ev(m, b, i=i):
                t, tr = self.tmpbuf()
                if m is None:
                    K.op("act", lambda e: e.activation(out=t[:, :4 * T], in_=self.pb[b][:, :4 * T], func=AF.Relu),
                         reads=[self.pbr[b]], writes=[tr])
                    K.op("dve", lambda e: e.scalar_tensor_tensor(out=hid[:, 4 * i:4 * i + 4, :T],
                                                                in0=self.pb[b][:, :4 * T].rearrange("p (m t) -> p m t", t=T), scalar=0.0,
                                                                in1=t[:, :4 * T].rearrange("p (m t) -> p m t", t=T), op0=ALU.max, op1=ALU.mult),
                         reads=[self.pbr[b], tr], writes=hidr[4 * i:4 * i + 4])
                    return
                K.op("act", lambda e: e.activation(out=t[:, :T], in_=self.pb[b][:, :T], func=AF.Relu),
                     reads=[self.pbr[b]], writes=[tr])
                K.op("dve", lambda e: e.scalar_tensor_tensor(out=hid[:, 4 * i + m, :T], in0=self.pb[b][:, :T], scalar=0.0,
                                                            in1=t[:, :T], op0=ALU.max, op1=ALU.mult),
                     reads=[self.pbr[b], tr], writes=[hidr[4 * i + m]])
            self.proj_fm(wt, wres, range(4), self.actT, self.actr, T, ev)
        if pro_a is not None:
            pro_a()
        for n in range(2):
            bs = [self.bank() for _ in range(nsub)]
            for j in range(4):
                wt, wres = self.W(f"dn{j}_{n}")
                for s in range(nsub):
                    pairs = [(hid[:, 8 * j + kc, s * sp:(s + 1) * sp], wt[:, kc, :]) for kc in range(8)]
                    self.mm(self.pb[bs[s]][:sp, :], pairs, [wres] + hidr[8 * j:8 * j + 8], self.pbr[bs[s]],
                            start=(j == 0), stop=(j == 3))
            for s in range(nsub):
                b = bs[s]
                K.op("dve", lambda e, s=s, b=b, n=n: e.tensor_tensor(out=self.xh[:sp, s, n * 512:(n + 1) * 512],
                                                                    in0=self.xh[:sp, s, n * 512:(n + 1) * 512],
                                                                    in1=self.pb[b][:sp, :], op=ALU.add),
                     reads=[self.pbr[b], self.xhr[s]], writes=[self.xhr[s]])
            if n == 0 and pro_b is not None:
                pro_b()
        yst = self.R1[:, 0:8192].bitcast(F32).rearrange("p (s d) -> p s d", d=D)
        ss = self.small[:sp, 56:56 + nsub]
        rs = self.small[:sp, 60:60 + nsub]
        for s in range(nsub):
            K.op("act", lambda e, s=s: e.activation(out=self.junk2[:sp, :], in_=self.xh[:sp, s, :], func=AF.Square,
                                                    accum_out=self.small[:sp, 56 + s:57 + s]),
                 reads=[self.xhr[s], self.smallr], writes=[self.junk2r, self.smallr])
        self.rstd_from_ss(ss, rs, 1.0 / D, sp)
        for s in range(nsub):
            K.op("dve", lambda e, s=s: e.scalar_tensor_tensor(out=yst[:sp, s, :], in0=self.xh[:sp, s, :],
                                                             scalar=self.small[:sp, 60 + s:61 + s], in1=self.gfin[:sp, :],
                                                             op0=ALU.mult, op1=ALU.mult),
                 reads=[self.xhr[s], self.smallr, self.gfinr], writes=self.R1r[4 * s:4 * s + 4])
            K.dma("sp", yres_fn(s), yst[:sp, s, :], reads=self.R1r[4 * s:4 * s + 4])

    def prompt_tile(self, ti):
        K = self.K
        I, O = self.I, self.O
        T, nsub, sp = 512, 4, 128
        if ti == 0:
            self.norm_to_T("g_mix", nsub, sp, T)
        else:
            for s in range(nsub):
                xa, xr = self.xnext(s)
                K.op("dve", lambda e, s=s, xa=xa: e.tensor_copy(self.xh[:, s, :], xa), reads=list(xr), writes=[self.xhr[s]])
        self.dbg("xnT", self.actT[:, :, :], self.actr, [128, 8, 512])
        uslots = [(ti * 4 + s) % 5 for s in range(4)]
        last = None
        if ti == SEQ // 512 - 1:
            last = self.r1_dummy_ufp()
        self.in_proj(nsub, sp, T, uslots, last_u_fp32=last)
        self.dbg("kd", self.kd[:, :, :], self.kdr, [128, 4, 512])
        self.dbg("qi", self.qi[:, :, :], self.qir, [128, 4, 512])
        if last is not None:
            K.dma("sp", O["spp"][:, :], last[0][113:128, :], reads=list(last[1]))
        o2 = self.xn
        o2r = self.xnr
        for c in range(4):
            cs = slice(c * 128, (c + 1) * 128)
            b = self.bank()
            for h in range(4):
                self.mm(self.pb[b][:, h * 128:(h + 1) * 128], [(self.kd[:, h, cs], self.qi[:, h, cs])],
                        [self.kdr[h], self.qir[h]], self.pbr[b], signal=(h == 3))
            K.op("dve", lambda e, b=b, c=c: e.tensor_tensor(
                out=self.att[:, c, :, :], in0=self.pb[b][:, :].rearrange("p (h t) -> p h t", t=128),
                in1=self.maskT[:, :].unsqueeze(1).to_broadcast([128, 4, 128]), op=ALU.mult),
                 reads=[self.pbr[b], self.maskTr], writes=[self.attr[c]])
            for hp in range(2):
                b2 = self.bank()
                for h in (2 * hp, 2 * hp + 1):
                    self.mm(self.pb[b2][:, (h % 2) * 256:(h % 2 + 1) * 256],
                            [(self.kdt[:, c, h * 128:(h + 1) * 128], self.vt[:, c, h * 256:(h + 1) * 256])],
                            [self.kdtr[c], self.vtr[c]], self.pbr[b2], signal=(h % 2 == 1))
                for h in (2 * hp, 2 * hp + 1):
                    K.op("act", lambda e, h=h, c=c: e.activation(out=self.Sb[:, c, h, :], in_=self.S[:, h, :], func=AF.Copy,
                                                                 scale=self.eB[:, h, c:c + 1]),
                         reads=[self.Sr[h], self.eBr], writes=[self.Sbr[c * 4 + h]])
                    K.op("dve", lambda e, h=h, b2=b2, c=c: e.scalar_tensor_tensor(
                        out=self.S[:, h, :], in0=self.S[:, h, :], scalar=self.eB[:, h, c:c + 1],
                        in1=self.pb[b2][:, (h % 2) * 256:(h % 2 + 1) * 256], op0=ALU.mult, op1=ALU.add),
                         reads=[self.Sr[h], self.eBr, self.pbr[b2]], writes=[self.Sr[h]])
        dT, dTr = self.r1_bf(0)
        pT, pTr = self.r1_bf(1)

        def pool_cc(cc):
            g = cc // 2
            b = self.bank()
            for s in range(4):
                cur = uslots[s]
                prev = (cur + 4) % 5
                csl = slice(cc * 128, (cc + 1) * 128)
                if ti == 0 and s == 0:
                    pairs = [(self.ur[:, cur, csl], self.pmat[:, 2, g, :]), (self.ur[:, cur, csl], self.pmat[:, 3, g, :])]
                    rd = [self.urr[cur], self.pmatr]
                else:
                    pairs = [(self.ur[:, prev, csl], self.pmat[:, 1, g, :]), (self.ur[:, cur, csl], self.pmat[:, 0, g, :])]
                    rd = [self.urr[cur], self.urr[prev], self.pmatr]
                self.mm(self.pb[b][:, s * 128:(s + 1) * 128], pairs, rd, self.pbr[b], signal=(s == 3))
            K.op("act", lambda e, b=b, cc=cc: e.activation(out=dT[:, cc, :], in_=self.pb[b][:, :], func=AF.Copy),
                 reads=[self.pbr[b]], writes=[dTr[cc]])

        for cc in range(4):
            pool_cc(cc)
        for c in range(4):
            cs = slice(c * 128, (c + 1) * 128)
            sr = self.osr[c]
            obanks = []
            for hp in range(2):
                b3 = self.bank()
                obanks.append(b3)
                for h in (2 * hp, 2 * hp + 1):
                    self.mm(self.pb[b3][:, (h % 2) * 256:(h % 2 + 1) * 256],
                            [(self.att[:, c, h, :], self.vt[:, c, h * 256:(h + 1) * 256]),
                             (self.qi[:, h, cs], self.Sb[:, c, h, :])],
                            [self.attr[c], self.vtr[c], self.qir[h], self.Sbr[c * 4 + h]], self.pbr[b3], signal=(h % 2 == 1))
                for h in (2 * hp, 2 * hp + 1):
                    K.op("act", lambda e, h=h, b3=b3, c=c: e.activation(out=self.junk[:, :256], in_=self.pb[b3][:, (h % 2) * 256:(h % 2 + 1) * 256],
                                                                        func=AF.Square, accum_out=self.small[:, 24 + c * 4 + h:25 + c * 4 + h]),
                         reads=[self.pbr[b3], sr], writes=[self.junkr, sr])
            self.rstd_from_ss(self.small[:, 24 + c * 4:28 + c * 4], self.small[:, 40 + c * 4:44 + c * 4], 1.0 / 256, 128, sr=sr)
            for h in range(4):
                b3 = obanks[h // 2]
                K.op("dve", lambda e, h=h, b3=b3, c=c: e.scalar_tensor_tensor(
                    out=o2[:, c, h * 256:(h + 1) * 256], in0=self.pb[b3][:, (h % 2) * 256:(h % 2 + 1) * 256],
                    scalar=self.small[:, 40 + c * 4 + h:41 + c * 4 + h], in1=self.ogs[:, c, h * 256:(h + 1) * 256],
                    op0=ALU.mult, op1=ALU.mult),
                     reads=[self.pbr[b3], sr, self.ogsr[c]], writes=[o2r[c]])
        for cc in range(4, 8):
            pool_cc(cc)
        self.pool_mix(dT, dTr, pT, pTr, T)
        for c in range(4):
            self.trans_sub(o2, o2r, self.actT, self.actr, c, sp, "g_gla", gmod=2)
        self.branches_out(self.actT, self.actr, pT, pTr, nsub, sp, T)
        self.dbg("h1", self.xh[:, :, :], self.xhr, [128, 4, 1024])
        qx, qxr = self.r1_bf(0)
        PT, PTr = self.r1_bf(1)
        ox, oxr = self.r1_bf(3)
        for n in range(2):
            wt, wres = self.W(f"xq{n}")

            def ev(m, b, n=n):
                K.op("act", lambda e: e.activation(out=qx[:, n * 4 + m, :], in_=self.pb[b][:, :], func=AF.Copy, scale=1.0 / 16),
                     reads=[self.pbr[b]], writes=[qxr[n * 4 + m]])
            self.proj_fm(wt, wres, range(4), self.actT, self.actr, T, ev)
        Pns = [self.att[:, 2 * par:2 * par + 2, :, :].rearrange("p a h t -> p (a h t)").rearrange("p (h m) -> p h m", m=256)
               for par in range(2)]
        Pnrs = [[self.attr[0], self.attr[1]], [self.attr[2], self.attr[3]]]

        def scores(s):
            ss_ = slice(s * 128, (s + 1) * 128)
            banks = []
            for hp in range(2):
                b = self.bank()
                banks.append(b)
                for h in (2 * hp, 2 * hp + 1):
                    pairs = [(qx[:, 2 * h + dc, ss_], self.KTp[:, 2 * h + dc, :]) for dc in range(2)]
                    self.mm(self.pb[b][:, (h % 2) * 256:(h % 2 + 1) * 256], pairs,
                            [qxr[2 * h], qxr[2 * h + 1], self.KTpr], self.pbr[b], signal=(h % 2 == 1))
            return self.softmax_stages([(banks[h // 2], (h % 2) * 256) for h in range(4)], 128, Pns[s % 2], Pnrs[s % 2], par=s % 3,
                                       c_eng=POOL_SOFTMAX)

        def ptrans(s):
            ss_ = slice(s * 128, (s + 1) * 128)
            Pn, Pnr = Pns[s % 2], Pnrs[s % 2]
            b = self.bank()
            for h in range(4):
                for mt in range(2):
                    j = h * 2 + mt
                    K.op("pe", lambda e, b=b, h=h, mt=mt, j=j: e.transpose(self.pbf[b][:, j * 128:(j + 1) * 128],
                                                                          Pn[:, h, mt * 128:(mt + 1) * 128], self.ident[:, :]),
                         reads=list(Pnr) + [self.identr], writes=[self.pbr[b]], signal=(j == 7))
            K.op("dve", lambda e, b=b, ss_=ss_: e.tensor_copy(PT[:, :, ss_], self.pbf[b][:, :].rearrange("p (j t) -> p j t", t=128)),
                 reads=[self.pbr[b]], writes=PTr)

        st = {}
        st[0] = scores(0); st[0][0](); st[0][1]()
        st[1] = scores(1); st[1][0]()
        st[2] = scores(2); st[2][0]()
        st[0][2](); st[1][1]()
        ptrans(0)
        st[3] = scores(3); st[3][0]()
        st[1][2](); st[2][1]()
        ptrans(1)
        st[2][2](); st[3][1]()
        ptrans(2)
        st[3][2]()
        ptrans(3)
        for h in range(4):
            for dc in range(2):
                b = self.bank()
                pairs = [(self.Vp[:, mt, h * 256 + dc * 128:h * 256 + (dc + 1) * 128], PT[:, h * 2 + mt, :]) for mt in range(2)]
                self.mm(self.pb[b][:, :], pairs, [self.Vpr, PTr[h * 2], PTr[h * 2 + 1]], self.pbr[b])
                K.op("act", lambda e, b=b, h=h, dc=dc: e.activation(out=ox[:, 2 * h + dc, :], in_=self.pb[b][:, :], func=AF.Copy),
                     reads=[self.pbr[b]], writes=[oxr[2 * h + dc]])
        self.resid_norm("xo", ox, oxr, nsub, sp, "g_mlp")
        self.dbg("h2", self.xh[:, :, :], self.xhr, [128, 4, 1024])
        pro_a = pro_b = None
        if ti + 1 == SEQ // 512 and self.do_sample:
            def pro_a():
                xa, xr = self.xnext(0)
                K.dma("sp", xa[:SB * ST, :], I["xs"][:, :], writes=xr)
                self.norm_sub(0, SB * ST, src=xa[:SB * ST, :], srcr=xr)

            def pro_b():
                self.trans_sub(self.xn, self.xnr, self.actT, self.actr, 0, SB * ST, "g_mix")
        if ti + 1 < SEQ // 512:
            def pro_a():
                for s in range(nsub):
                    xa, xr = self.xnext(s)
                    r0 = (ti + 1) * 512 + s * 128
                    K.dma("sp", xa, I["xp"][r0:r0 + 128, :], writes=xr)
                for s in range(nsub):
                    xa, xr = self.xnext(s)
                    self.norm_sub(s, sp, src=xa, srcr=xr)

            def pro_b():
                for s in range(nsub):
                    self.trans_sub(self.xn, self.xnr, self.actT, self.actr, s, sp, "g_mix")
        self.mlp_final(nsub, sp, T, None, lambda s: O["yp"][ti * 512 + s * 128:ti * 512 + (s + 1) * 128, :],
                       pro_a=pro_a, pro_b=pro_b)

    def r1_dummy_ufp(self):
        return self.ufp, [self.xnr[0], self.xnr[1]]

    def prompt_finish(self):
        K = self.K
        O = self.O
        for h in range(4):
            K.dma("sp", O["sgp"][h, :, :], self.S[:, h, :], reads=[self.Sr[h]])

    def sample_pass(self):
        K = self.K
        I, O = self.I, self.O
        T, nsub, sp = 64, 1, 64
        NB = SB
        xa, xr = self.xnext(0)
        K.op("dve", lambda e: e.tensor_copy(self.xh[:sp, 0, :], xa[:sp, :]), reads=list(xr), writes=[self.xhr[0]])
        ufp, ufpr = self.r1_dummy_ufp()
        self.in_proj(nsub, sp, T, [0], last_u_fp32=(ufp, ufpr))
        qblk = self.Sb[:, :, :, :].rearrange("p a b c -> p (a b c)").rearrange("p (h b t) -> p h b t", h=4, b=NB)
        qblkr = self.Sbr
        K.op("dve", lambda e: e.tensor_tensor(out=qblk, in0=self.qi[:, :, :T].unsqueeze(2).to_broadcast([128, 4, NB, T]),
                                             in1=self.bmq[:, :, :].unsqueeze(1).to_broadcast([128, 4, NB, T]), op=ALU.mult),
             reads=self.qir + [self.bmqr], writes=qblkr)
        kblk = self.R1[:sp, 0:8192].rearrange("p (b d) -> p b d", d=512)
        kblkr = self.R1r[0:16]
        K.op("dve", lambda e: e.tensor_tensor(out=kblk, in0=self.kdt[:sp, 0, :].unsqueeze(1).to_broadcast([sp, NB, 512]),
                                             in1=self.bmk[:sp, :].unsqueeze(2).to_broadcast([sp, NB, 512]), op=ALU.mult),
             reads=[self.kdtr[0], self.bmkr], writes=kblkr)
        b0 = self.bank()
        for h in range(4):
            self.mm(self.pb[b0][:sp, h * 64:(h + 1) * 64], [(self.kd[:, h, :T], self.qi[:, h, :T])],
                    [self.kdr[h], self.qir[h]], self.pbr[b0], signal=(h == 3))
        K.op("dve", lambda e: e.tensor_tensor(out=self.att[:sp, 0, :, :64],
                                             in0=self.pb[b0][:sp, :256].rearrange("p (h t) -> p h t", t=64),
                                             in1=self.smask[:, :].unsqueeze(1).to_broadcast([sp, 4, 64]), op=ALU.mult),
             reads=[self.pbr[b0], self.smaskr], writes=[self.attr[0]])
        ob = self.hold(4)
        for h in range(4):
            self.mm(self.pb[ob[h]][:sp, :256], [(self.att[:sp, 0, h, :64], self.vt[:sp, 0, h * 256:(h + 1) * 256])],
                    [self.attr[0], self.vtr[0]], self.pbr[ob[h]], start=True, stop=False)
        s0in = [self.xh[:, 1, :].rearrange("p (h v) -> p h v", v=256), self.xh[:, 2, :].rearrange("p (h v) -> p h v", v=256),
                self.Vp[:, :, :].rearrange("p a b -> p (a b)").bitcast(F32).rearrange("p (h v) -> p h v", v=256),
                self.S[:, :, :]]
        s0inr = [self.xhr[1], self.xhr[2], self.Vpr, self.Sr]
        NS0 = 4
        s0p = [self.vt[:, 1, :].rearrange("p (h v) -> p h v", v=256), self.vt[:, 2, :].rearrange("p (h v) -> p h v", v=256)]
        s0pr = [self.vtr[1], self.vtr[2]]
        snew = [self.xh[:, 3, :].rearrange("p (h v) -> p h v", v=256),
                self.KTp[:, :, :].rearrange("p a b -> p (a b)").bitcast(F32).rearrange("p (h v) -> p h v", v=256)]
        snewr = [self.xhr[3], self.KTpr]
        def s0w(i):
            r = s0inr[i % NS0]
            return list(r) if isinstance(r, (list, tuple)) else [r]

        for b in range(NS0 - 1):
            K.dma("sp", s0in[b], I["sgla"][b].rearrange("h k v -> k h v"), writes=s0w(b))
        cpr = Res("spscopy")
        K.dma("sp", O["sps"][:, 0:11, :], I["spool"][:, 4:15, :], writes=[cpr])
        for b in range(NB):
            K.dma("sp", O["sps"][b, 11:15, :], ufp[b * ST:(b + 1) * ST, :], reads=list(ufpr))
        for b in range(NB):
            bb = b % 2
            if b + NS0 - 1 < NB:
                K.dma("sp", s0in[(b + NS0 - 1) % NS0], I["sgla"][b + NS0 - 1].rearrange("h k v -> k h v"), writes=s0w(b + NS0 - 1))
            for h in range(4):
                K.op("act", lambda e, h=h, b=b, bb=bb: e.activation(out=s0p[bb][:, h, :], in_=s0in[b % NS0][:, h, :], func=AF.Copy,
                                                                    scale=self.eB[:, h, b:b + 1]),
                     reads=s0w(b) + [self.eBr], writes=[s0pr[bb]])
            for h in range(4):
                self.mm(self.pb[ob[h]][:sp, :256], [(qblk[:, h, b, :], s0p[bb][:, h, :])], qblkr + [s0pr[bb]], self.pbr[ob[h]],
                        start=False, stop=(b == NB - 1))
            for hp in range(2):
                b2 = self.bank()
                for h in (2 * hp, 2 * hp + 1):
                    self.mm(self.pb[b2][:, (h % 2) * 256:(h % 2 + 1) * 256],
                            [(kblk[:, b, h * 128:(h + 1) * 128], self.vt[:sp, 0, h * 256:(h + 1) * 256])],
                            kblkr + [self.vtr[0]], self.pbr[b2], signal=(h % 2 == 1))
                for h in (2 * hp, 2 * hp + 1):
                    K.op("dve", lambda e, h=h, b=b, bb=bb, b2=b2: e.scalar_tensor_tensor(
                        out=snew[bb][:, h, :], in0=s0in[b % NS0][:, h, :], scalar=self.eB[:, h, b:b + 1],
                        in1=self.pb[b2][:, (h % 2) * 256:(h % 2 + 1) * 256], op0=ALU.mult, op1=ALU.add),
                         reads=s0w(b) + [self.eBr, self.pbr[b2]], writes=[snewr[bb]])
            K.dma("pool", O["sgs"][b].rearrange("h k v -> k h v"), snew[bb], reads=[snewr[bb]])
        o2, o2r = self.xn, self.xnr
        for h in range(4):
            K.op("act", lambda e, h=h: e.activation(out=self.junk[:sp, :256], in_=self.pb[ob[h]][:sp, :256], func=AF.Square,
                                                    accum_out=self.small[:sp, 24 + h:25 + h]),
                 reads=[self.pbr[ob[h]], self.smallr], writes=[self.junkr, self.smallr])
        self.rstd_from_ss(self.small[:sp, 24:28], self.small[:sp, 40:44], 1.0 / 256, sp)
        for h in range(4):
            K.op("dve", lambda e, h=h: e.scalar_tensor_tensor(
                out=o2[:sp, 0, h * 256:(h + 1) * 256], in0=self.pb[ob[h]][:sp, :256], scalar=self.small[:sp, 40 + h:41 + h],
                in1=self.ogs[:sp, 0, h * 256:(h + 1) * 256], op0=ALU.mult, op1=ALU.mult),
                 reads=[self.pbr[ob[h]], self.smallr, self.ogsr[0]], writes=[o2r[0]])
        self.release(ob)
        self.trans_sub(o2, o2r, self.actT, self.actr, 0, sp, "g_gla", gmod=2)
        for kt in range(2):
            K.dma("pool", self.ur[:120, 1 + kt, :], I["spool"][kt * 8:(kt + 1) * 8].rearrange("b j d -> (b j) d"),
                  writes=[self.urr[1 + kt]])
        dT, dTr = self.r1_bf(0)
        pT, pTr = self.r1_bf(1)
        for cc in range(8):
            g = cc // 2
            csl = slice(cc * 128, (cc + 1) * 128)
            b = self.bank()
            pairs = [(self.ur[:120, 1, csl], self.smb[:, 0, g, :]), (self.ur[:120, 2, csl], self.smb[:, 1, g, :]),
                     (self.ur[:sp, 0, csl], self.smu[:, g, :])]
            self.mm(self.pb[b][:, :T], pairs, [self.urr[0], self.urr[1], self.urr[2], self.smbr, self.smur], self.pbr[b])
            K.op("act", lambda e, b=b, cc=cc: e.activation(out=dT[:, cc, :T], in_=self.pb[b][:, :T], func=AF.Copy),
                 reads=[self.pbr[b]], writes=[dTr[cc]])
        self.pool_mix(dT, dTr, pT, pTr, T)
        self.branches_out(self.actT, self.actr, pT, pTr, nsub, sp, T)
        qx, qxr = self.r1_bf(0)
        for n in range(2):
            wt, wres = self.W(f"xq{n}")

            def ev(m, b, n=n):
                if m is None:
                    K.op("act", lambda e: e.activation(out=qx[:, n * 4:n * 4 + 4, :T],
                                                       in_=self.pb[b][:, :4 * T].rearrange("p (m t) -> p m t", t=T), func=AF.Copy, scale=1.0 / 16),
                         reads=[self.pbr[b]], writes=qxr[n * 4:n * 4 + 4])
                    return
                K.op("act", lambda e: e.activation(out=qx[:, n * 4 + m, :T], in_=self.pb[b][:, :T], func=AF.Copy, scale=1.0 / 16),
                     reads=[self.pbr[b]], writes=[qxr[n * 4 + m]])
            self.proj_fm(wt, wres, range(4), self.actT, self.actr, T, ev)
        qxb = self.R1[:, 4096:12288].rearrange("p (j b t) -> p j b t", j=8, b=NB)
        qxbr = self.R1r[8:24]
        K.op("dve", lambda e: e.tensor_tensor(out=qxb, in0=qx[:, :, :T].unsqueeze(2).to_broadcast([128, 8, NB, T]),
                                             in1=self.bmq[:, :, :].unsqueeze(1).to_broadcast([128, 8, NB, T]), op=ALU.mult),
             reads=qxr + [self.bmqr], writes=qxbr)
        q3, q3r = self.r1_bf(3)
        PT = q3[:, :, 0:64]
        ox = q3[:, :, 64:128]
        kvb = [self.ogs[:, 1:3, :], self.ur[:, 3:5, :],
               self.xh[:, 1, :].bitcast(BF16).rearrange("p (a d) -> p a d", d=D),
               self.xh[:, 2, :].bitcast(BF16).rearrange("p (a d) -> p a d", d=D)]
        kvbr = [[self.ogsr[1], self.ogsr[2]], [self.urr[3], self.urr[4]], [self.xhr[1]], [self.xhr[2]]]
        NKV = 4
        kbT = [self.Sb[:, 0:2, :, :].rearrange("p a b c -> p (a b c)").rearrange("p (j m) -> p j m", m=256),
               self.Sb[:, 2:4, :, :].rearrange("p a b c -> p (a b c)").rearrange("p (j m) -> p j m", m=256)]
        kbTr = [self.Sbr[0:8], self.Sbr[8:16]]
        sbk = self.hold(4)

        def k_load_trans(b):
            bb = b % 2
            if USE_KVSCR:
                K.dma("pool", kvb[b % NKV], self.kvscr[0, b].rearrange("(mt p) d -> p mt d", p=128),
                      reads=[self.kvscr_res], writes=kvbr[b % NKV], sem_res=kvbr[b % NKV][0])
            else:
                K.dma("pool", kvb[b % NKV], I["ck"][b].rearrange("(mt p) d -> p mt d", p=128), writes=kvbr[b % NKV])
            for half in range(2):
                bt = self.bank()
                for jj in range(4):
                    j = half * 4 + jj
                    for mt in range(2):
                        K.op("pe", lambda e, bt=bt, jj=jj, mt=mt, j=j, b=b: e.transpose(
                            self.pbf[bt][:, jj * 256 + mt * 128:jj * 256 + (mt + 1) * 128],
                            kvb[b % NKV][:, mt, j * 128:(j + 1) * 128], self.ident[:, :]),
                             reads=kvbr[b % NKV] + [self.identr], writes=[self.pbr[bt]], signal=(jj == 3 and mt == 1))
                if half == 0:
                    K.op("act", lambda e, bt=bt, bb=bb: e.activation(out=kbT[bb][:, 0:4, :],
                                                                     in_=self.pbf[bt][:, :].rearrange("p (j m) -> p j m", m=256), func=AF.Copy),
                         reads=[self.pbr[bt]], writes=kbTr[bb])
                else:
                    K.op("dve", lambda e, bt=bt, bb=bb: e.tensor_copy(kbT[bb][:, 4:8, :],
                                                                      self.pbf[bt][:, :].rearrange("p (j m) -> p j m", m=256)),
                         reads=[self.pbr[bt]], writes=kbTr[bb])

        def k_scores(b):
            bb = b % 2
            for h in range(4):
                pairs = [(qxb[:, 2 * h + dc, b, :], kbT[bb][:, 2 * h + dc, :]) for dc in range(2)]
                self.mm(self.pb[sbk[h]][:sp, :256], pairs, qxbr + kbTr[bb], self.pbr[sbk[h]], start=(b == 0), stop=(b == NB - 1))

        k_load_trans(0)
        for b in range(NB):
            if b + 1 < NB:
                k_load_trans(b + 1)
            k_scores(b)
        Pn = self.att[:, 0:2, :, :].rearrange("p a h t -> p (a h t)").rearrange("p (h m) -> p h m", m=256)
        Pnr = [self.attr[0], self.attr[1]]
        self.softmax_rows([(sbk[h], 0) for h in range(4)], sp, Pn, Pnr)
        self.release(sbk)
        bt = self.bank()
        for h in range(4):
            for mt in range(2):
                j = h * 2 + mt
                K.op("pe", lambda e, bt=bt, h=h, mt=mt, j=j: e.transpose(self.pbf[bt][:, j * 64:(j + 1) * 64],
                                                                        Pn[:sp, h, mt * 128:(mt + 1) * 128], self.ident[:sp, :sp]),
                     reads=list(Pnr) + [self.identr], writes=[self.pbr[bt]], signal=(j == 7))
        K.op("act", lambda e, bt=bt: e.activation(out=PT, in_=self.pbf[bt][:, :512].rearrange("p (j t) -> p j t", t=64), func=AF.Copy),
             reads=[self.pbr[bt]], writes=q3r)
        bo = self.hold(1)[0]
        for b in range(NB):
            bb = b % 2
            if USE_KVSCR:
                K.dma("pool", kvb[b % NKV], self.kvscr[1, b].rearrange("(mt p) d -> p mt d", p=128),
                      reads=[self.kvscr_res], writes=kvbr[b % NKV], sem_res=kvbr[b % NKV][0])
            else:
                K.dma("pool", kvb[b % NKV], I["cv"][b].rearrange("(mt p) d -> p mt d", p=128), writes=kvbr[b % NKV])
            for j in range(8):
                h = j // 2
                pairs = [(kvb[b % NKV][:, mt, j * 128:(j + 1) * 128], PT[:, h * 2 + mt, b * ST:(b + 1) * ST]) for mt in range(2)]
                self.mm(self.pb[bo][:, j * 64 + b * ST:j * 64 + (b + 1) * ST], pairs, kvbr[b % NKV] + q3r, self.pbr[bo],
                        signal=(j == 7))
        K.op("act", lambda e: e.activation(out=ox, in_=self.pb[bo][:, :].rearrange("p (j t) -> p j t", t=64), func=AF.Copy),
             reads=[self.pbr[bo]], writes=q3r)
        self.release([bo])
        self.resid_norm("xo", ox, q3r, nsub, sp, "g_mlp")
        self.mlp_final(nsub, sp, T, None, lambda s: O["ys"][:, :])


def _extra_alloc(self):
    K = self.K
    self.eB, self.eBr = self.T_("eB", [128, 4, 16], F32)
    self.junk2 = self.vt[:, 3, :]
    self.junk2r = self.vtr[3]
    self.ufp = self.xn[:, 0:2, :].rearrange("p a d -> p (a d)").bitcast(F32)
    self.ufpr = None


_old_alloc = Prog.alloc


def _alloc(self):
    _old_alloc(self)
    _extra_alloc(self)


Prog.alloc = _alloc


def _cols_table(p):
    def colmaj(v, n):
        return np.ascontiguousarray(np.asarray(v, np.float32).reshape(n, 128).T)
    parts = [colmaj(p["norm_mix_g"][0], 8), colmaj(p["norm_x_g"][0], 8), colmaj(p["norm_mlp_g"][0], 8),
             colmaj(p["norm_mem_g"][0], 8), colmaj(p["gla_norm_g"][0], 2), colmaj(p["pool_scale"][0], 8),
             colmaj(p["b_gk"][0], 4)]
    return np.ascontiguousarray(np.concatenate(parts, axis=1))


def make_in_maps(inputs, cores):
    p = inputs
    consts = make_consts()
    shared = {
        "w_in": np.ascontiguousarray(p["w_in"][0]), "w_gk": np.ascontiguousarray(p["w_gk_up"][0]),
        "w_pm": np.ascontiguousarray(p["w_pool_mix"][0]),
        "w_a": np.ascontiguousarray(p["w_branch_a"][0]), "w_b": np.ascontiguousarray(p["w_branch_b"][0]),
        "w_o": np.ascontiguousarray(p["w_out"][0]), "w_xq": np.ascontiguousarray(p["w_xq"][0]),
        "w_xk": np.ascontiguousarray(p["w_xk"][0]), "w_xv": np.ascontiguousarray(p["w_xv"][0]),
        "w_xo": np.ascontiguousarray(p["w_xo"][0]), "w_up": np.ascontiguousarray(p["w_up"][0]),
        "w_dn": np.ascontiguousarray(p["w_down"][0]),
        "cols": _cols_table(p), "g_fin": np.ascontiguousarray(p["norm_final_g"]),
    }
    for k, v in consts.items():
        shared["c_" + k] = v
    maps = []
    for c in cores:
        m = dict(shared)
        m["xp"] = np.ascontiguousarray(p["x_prompt"][c])
        m["xs"] = np.ascontiguousarray(p["x_sample"][c * SB:(c + 1) * SB].reshape(SB * ST, D))
        m["mem"] = np.ascontiguousarray(p["mem_prompt"][c])
        m["sgla"] = np.ascontiguousarray(p["state_gla"][0, c * SB:(c + 1) * SB])
        m["spool"] = np.ascontiguousarray(p["state_pool"][0, c * SB:(c + 1) * SB])
        m["ck"] = np.ascontiguousarray(p["cache_mem_k"][0, c * SB:(c + 1) * SB].reshape(SB, MEM, D))
        m["cv"] = np.ascontiguousarray(p["cache_mem_v"][0, c * SB:(c + 1) * SB].reshape(SB, MEM, D))
        maps.append(m)
    return maps


_PROG_CACHE = {}


def get_prog(do_sample=True, dbg=()):
    key = (do_sample, tuple(dbg))
    if key not in _PROG_CACHE:
        _PROG_CACHE[key] = Prog(do_sample=do_sample, dbg=dbg)
    return _PROG_CACHE[key]


def kernel(**inputs):
    inputs = {k: np.asarray(v) for k, v in inputs.items()}
    prog = get_prog(True)
    cores = list(range(NCORE))
    maps = make_in_maps(inputs, cores)
    res = run_bass_kernel_spmd(prog.nc, maps, core_ids=cores)
    R = res.results
    yp = np.stack([R[c]["yp"] for c in cores]).astype(np.float32)
    ys = np.concatenate([R[c]["ys"].reshape(SB, ST, D) for c in cores]).astype(np.float32)
    mk = np.stack([R[c]["mk"].reshape(MEM, 4, 256) for c in cores])[None].astype(np.float32)
    mv = np.stack([R[c]["mv"].reshape(MEM, 4, 256) for c in cores])[None].astype(np.float32)
    sgp = np.stack([R[c]["sgp"] for c in cores])[None].astype(np.float32)
    sgs = np.concatenate([R[c]["sgs"] for c in cores])[None].astype(np.float32)
    spp = np.stack([R[c]["spp"] for c in cores])[None].astype(np.float32)
    sps = np.concatenate([R[c]["sps"] for c in cores])[None].astype(np.float32)
    return (yp, ys, mk, mv, sgp, sgs, spp, sps)
```

```python
import contextlib
import math
import numpy as np
import concourse.bass as bass
import concourse.mybir as mybir
from concourse.bass_utils import run_bass_kernel_spmd

F32 = mybir.dt.float32
BF16 = mybir.dt.bfloat16
AF = mybir.ActivationFunctionType
ALU = mybir.AluOpType
AX = mybir.AxisListType

COMPUTE = ("pe", "act", "dve", "pool")

D = 1024
SEQ = 2048
NCORE = 8
SB = 16
ST = 4
MEM = 256
DFF = 4096
INW = 6160
EPS = 1e-6
NSLOT = 5
USE_SCRATCH = True
USE_KVSCR = True
SCR_SPREAD = 3
HIDE_TAIL = True
POOL_SOFTMAX = "pool"


class Res:
    __slots__ = ("name", "last_w", "reads", "dsem", "dcount", "excl")

    def __init__(self, name, excl=False):
        self.name = name
        self.excl = excl
        self.last_w = None
        self.reads = {}
        self.dsem = None
        self.dcount = 0


class Sched:
    def __init__(self, nc, stack):
        self.nc = nc
        self.stack = stack
        self.streams = {e: [] for e in COMPUTE + ("sp",)}
        self.count = {e: 0 for e in COMPUTE}
        self.sem = {e: stack.enter_context(nc.semaphore(f"S_{e}")) for e in COMPUTE}
        self.waited = {e: {} for e in COMPUTE + ("sp",)}
        self.dma_res = []
        self.n_waits = 0
        self.n_ops = 0
        self.sb_bytes = 0

    def sb(self, name, shape, dt):
        n = 1
        for s in shape[1:]:
            n *= s
        self.sb_bytes += n * (2 if dt == BF16 else 4)
        return self.stack.enter_context(self.nc.sbuf_tensor("sb_" + name, list(shape), dt))

    def ps(self, name, shape, dt):
        return self.stack.enter_context(self.nc.psum_tensor("ps_" + name, list(shape), dt))

    def new_sem(self, name):
        return self.stack.enter_context(self.nc.semaphore(name))

    def _deps(self, eng, reads, writes):
        deps = {}

        def add(ev, raw):
            if ev is None:
                return
            sem, val, src = ev
            if (not raw) and src == eng and eng == "pe":
                return
            k = id(sem)
            if k not in deps or deps[k][1] < val:
                deps[k] = ev

        for r in reads:
            add(r.last_w, True)
            if r.excl:
                for ev in r.reads.values():
                    add(ev, False)
        for w in writes:
            add(w.last_w, False)
            for ev in w.reads.values():
                add(ev, False)
        return deps

    def _emit_waits(self, eng, deps):
        wd = self.waited[eng]
        out = []
        for k, (sem, val, src) in deps.items():
            if wd.get(k, 0) >= val:
                continue
            wd[k] = val
            out.append((sem, val))
        return out

    def _record(self, ev, reads, writes):
        k = id(ev[0])
        for r in reads:
            old = r.reads.get(k)
            if old is None or old[1] < ev[1]:
                r.reads[k] = ev
        for w in writes:
            w.last_w = ev
            w.reads = {}

    def op(self, eng, fn, reads=(), writes=(), signal=True):
        waits = self._emit_waits(eng, self._deps(eng, reads, writes))
        self.n_waits += len(waits)
        self.n_ops += 1
        if signal:
            self.count[eng] += 1
            val = self.count[eng]
        else:
            val = self.count[eng] + 1
        sem = self.sem[eng]
        ev = (sem, val, eng)

        def run(e, waits=waits, fn=fn, signal=signal, sem=sem):
            for s, v in waits:
                e.wait_ge(s, v)
            ins = fn(e)
            if signal:
                ins.then_inc(sem, 1)

        self.streams[eng].append(run)
        self._record(ev, reads, writes)
        return ev

    def dma(self, queue, out, in_, reads=(), writes=(), sem_res=None):
        if sem_res is None:
            sem_res = (list(writes) + list(reads))[0]
        qk = "sw" if queue == "pool" else "hw"
        if sem_res.dsem is None:
            sem_res.dsem = {}
        if qk not in sem_res.dsem:
            ent = [self.new_sem(f"D{qk}_{sem_res.name}"), 0]
            sem_res.dsem[qk] = ent
            self.dma_res.append(ent)
        ent = sem_res.dsem[qk]
        waits = self._emit_waits(queue, self._deps("dma", reads, writes))
        self.n_waits += len(waits)
        ent[1] += 16
        sem = ent[0]
        ev = (sem, ent[1], "dma")

        def run(e, waits=waits, out=out, in_=in_, sem=sem):
            for s, v in waits:
                e.wait_ge(s, v)
            e.dma_start(out=out, in_=in_).then_inc(sem, 16)

        self.streams[queue].append(run)
        self._record(ev, reads, writes)
        return ev

    def finish(self):
        waits = [(ent[0], ent[1]) for ent in self.dma_res]
        for e in COMPUTE:
            if self.count[e]:
                waits.append((self.sem[e], self.count[e]))

        def run(e, waits=waits):
            for s, v in waits:
                e.wait_ge(s, v)

        self.streams["sp"].append(run)

    def emit(self):
        st = self.streams
        with self.nc.Block() as block:
            @block.sync
            def _(e):
                for f in st["sp"]:
                    f(e)

            @block.tensor
            def _(e):
                for f in st["pe"]:
                    f(e)

            @block.scalar
            def _(e):
                for f in st["act"]:
                    f(e)

            @block.vector
            def _(e):
                for f in st["dve"]:
                    f(e)

            @block.gpsimd
            def _(e):
                for f in st["pool"]:
                    f(e)


POOL_WINDOWS = (2, 4, 8, 16)


def _bf16_round(a):
    a = np.asarray(a, np.float32)
    u = a.view(np.uint32).astype(np.uint64)
    r = ((u + 0x7FFF + ((u >> 16) & 1)) >> 16) << 16
    return r.astype(np.uint32).view(np.float32)


def make_consts():
    c = {}
    c["ident"] = np.eye(128, dtype=np.float32)
    s = np.arange(128)[:, None]
    t = np.arange(128)[None, :]
    c["maskT"] = (s <= t).astype(np.float32)
    s6 = np.arange(64)[:, None]
    t6 = np.arange(64)[None, :]
    c["smask"] = ((s6 // ST == t6 // ST) & (s6 <= t6)).astype(np.float32)
    r = np.ones((128, 512), np.float32)
    r[:, ::128] = 0.0
    c["rst512"] = r
    r = np.ones((128, 64), np.float32)
    r[:, ::ST] = 0.0
    c["rst64"] = r
    pm = np.zeros((128, 4, 4, 128), np.float32)
    for g, w in enumerate(POOL_WINDOWS):
        cur = ((s <= t) & (s >= t - w + 1)).astype(np.float32) / w - (s == t).astype(np.float32)
        prev = ((s - 128) >= (t - w + 1)).astype(np.float32) / w
        cnt = np.minimum(t + 1, w).astype(np.float32)
        m0 = ((s <= t) & (s >= t - w + 1)).astype(np.float32) / cnt - (s == t).astype(np.float32)
        hi = _bf16_round(m0)
        lo = _bf16_round(m0 - hi)
        pm[:, 0, g], pm[:, 1, g], pm[:, 2, g], pm[:, 3, g] = cur, prev, hi, lo
    c["pmat"] = pm
    mb = np.zeros((120, 2, 4, 64), np.float32)
    mu = np.zeros((64, 4, 64), np.float32)
    for g, w in enumerate(POOL_WINDOWS):
        for b in range(SB):
            for tt in range(ST):
                col = b * ST + tt
                lo_idx = 15 + tt - w + 1
                for j in range(15):
                    if j >= lo_idx:
                        mb[(b % 8) * 15 + j, b // 8, g, col] = 1.0 / w
                for t2 in range(ST):
                    v = 0.0
                    if t2 <= tt and (15 + t2) >= lo_idx:
                        v += 1.0 / w
                    if t2 == tt:
                        v -= 1.0
                    mu[b * ST + t2, g, col] = v
    c["smb"] = mb
    c["smu"] = mu
    bq = np.zeros((128, SB, 64), np.float32)
    for b in range(SB):
        bq[:, b, b * ST:(b + 1) * ST] = 1.0
    c["bmq"] = bq
    bk = np.zeros((64, SB), np.float32)
    for b in range(SB):
        bk[b * ST:(b + 1) * ST, b] = 1.0
    c["bmk"] = bk
    return c


CONST_SHAPES = {k: v.shape for k, v in make_consts().items()}

COLS = {}
_o = 0
for _n, _w in (("g_mix", 8), ("g_x", 8), ("g_mlp", 8), ("g_mem", 8), ("g_gla", 2), ("pscale", 8), ("bgk", 4)):
    COLS[_n] = (_o, _w)
    _o += _w
NCOL = _o


class StopBuild(Exception):
    pass


class Prog:
    def __init__(self, do_sample=True, dbg=(), stop=None):
        self.stop = stop
        self.dbg_names = dbg
        self.do_sample = do_sample
        self.nc = nc = bass.Bass("TRN2", target_bir_lowering=False)
        self.I = {}
        self.O = {}

        def inp(name, shape):
            self.I[name] = nc.dram_tensor(name, list(shape), F32, kind="ExternalInput").ap()

        def outp(name, shape):
            self.O[name] = nc.dram_tensor(name, list(shape), F32, kind="ExternalOutput").ap()

        inp("xp", [SEQ, D]); inp("xs", [SB * ST, D]); inp("mem", [MEM, D])
        inp("sgla", [SB, 4, 128, 256]); inp("spool", [SB, 15, D])
        inp("ck", [SB, MEM, D]); inp("cv", [SB, MEM, D])
        inp("w_in", [D, INW]); inp("w_gk", [16, 512]); inp("w_pm", [4, 256, 256])
        for n in ("w_a", "w_b", "w_o", "w_xq", "w_xk", "w_xv", "w_xo"):
            inp(n, [D, D])
        inp("w_up", [D, DFF]); inp("w_dn", [DFF, D])
        inp("cols", [128, NCOL]); inp("g_fin", [D])
        for k, shp in CONST_SHAPES.items():
            inp("c_" + k, shp)
        outp("yp", [SEQ, D]); outp("ys", [SB * ST, D]); outp("mk", [MEM, D]); outp("mv", [MEM, D])
        outp("sgp", [4, 128, 256]); outp("sgs", [SB, 4, 128, 256]); outp("spp", [15, D]); outp("sps", [SB, 15, D])
        self.dbg_out = {}

        with contextlib.ExitStack() as stack:
            self.K = K = Sched(nc, stack)
            self.alloc()
            for s_ in range(2):
                xa, xr = self.xnext(s_)
                K.dma("sp", xa, self.I["mem"][s_ * 128:(s_ + 1) * 128, :], writes=xr)
            self.plan_units()
            self.load_consts()
            try:
                self.stage("consts")
                for s_ in range(4):
                    K.dma("sp", self.xh[:, s_, :], self.I["xp"][s_ * 128:(s_ + 1) * 128, :], writes=[self.xhr[s_]])
                self.mem_kv()
                self.stage("memkv")
                for ti in range(SEQ // 512):
                    self.kv_active = ti >= 2
                    self.prompt_tile(ti)
                    self.stage(f"tile{ti}")
                self.kv_active = False
                while self.kv_todo:
                    w, b = self.kv_todo.pop(0)
                    K.dma("pool", self.kvscr[w, b], self.I["ck" if w == 0 else "cv"][b], writes=[self.kvscr_res])
                if self.kvscr_res.dsem is not None:
                    ent = self.kvscr_res.dsem["sw"]
                    self.kvscr_res.last_w = (ent[0], ent[1], "dma")
                self.prompt_finish()
                if do_sample:
                    self.sample_pass()
                assert self.wi == len(self.units), (self.wi, len(self.units))
            except StopBuild:
                print("[kernel] build stopped at", self.stop)
            K.finish()
            K.emit()
            print(f"[kernel] ops={K.n_ops} waits={K.n_waits} sbuf_bytes/partition={K.sb_bytes} "
                  f"dma_sems={len(K.dma_res)} counts={K.count}")

    def stage(self, name):
        if self.stop == name:
            raise StopBuild()

    def T_(self, name, shape, dt, nres=1):
        t = self.K.sb(name, shape, dt)
        if nres == 1:
            return t, Res(name)
        return t, [Res(f"{name}{i}") for i in range(nres)]

    def alloc(self):
        K = self.K
        self.pb = [K.ps(f"pb{i}", [128, 512], F32) for i in range(8)]
        self.pbf = [p.bitcast(BF16) for p in self.pb]
        self.pbr = [Res(f"pb{i}", excl=True) for i in range(8)]
        self.bi = 0
        self.held = set()
        self.pending_trans = None
        self.wt = [K.sb(f"wt{i}", [128, 8, 512], BF16) for i in range(NSLOT)]
        self.wr = [Res(f"wt{i}") for i in range(NSLOT)]
        self.xh, self.xhr = self.T_("xh", [128, 4, D], F32, 4)
        self.xn, self.xnr = self.T_("xn", [128, 4, D], BF16, 4)
        self.actT, self.actr = self.T_("actT", [128, 8, 512], BF16, 8)
        self.S, self.Sr = self.T_("S", [128, 4, 256], F32, 4)
        self.Sb, self.Sbr = self.T_("Sb", [128, 4, 4, 256], BF16, 16)
        self.ur, self.urr = self.T_("uring", [128, 5, D], BF16, 5)
        self.KTp, self.KTpr = self.T_("KTp", [128, 8, 256], BF16)
        self.Vp, self.Vpr = self.T_("Vp", [128, 2, D], BF16)
        self.junk, self.junkr = self.T_("junk", [128, 256], BF16)
        self.small, self.smallr = self.T_("small", [128, 96], F32)
        self.sm2r = [Res("sm_a"), Res("sm_b"), Res("sm_c")]
        self.nsr = [Res(f"ns{i}") for i in range(4)]
        self.osr = [Res(f"os{i}") for i in range(4)]
        self.ident, self.identr = self.T_("ident", [128, 128], BF16)
        self.maskT, self.maskTr = self.T_("maskT", [128, 128], F32)
        self.smask, self.smaskr = self.T_("smask", [64, 64], F32)
        self.rst512, self.rst512r = self.T_("rst512", [128, 512], BF16)
        self.rst64, self.rst64r = self.T_("rst64", [128, 64], F32)
        self.pmat, self.pmatr = self.T_("pmat", [128, 4, 4, 128], BF16)
        self.smb, self.smbr = self.T_("smb", [120, 2, 4, 64], BF16)
        self.smu, self.smur = self.T_("smu", [64, 4, 64], BF16)
        self.bmq, self.bmqr = self.T_("bmq", [128, SB, 64], BF16)
        self.bmk, self.bmkr = self.T_("bmk", [64, SB], BF16)
        self.cols, self.colsr = self.T_("cols", [128, NCOL], F32)
        self.ncols, self.ncolsr = self.T_("ncols", [128, 8], F32)
        self.gfin, self.gfinr = self.T_("gfin", [128, D], F32)
        self.wgz, self.wgzr = self.T_("wgz", [128, 8, 16], BF16)
        self.wgk, self.wgkr = self.T_("wgk", [16, 512], BF16)
        self.cres = Res("consts")
        self.R1 = K.sb("R1", [128, 16384], BF16)
        self.R1r = [Res(f"R1_{i}") for i in range(32)]
        self.qi, self.qir = self.T_("qi", [128, 4, 512], BF16, 4)
        self.kd, self.kdr = self.T_("kd", [128, 4, 512], BF16, 4)
        self.kdt, self.kdtr = self.T_("kdt", [128, 4, 512], BF16, 4)
        self.vt, self.vtr = self.T_("vt", [128, 4, D], BF16, 4)
        self.ogs, self.ogsr = self.T_("ogs", [128, 4, D], BF16, 4)
        self.ga, self.gar = self.T_("ga", [128, 8, 512], BF16, 8)
        self.gb, self.gbr = self.T_("gb", [128, 8, 512], BF16, 8)
        self.att, self.attr = self.T_("att", [128, 4, 4, 128], BF16, 4)
        self.tmp, self.tmpr = self.T_("tmp", [128, 4, 512], F32, 4)
        self.tp = 0

    def r1_f32(self, i):
        v = self.R1[:, i * 4096:(i + 1) * 4096].bitcast(F32)
        return v.rearrange("p (h t) -> p h t", t=512), self.R1r[i * 8:(i + 1) * 8]

    def r1_bf(self, i):
        v = self.R1[:, i * 4096:(i + 1) * 4096]
        return v.rearrange("p (c t) -> p c t", t=512), self.R1r[i * 8:(i + 1) * 8]

    def bank(self):
        while True:
            b = self.bi
            self.bi = (self.bi + 1) % 8
            if b not in self.held:
                return b

    def hold(self, n):
        bs = []
        for _ in range(n):
            b = self.bank()
            self.held.add(b)
            bs.append(b)
        return bs

    def release(self, bs):
        for b in bs:
            self.held.discard(b)

    def tmpbuf(self):
        i = self.tp
        self.tp = (self.tp + 1) % 4
        return self.tmp[:, i, :], self.tmpr[i]

    def col(self, name, j=0, n=1, P=128):
        o, w = COLS[name]
        return self.cols[:P, o + j:o + j + n]

    def load_consts(self):
        K = self.K
        I = self.I
        cr = self.cres
        K.dma("sp", self.cols[:], I["cols"][:, :], writes=[self.colsr], sem_res=cr)
        K.dma("sp", self.maskT[:], I["c_maskT"][:, :], writes=[self.maskTr], sem_res=cr)
        K.dma("sp", self.smask[:], I["c_smask"][:, :], writes=[self.smaskr], sem_res=cr)
        K.dma("sp", self.rst64[:], I["c_rst64"][:, :], writes=[self.rst64r], sem_res=cr)
        K.dma("sp", self.gfin[:], I["g_fin"].partition_broadcast(128), writes=[self.gfinr], sem_res=cr)
        K.dma("pool", self.ident[:], I["c_ident"][:, :], writes=[self.identr], sem_res=self.identr)
        self.prefetch(0)
        K.dma("pool", self.rst512[:], I["c_rst512"][:, :], writes=[self.rst512r], sem_res=cr)
        K.dma("pool", self.pmat[:], I["c_pmat"][:, :, :, :], writes=[self.pmatr], sem_res=cr)
        K.dma("pool", self.smb[:], I["c_smb"][:, :, :, :], writes=[self.smbr], sem_res=cr)
        K.dma("pool", self.smu[:], I["c_smu"][:, :, :], writes=[self.smur], sem_res=cr)
        K.dma("pool", self.bmq[:], I["c_bmq"][:, :, :], writes=[self.bmqr], sem_res=cr)
        K.dma("pool", self.bmk[:], I["c_bmk"][:, :], writes=[self.bmkr], sem_res=cr)
        K.dma("pool", self.wgk[:], I["w_gk"][:, :], writes=[self.wgkr], sem_res=cr)
        K.dma("pool", self.wgz[:], I["w_in"][:, 2048:2064].rearrange("(kc p) n -> p kc n", p=128),
              writes=[self.wgzr], sem_res=cr)
        for r in (self.colsr, self.maskTr, self.smaskr, self.rst64r, self.gfinr):
            r.last_w = (cr.dsem["hw"][0], cr.dsem["hw"][1], "dma")
        for r in (self.rst512r, self.pmatr, self.smbr, self.smur, self.bmqr, self.bmkr, self.wgkr, self.wgzr):
            r.last_w = (cr.dsem["sw"][0], cr.dsem["sw"][1], "dma")
        o, w = COLS["bgk"]
        K.op("dve", lambda e: e.tensor_scalar(out=self.ncols[:, 0:4], in0=self.cols[:, o:o + 4], scalar1=-1.0,
                                             scalar2=None, op0=ALU.mult), reads=[self.colsr], writes=[self.ncolsr])
        K.op("dve", lambda e: e.memset(self.ncols[:, 4:5], math.log(128.0 ** -0.5)), writes=[self.ncolsr])
        K.op("dve", lambda e: e.memset(self.ncols[:, 5:6], 1.0), writes=[self.ncolsr])
        K.op("dve", lambda e: e.memset(self.ncols[:, 6:7], EPS), writes=[self.ncolsr])
        for h in range(4):
            K.op("dve", lambda e, h=h: e.memset(self.S[:, h, :], 0.0), writes=[self.Sr[h]])
        K.op("dve", lambda e: e.memset(self.ur[:, 4, :], 0.0), writes=[self.urr[4]])

    def plan_units(self):
        I = self.I
        U = []

        def full(w, r0, c0, name, sidx=None, p=0):
            U.append((name, w[r0:r0 + 1024, c0:c0 + 512].rearrange("(kc p) n -> p kc n", p=128), 512, sidx, mode(sidx, p)))

        def mode(sidx, p):
            if sidx is None or not USE_SCRATCH:
                return 0
            tp = sidx % SCR_SPREAD
            return 0 if p < tp else (1 if p == tp else 2)

        def one_pass(first):
            k = 0
            for nm, c0 in (("v0", 1024), ("v1", 1536), ("og0", 2064), ("og1", 2576), ("u0", 3088), ("u1", 3600),
                           ("ga0", 4112), ("ga1", 4624), ("gb0", 5136), ("gb1", 5648), ("k", 512), ("q", 0)):
                full(I["w_in"], 0, c0, nm, k, first); k += 1
            U.append(("pm", I["w_pm"].rearrange("g (kc p) d -> p (g kc) d", p=128), 256, k, mode(k, first))); k += 1
            for nm, w, c0 in (("b0", "w_b", 0), ("a0", "w_a", 0), ("b1", "w_b", 512), ("a1", "w_a", 512),
                              ("o0", "w_o", 0), ("o1", "w_o", 512),
                              ("xq0", "w_xq", 0), ("xq1", "w_xq", 512), ("xo0", "w_xo", 0), ("xo1", "w_xo", 512)):
                full(I[w], 0, c0, nm, k, first); k += 1
            for i in range(8):
                full(I["w_up"], 0, i * 512, f"up{i}", k, first); k += 1
            for n in range(2):
                for j in range(4):
                    full(I["w_dn"], j * 1024, n * 512, f"dn{j}_{n}", k, first); k += 1
            return k

        for nm, w, c0 in (("xk0", "w_xk", 0), ("xk1", "w_xk", 512), ("xv0", "w_xv", 0), ("xv1", "w_xv", 512)):
            full(I[w], 0, c0, nm)
        npass = SEQ // 512 + (1 if self.do_sample else 0)
        for p in range(npass):
            nper = one_pass(p)
        self.units = U
        self.pend_st = []
        self.wi = 0
        self.wl = 0
        self.scr = self.nc.dram_tensor("wscratch", [nper, 128, 8 * 512], BF16, kind="Internal").ap()
        self.scrr = [Res(f"scr{i}") for i in range(nper)]
        self.kvscr = self.nc.dram_tensor("kvscratch", [2, SB, MEM, D], BF16, kind="Internal").ap()
        self.kvscr_res = Res("kvscr")
        self.kv_todo = [(w, b) for w in range(2) for b in range(SB)] if (self.do_sample and USE_KVSCR) else []
        self.kv_tick = 0
        self.kv_active = False

    def W(self, name):
        i = self.wi
        assert self.units[i][0] == name, (self.units[i][0], name)
        self.prefetch(i)
        self.kv_tick += 1
        if self.kv_active and self.kv_todo and self.kv_tick % 2 == 0:
            w, b = self.kv_todo.pop(0)
            src = self.I["ck" if w == 0 else "cv"][b]
            self.K.dma("pool", self.kvscr[w, b], src, writes=[self.kvscr_res])
        self.wi += 1
        s = i % NSLOT
        return self.wt[s], self.wr[s]

    def flush_one_store(self):
        j, s, sidx, nc_ = self.pend_st.pop(0)
        self.K.dma("pool", self.scr[sidx].rearrange("p (kc n) -> p kc n", n=512)[:, :, 0:nc_], self.wt[s][:, :, 0:nc_],
                   reads=[self.wr[s]], writes=[self.scrr[sidx]], sem_res=self.scrr[sidx])

    def prefetch(self, i):
        K = self.K
        lim = min(len(self.units), i + NSLOT - 1)
        while self.wl < lim:
            j = self.wl
            s = j % NSLOT
            nm, src, nc_, sidx, md = self.units[j]
            if md in (0, 1):
                K.dma("pool", self.wt[s][:, :, 0:nc_], src, writes=[self.wr[s]])
                if md == 1:
                    self.pend_st.append((j, s, sidx, nc_))
            else:
                K.dma("pool", self.wt[s][:, :, 0:nc_], self.scr[sidx].rearrange("p (kc n) -> p kc n", n=512)[:, :, 0:nc_],
                      reads=[self.scrr[sidx]], writes=[self.wr[s]])
            self.wl += 1
            while self.pend_st and self.pend_st[0][0] <= j - 2:
                self.flush_one_store()

    def mm(self, out, pairs, reads, bres, start=True, stop=True, signal=True):
        def fn(e, out=out, pairs=pairs, start=start, stop=stop):
            n = len(pairs)
            ins = None
            for i, (l, r) in enumerate(pairs):
                ins = e.matmul(out, l, r, start=(start and i == 0), stop=(stop and i == n - 1))
            return ins
        return self.K.op("pe", fn, reads=reads, writes=[bres], signal=signal)

    def dbg(self, name, ap, res, shape):
        if name not in self.dbg_names or name in self.dbg_out:
            return
        o = self.nc.dram_tensor("dbg_" + name, list(shape), F32, kind="ExternalOutput").ap()
        self.dbg_out[name] = o
        st = self.K.sb("dbgs_" + name, list(shape), F32)
        sr = Res("dbgs_" + name)
        rl = res if isinstance(res, (list, tuple)) else [res]
        self.K.op("dve", lambda e: e.tensor_copy(st[:], ap), reads=rl, writes=[sr])
        self.K.dma("sp", o, st[:], reads=[sr])

    def rstd_from_ss(self, ss_ap, out_ap, inv_n, P, sr=None):
        K = self.K
        sr = self.smallr if sr is None else sr
        K.op("act", lambda e: e.activation(out=out_ap, in_=ss_ap, func=AF.Ln, scale=inv_n, bias=self.ncols[:P, 6:7]),
             reads=[sr, self.ncolsr], writes=[sr])
        K.op("act", lambda e: e.activation(out=out_ap, in_=out_ap, func=AF.Exp, scale=-0.5), reads=[sr], writes=[sr])

    def xnext(self, s):
        g, gr = (self.ga, self.gar) if s < 2 else (self.gb, self.gbr)
        c0 = (s % 2) * 4
        return g[:, c0:c0 + 4, :].rearrange("p c t -> p (c t)").bitcast(F32), gr[c0:c0 + 4]

    def norm_sub(self, s, sp, src=None, srcr=None):
        K = self.K
        sr = self.nsr[s]
        if src is None:
            src, srcr = self.xh[:sp, s, :], [self.xhr[s]]
        K.op("act", lambda e: e.activation(out=self.xn[:sp, s, :], in_=src, func=AF.Square,
                                           accum_out=self.small[:sp, s:s + 1]),
             reads=list(srcr) + [sr], writes=[self.xnr[s], sr])
        self.rstd_from_ss(self.small[:sp, s:s + 1], self.small[:sp, 4 + s:5 + s], 1.0 / D, sp, sr=sr)
        K.op("dve", lambda e: e.tensor_scalar(out=self.xn[:sp, s, :], in0=src,
                                             scalar1=self.small[:sp, 4 + s:5 + s], scalar2=None, op0=ALU.mult),
             reads=list(srcr) + [sr], writes=[self.xnr[s]])

    def trans_sub(self, src, srcr, dst, dstr, s, sp, gname, gmod=8):
        K = self.K
        b = self.bank()
        for c in range(8):
            K.op("pe", lambda e, b=b, c=c: e.transpose(self.pbf[b][:, c * sp:(c + 1) * sp], src[:sp, s, c * 128:(c + 1) * 128],
                                                      self.ident[:sp, :sp]),
                 reads=[srcr[s], self.identr], writes=[self.pbr[b]], signal=(c == 7))
        o, w = COLS[gname]
        if gmod == 8:
            outv = dst[:, :, s * sp:(s + 1) * sp]
            inv = self.pbf[b][:, :8 * sp].rearrange("p (c t) -> p c t", t=sp)
            gv = self.cols[:, o:o + 8].unsqueeze(2).to_broadcast([128, 8, sp])
        else:
            outv = dst[:, :, s * sp:(s + 1) * sp].rearrange("p (a b) t -> p a b t", b=gmod)
            inv = self.pbf[b][:, :8 * sp].rearrange("p (a b t) -> p a b t", b=gmod, t=sp)
            gv = self.cols[:, o:o + gmod].unsqueeze(1).unsqueeze(3).to_broadcast([128, 8 // gmod, gmod, sp])
        K.op("dve", lambda e: e.tensor_tensor(out=outv, in0=inv, in1=gv, op=ALU.mult),
             reads=[self.pbr[b], self.colsr], writes=list(dstr))

    def norm_to_T(self, gname, nsub, sp, T):
        for s in range(nsub):
            self.norm_sub(s, sp)
        for s in range(nsub):
            self.trans_sub(self.xn, self.xnr, self.actT, self.actr, s, sp, gname)

    def to_T(self, src, srcr, dst, dstr, nsub, sp, T, gname, gmod=8):
        K = self.K
        for c in range(8):
            b = self.bank()
            for s in range(nsub):
                K.op("pe", lambda e, b=b, s=s, c=c: e.transpose(self.pbf[b][:, s * sp:(s + 1) * sp],
                                                                src[:sp, s, c * 128:(c + 1) * 128],
                                                                self.ident[:sp, :sp]),
                     reads=[srcr[s], self.identr], writes=[self.pbr[b]], signal=(s == nsub - 1))
            K.op("act", lambda e, b=b, c=c: e.activation(out=dst[:, c, :T], in_=self.pbf[b][:, :T], func=AF.Copy,
                                                         scale=self.col(gname, c % gmod)),
                 reads=[self.pbr[b], self.colsr], writes=[dstr[c]])

    def proj_fm(self, wt, wres, ms, srcT, srcr, T, evac):
        ms = list(ms)
        if T * len(ms) <= 512 and len(ms) == 4:
            b = self.bank()
            for m in ms:
                pairs = [(wt[:, kc, m * 128:(m + 1) * 128], srcT[:, kc, :T]) for kc in range(8)]
                self.mm(self.pb[b][:, m * T:(m + 1) * T], pairs, [wres] + list(srcr), self.pbr[b], signal=(m == ms[-1]))
            evac(None, b)
            return
        if self.pending_trans is not None and T == 512:
            pend, self.pending_trans = self.pending_trans, None
            TA = T - 128
            head = ms[:3]
            hb = []
            for m in head:
                b = self.bank()
                hb.append(b)
                pairs = [(wt[:, kc, m * 128:(m + 1) * 128], srcT[:, kc, :TA]) for kc in range(8)]
                self.mm(self.pb[b][:, :TA], pairs, [wres] + list(srcr), self.pbr[b])
            pend()
            for m, b in zip(head, hb):
                pairs = [(wt[:, kc, m * 128:(m + 1) * 128], srcT[:, kc, TA:T]) for kc in range(8)]
                self.mm(self.pb[b][:, TA:T], pairs, [wres] + list(srcr), self.pbr[b])
                evac(m, b)
            ms = ms[3:]
        for m in ms:
            b = self.bank()
            pairs = [(wt[:, kc, m * 128:(m + 1) * 128], srcT[:, kc, :T]) for kc in range(8)]
            self.mm(self.pb[b][:, :T], pairs, [wres] + list(srcr), self.pbr[b])
            evac(m, b)

    def proj_tm(self, wt, wres, srcT, srcr, nsub, sp, evac):
        for s in range(nsub):
            b = self.bank()
            pairs = [(srcT[:, kc, s * sp:(s + 1) * sp], wt[:, kc, :]) for kc in range(8)]
            self.mm(self.pb[b][:sp, :], pairs, [wres] + list(srcr), self.pbr[b])
            evac(s, b)

    def sigmoid_to(self, b, T, out_ap, out_res, P=128, ncol=None, fold=None):
        K = self.K
        ncol = T if ncol is None else ncol
        t, tr = self.tmpbuf()
        out_res_l = out_res if isinstance(out_res, (list, tuple)) else [out_res]
        if fold is not None:
            tv = t[:P, :ncol].rearrange("p (m t) -> p m t", t=fold)
            K.op("act", lambda e: e.activation(out=t[:P, :ncol], in_=self.pb[b][:P, :ncol], func=AF.Exp, scale=-1.0),
                 reads=[self.pbr[b]], writes=[tr])
            K.op("act", lambda e: e.activation(out=t[:P, :ncol], in_=t[:P, :ncol], func=AF.Ln, bias=self.ncols[:P, 5:6]),
                 reads=[tr, self.ncolsr], writes=[tr])
            K.op("act", lambda e: e.activation(out=out_ap, in_=tv, func=AF.Exp, scale=-1.0),
                 reads=[tr], writes=list(out_res_l))
            return t, tr
        K.op("act", lambda e: e.activation(out=t[:P, :ncol], in_=self.pb[b][:P, :ncol], func=AF.Exp, scale=-1.0),
             reads=[self.pbr[b]], writes=[tr])
        K.op("act", lambda e: e.activation(out=t[:P, :ncol], in_=t[:P, :ncol], func=AF.Ln, bias=self.ncols[:P, 5:6]),
             reads=[tr, self.ncolsr], writes=[tr])
        K.op("act", lambda e: e.activation(out=out_ap, in_=t[:P, :ncol], func=AF.Exp, scale=-1.0),
             reads=[tr], writes=[out_res])
        return t, tr

    def mem_kv(self):
        K = self.K
        I, O = self.I, self.O
        for s in range(2):
            xa, xr = self.xnext(s)
            self.norm_sub(s, 128, src=xa, srcr=xr)
        memT = self.kd[:, :, :].rearrange("p h t -> p (h t)").rearrange("p (c t) -> p c t", t=256)
        memTr = self.kdr
        for s in range(2):
            self.trans_sub(self.xn, self.xnr, memT, memTr, s, 128, "g_mem")
        self.norm_to_T("g_mix", 4, 128, 512)
        self.stage("memkv_norm")
        stg, stgr = self.r1_f32(0)
        stg2, stg2r = self.r1_f32(1)
        for which, (st, str_), outn in (("xk", (stg, stgr), "mk"), ("xv", (stg2, stg2r), "mv")):
            if which == "xv":
                self.stage("memkv_xk")
            for n in range(2):
                wt, wres = self.W(f"{which}{n}")
                for mt in range(2):
                    b = self.bank()
                    pairs = [(memT[:, kc, mt * 128:(mt + 1) * 128], wt[:, kc, :]) for kc in range(8)]
                    self.mm(self.pb[b][:, :], pairs, [wres] + memTr, self.pbr[b])
                    g = mt * 2 + n
                    K.op("act", lambda e, b=b, g=g, st=st: e.activation(out=st[:, g, :], in_=self.pb[b][:, :], func=AF.Copy),
                         reads=[self.pbr[b]], writes=[str_[g * 2], str_[g * 2 + 1]])
                    if which == "xv":
                        K.op("dve", lambda e, b=b, mt=mt, n=n: e.tensor_copy(self.Vp[:, mt, n * 512:(n + 1) * 512], self.pb[b][:, :]),
                             reads=[self.pbr[b]], writes=[self.Vpr])
                    K.dma("sp", O[outn][mt * 128:(mt + 1) * 128, n * 512:(n + 1) * 512], st[:, g, :],
                          reads=[str_[g * 2], str_[g * 2 + 1]])
                if which == "xk":
                    for m in range(4):
                        b = self.bank()
                        pairs = [(wt[:, kc, m * 128:(m + 1) * 128], memT[:, kc, :256]) for kc in range(8)]
                        self.mm(self.pb[b][:, :256], pairs, [wres] + memTr, self.pbr[b])
                        K.op("dve", lambda e, b=b, m=m, n=n: e.tensor_copy(self.KTp[:, n * 4 + m, :], self.pb[b][:, :256]),
                             reads=[self.pbr[b]], writes=[self.KTpr])

    def in_proj(self, nsub, sp, T, uslots, last_u_fp32=None):
        K = self.K
        gzT = self.att[:16, 0, :, :].rearrange("p a b -> p (a b)")
        gzr = self.attr[0]
        b = self.bank()
        pairs = [(self.wgz[:, kc, :], self.actT[:, kc, :T]) for kc in range(8)]
        self.mm(self.pb[b][:16, :T], pairs, [self.wgzr] + self.actr, self.pbr[b])
        K.op("act", lambda e, b=b: e.activation(out=gzT[:, :T], in_=self.pb[b][:16, :T], func=AF.Copy),
             reads=[self.pbr[b]], writes=[gzr])
        T1, T1r = self.r1_f32(0)
        Bc, Bcr = self.r1_f32(1)
        E1, E1r = self.r1_f32(2)
        E2, E2r = self.r1_f32(3)
        CL = 128 if T == 512 else ST
        nch = T // CL

        def decay_chain():
            for h in range(4):
                b = self.bank()
                self.mm(self.pb[b][:, :T], [(self.wgk[:, h * 128:(h + 1) * 128], gzT[:, :T])], [self.wgkr, gzr], self.pbr[b])
                K.op("act", lambda e, b=b, h=h: e.activation(out=T1[:, h, :T], in_=self.pb[b][:, :T], func=AF.Exp,
                                                             scale=-1.0, bias=self.ncols[:, h:h + 1]),
                     reads=[self.pbr[b], self.ncolsr], writes=T1r[2 * h:2 * h + 2])
                K.op("act", lambda e, h=h: e.activation(out=T1[:, h, :T], in_=T1[:, h, :T], func=AF.Ln,
                                                        bias=self.ncols[:, 5:6]),
                     reads=T1r[2 * h:2 * h + 2] + [self.ncolsr], writes=T1r[2 * h:2 * h + 2])
                rst = self.rst512 if T == 512 else self.rst64
                rstr = self.rst512r if T == 512 else self.rst64r
                K.op("dve", lambda e, h=h, rst=rst: e.tensor_tensor_scan(out=Bc[:, h, :T], data0=rst[:, :T], data1=T1[:, h, :T],
                                                                         initial=0.0, op0=ALU.mult, op1=ALU.add),
                     reads=T1r[2 * h:2 * h + 2] + [rstr], writes=Bcr[2 * h:2 * h + 2])
            CL = 128 if T == 512 else ST
            nch = T // CL
            Bv = Bc[:, :, :T].rearrange("p h (c t) -> p h c t", t=CL)
            Dv = T1[:, :, :T].rearrange("p h (c t) -> p h c t", t=CL)
            Bend = Bv[:, :, :, CL - 1:CL]
            K.op("dve", lambda e: e.tensor_tensor(out=Dv, in0=Bv, in1=Bend.to_broadcast([128, 4, nch, CL]), op=ALU.subtract),
                 reads=Bcr, writes=T1r)
            eB = self.eB
            K.op("act", lambda e: e.activation(out=eB[:, :, :nch], in_=Bv[:, :, :, CL - 1], func=AF.Exp, scale=-1.0 / 16),
                 reads=Bcr, writes=[self.eBr])
            K.op("act", lambda e: e.activation(out=E1[:, :, :T], in_=T1[:, :, :T], func=AF.Exp, scale=1.0 / 16),
                 reads=T1r, writes=E1r)
            K.op("act", lambda e: e.activation(out=E2[:, :, :T], in_=T1[:, :, :T], func=AF.Exp, scale=-1.0 / 16,
                                               bias=self.ncols[:, 4:5]),
                 reads=T1r + [self.ncolsr], writes=E2r)
        for n in range(2):
            wt, wres = self.W(f"v{n}")

            def ev(s, b, n=n):
                K.op("act", lambda e: e.activation(out=self.vt[:sp, s, n * 512:(n + 1) * 512], in_=self.pb[b][:sp, :],
                                                   func=AF.Copy), reads=[self.pbr[b]], writes=[self.vtr[s]])
            self.proj_tm(wt, wres, self.actT, self.actr, nsub, sp, ev)
        decay_chain()
        for n in range(2):
            wt, wres = self.W(f"og{n}")

            def ev(s, b, n=n):
                t, tr = self.tmpbuf()
                self.sigmoid_to(b, 512, t[:sp, :], tr, P=sp)
                K.op("dve", lambda e: e.tensor_tensor(out=self.ogs[:sp, s, n * 512:(n + 1) * 512], in0=self.pb[b][:sp, :],
                                                     in1=t[:sp, :], op=ALU.mult),
                     reads=[self.pbr[b], tr], writes=[self.ogsr[s]])
            self.proj_tm(wt, wres, self.actT, self.actr, nsub, sp, ev)
        for n in range(2):
            wt, wres = self.W(f"u{n}")

            def ev(s, b, n=n):
                sl = uslots[s]
                K.op("act", lambda e: e.activation(out=self.ur[:sp, sl, n * 512:(n + 1) * 512], in_=self.pb[b][:sp, :],
                                                   func=AF.Copy), reads=[self.pbr[b]], writes=[self.urr[sl]])
                if last_u_fp32 is not None and s == nsub - 1:
                    st, str_ = last_u_fp32
                    K.op("dve", lambda e: e.tensor_copy(st[:sp, n * 512:(n + 1) * 512], self.pb[b][:sp, :]),
                         reads=[self.pbr[b]], writes=list(str_))
            self.proj_tm(wt, wres, self.actT, self.actr, nsub, sp, ev)
        for gt, gr, nm in ((self.ga, self.gar, "ga"), (self.gb, self.gbr, "gb")):
            for n in range(2):
                wt, wres = self.W(f"{nm}{n}")

                def ev(m, b, n=n, gt=gt, gr=gr):
                    if m is None:
                        self.sigmoid_to(b, T, gt[:, n * 4:n * 4 + 4, :T], gr[n * 4:n * 4 + 4], ncol=4 * T, fold=T)
                        return
                    self.sigmoid_to(b, T, gt[:, n * 4 + m, :T], gr[n * 4 + m])
                self.proj_fm(wt, wres, range(4), self.actT, self.actr, T, ev)
        wt, wres = self.W("k")

        def evk(h, b):
            if h is None:
                K.op("dve", lambda e: e.tensor_tensor(out=self.kd[:, :, :T], in0=self.pb[b][:, :4 * T].rearrange("p (m t) -> p m t", t=T),
                                                     in1=E1[:, :, :T], op=ALU.mult),
                     reads=[self.pbr[b]] + E1r, writes=self.kdr)
                return
            K.op("dve", lambda e: e.tensor_tensor(out=self.kd[:, h, :T], in0=self.pb[b][:, :T], in1=E1[:, h, :T], op=ALU.mult),
                 reads=[self.pbr[b]] + E1r[2 * h:2 * h + 2], writes=[self.kdr[h]])
        self.proj_fm(wt, wres, range(4), self.actT, self.actr, T, evk)
        wt, wres = self.W("q")

        def evq(h, b):
            if h is None:
                K.op("dve", lambda e: e.tensor_tensor(out=self.qi[:, :, :T], in0=self.pb[b][:, :4 * T].rearrange("p (m t) -> p m t", t=T),
                                                     in1=E2[:, :, :T], op=ALU.mult),
                     reads=[self.pbr[b]] + E2r, writes=self.qir)
                return
            K.op("dve", lambda e: e.tensor_tensor(out=self.qi[:, h, :T], in0=self.pb[b][:, :T], in1=E2[:, h, :T], op=ALU.mult),
                 reads=[self.pbr[b]] + E2r[2 * h:2 * h + 2], writes=[self.qir[h]])
        self.proj_fm(wt, wres, range(4), self.actT, self.actr, T, evq)
        for c in range(nch if T == 512 else 1):
            cl = 128 if T == 512 else 64
            b = self.bank()
            for h in range(4):
                K.op("pe", lambda e, b=b, h=h, c=c, cl=cl: e.transpose(self.pbf[b][:cl, h * 128:(h + 1) * 128],
                                                                      self.kd[:, h, c * cl:(c + 1) * cl], self.ident[:, :]),
                     reads=[self.kdr[h], self.identr], writes=[self.pbr[b]], signal=(h == 3))
            K.op("act", lambda e, b=b, c=c, cl=cl: e.activation(out=self.kdt[:cl, c, :], in_=self.pbf[b][:cl, :512], func=AF.Copy),
                 reads=[self.pbr[b]], writes=[self.kdtr[c]])

    def branches_out(self, o2T, o2Tr, pooledT, pooledTr, nsub, sp, T):
        K = self.K
        mT, mTr = self.r1_bf(2)
        wts = {}
        for m in range(8):
            if m % 4 == 0:
                wts["b"] = self.W(f"b{m // 4}")
                wts["a"] = self.W(f"a{m // 4}")
            bb = self.bank()
            wt, wres = wts["b"]
            self.mm(self.pb[bb][:, :T], [(wt[:, kc, (m % 4) * 128:(m % 4 + 1) * 128], pooledT[:, kc, :T]) for kc in range(8)],
                    [wres] + list(pooledTr), self.pbr[bb])
            ba = self.bank()
            wt, wres = wts["a"]
            self.mm(self.pb[ba][:, :T], [(wt[:, kc, (m % 4) * 128:(m % 4 + 1) * 128], o2T[:, kc, :T]) for kc in range(8)],
                    [wres] + list(o2Tr), self.pbr[ba])
            t1, t1r = self.tmpbuf()
            t2, t2r = self.tmpbuf()
            K.op("dve", lambda e, bb=bb, m=m, t1=t1: e.tensor_tensor(out=t1[:, :T], in0=self.pb[bb][:, :T], in1=self.gb[:, m, :T], op=ALU.mult),
                 reads=[self.pbr[bb], self.gbr[m]], writes=[t1r])
            K.op("dve", lambda e, ba=ba, m=m, t2=t2: e.tensor_tensor(out=t2[:, :T], in0=self.pb[ba][:, :T], in1=self.ga[:, m, :T], op=ALU.mult),
                 reads=[self.pbr[ba], self.gar[m]], writes=[t2r])
            K.op("dve", lambda e, m=m, t1=t1, t2=t2: e.tensor_tensor(out=mT[:, m, :T], in0=t1[:, :T], in1=t2[:, :T], op=ALU.add),
                 reads=[t1r, t2r], writes=[mTr[m]])
        self.resid_norm("o", mT, mTr, nsub, sp, "g_x")

    def pool_mix(self, dT, dTr, pT, pTr, T):
        K = self.K
        wt, wres = self.W("pm")
        for g in range(4):
            for dch in range(2):
                b = self.bank()
                pairs = [(wt[:, g * 2 + kc, dch * 128:(dch + 1) * 128], dT[:, 2 * g + kc, :T]) for kc in range(2)]
                self.mm(self.pb[b][:, :T], pairs, [wres, dTr[2 * g], dTr[2 * g + 1]], self.pbr[b])
                K.op("act", lambda e, b=b, g=g, dch=dch: e.activation(out=pT[:, 2 * g + dch, :T], in_=self.pb[b][:, :T], func=AF.Copy,
                                                                    scale=self.col("pscale", 2 * g + dch)),
                     reads=[self.pbr[b], self.colsr], writes=[pTr[2 * g + dch]])

    def resid_norm(self, wname, srcT, srcr, nsub, sp, gname):
        K = self.K
        ws = [self.W(f"{wname}0"), self.W(f"{wname}1")]
        for s in range(nsub):
            for n in range(2):
                wt, wres = ws[n]
                b = self.bank()
                pairs = [(srcT[:, kc, s * sp:(s + 1) * sp], wt[:, kc, :]) for kc in range(8)]
                self.mm(self.pb[b][:sp, :], pairs, [wres] + list(srcr), self.pbr[b])
                K.op("dve", lambda e, s=s, n=n, b=b: e.tensor_tensor(out=self.xh[:sp, s, n * 512:(n + 1) * 512],
                                                                    in0=self.xh[:sp, s, n * 512:(n + 1) * 512],
                                                                    in1=self.pb[b][:sp, :], op=ALU.add),
                     reads=[self.pbr[b], self.xhr[s]], writes=[self.xhr[s]])
            if s >= 1:
                self.trans_sub(self.xn, self.xnr, self.actT, self.actr, s - 1, sp, gname)
            self.norm_sub(s, sp)
        if nsub == 4 and sp == 128 and HIDE_TAIL:
            self.pending_trans = lambda: self.trans_sub(self.xn, self.xnr, self.actT, self.actr, nsub - 1, sp, gname)
        else:
            self.trans_sub(self.xn, self.xnr, self.actT, self.actr, nsub - 1, sp, gname)

    def out_resid(self, wname, srcT, srcr, nsub, sp):
        K = self.K
        for n in range(2):
            wt, wres = self.W(f"{wname}{n}")

            def ev(s, b, n=n):
                K.op("dve", lambda e: e.tensor_tensor(out=self.xh[:sp, s, n * 512:(n + 1) * 512],
                                                     in0=self.xh[:sp, s, n * 512:(n + 1) * 512],
                                                     in1=self.pb[b][:sp, :], op=ALU.add),
                     reads=[self.pbr[b], self.xhr[s]], writes=[self.xhr[s]])
            self.proj_tm(wt, wres, srcT, srcr, nsub, sp, ev)

    def softmax_stages(self, hb, P, Pn, Pnr, par=0, c_eng="dve"):
        K = self.K
        sm = self.sm2r[par]
        cb = (8, 64, 80)[par]
        mx = self.small[:P, cb:cb + 4]
        nmx = self.small[:P, cb + 4:cb + 8]
        ssum = self.small[:P, cb + 8:cb + 12]
        rs = self.small[:P, cb + 12:cb + 16]
        bufs = {}

        def stage_a():
            h = 0
            while h < 4:
                b, co = hb[h]
                if h + 1 < 4 and hb[h + 1] == (b, co + 256):
                    K.op("dve", lambda e, b=b, h=h, co=co: e.reduce_max(
                        out=self.small[:P, cb + 4 + h:cb + 4 + h + 2], in_=self.pb[b][:P, co:co + 512].rearrange("p (h m) -> p h m", m=256),
                        axis=AX.X, negate=True),
                         reads=[self.pbr[b]], writes=[sm])
                    h += 2
                else:
                    K.op("dve", lambda e, b=b, h=h, co=co: e.reduce_max(out=self.small[:P, cb + 4 + h:cb + 4 + h + 1], in_=self.pb[b][:P, co:co + 256],
                                                                        axis=AX.X, negate=True),
                         reads=[self.pbr[b]], writes=[sm])
                    h += 1

        def stage_b():
            bufs["pf"] = [self.tmpbuf(), self.tmpbuf()]
            for h, (b, co) in enumerate(hb):
                pf, pfr = bufs["pf"][h // 2]
                K.op("act", lambda e, b=b, h=h, pf=pf, co=co: e.activation(out=pf[:P, (h % 2) * 256:(h % 2 + 1) * 256],
                                                                           in_=self.pb[b][:P, co:co + 256],
                                                                           func=AF.Exp, scale=1.0, bias=self.small[:P, cb + 4 + h:cb + 5 + h],
                                                                           accum_out=self.small[:P, cb + 8 + h:cb + 9 + h]),
                     reads=[self.pbr[b], sm], writes=[pfr, sm])

        def stage_c():
            K.op("dve", lambda e: e.reciprocal(out=rs, in_=ssum), reads=[sm], writes=[sm])
            for hp, (pf, pfr) in enumerate(bufs["pf"]):
                K.op(c_eng, lambda e, hp=hp, pf=pf: e.tensor_tensor(
                    out=Pn[:P, 2 * hp:2 * hp + 2, :], in0=pf[:P, :].rearrange("p (h m) -> p h m", m=256),
                    in1=self.small[:P, cb + 12 + 2 * hp:cb + 14 + 2 * hp].unsqueeze(2).to_broadcast([P, 2, 256]), op=ALU.mult),
                     reads=[pfr, sm], writes=list(Pnr))
        return stage_a, stage_b, stage_c

    def softmax_rows(self, hb, P, Pn, Pnr, par=0):
        fa, fb, fc = self.softmax_stages(hb, P, Pn, Pnr, par)
        fa()
        fb()
        fc()

    def mlp_final(self, nsub, sp, T, ydst, yres_fn, pro_a=None, pro_b=None):
        K = self.K
        hid = self.R1[:, :].rearrange("p (c t) -> p c t", t=512)
        hidr = self.R1r
        for i in range(8):
            wt, wres = self.W(f"up{i}")

            def ev(m, b, i=i):
                t, tr = self.tmpbuf()
                if m is None:
                    K.op("act", lambda e: e.activation(out=t[:, :4 * T], in_=self.pb[b][:, :4 * T], func=AF.Relu),
                         reads=[self.pbr[b]], writes=[tr])
                    K.op("dve", lambda e: e.scalar_tensor_tensor(out=hid[:, 4 * i:4 * i + 4, :T],
                                                                in0=self.pb[b][:, :4 * T].rearrange("p (m t) -> p m t", t=T), scalar=0.0,
                                                                in1=t[:, :4 * T].rearrange("p (m t) -> p m t", t=T), op0=ALU.max, op1=ALU.mult),
                         reads=[self.pbr[b], tr], writes=hidr[4 * i:4 * i + 4])
                    return
                K.op("act", lambda e: e.activation(out=t[:, :T], in_=self.pb[b][:, :T], func=AF.Relu),
                     reads=[self.pbr[b]], writes=[tr])
                K.op("dve", lambda e: e.scalar_tensor_tensor(out=hid[:, 4 * i + m, :T], in0=self.pb[b][:, :T], scalar=0.0,
                                                            in1=t[:, :T], op0=ALU.max, op1=ALU.mult),
                     reads=[self.pbr[b], tr], writes=[hidr[4 * i + m]])
            self.proj_fm(wt, wres, range(4), self.actT, self.actr, T, ev)
        if pro_a is not None:
            pro_a()
        for n in range(2):
            bs = [self.bank() for _ in range(nsub)]
            for j in range(4):
                wt, wres = self.W(f"dn{j}_{n}")
                for s in range(nsub):
                    pairs = [(hid[:, 8 * j + kc, s * sp:(s + 1) * sp], wt[:, kc, :]) for kc in range(8)]
                    self.mm(self.pb[bs[s]][:sp, :], pairs, [wres] + hidr[8 * j:8 * j + 8], self.pbr[bs[s]],
                            start=(j == 0), stop=(j == 3))
            for s in range(nsub):
                b = bs[s]
                K.op("dve", lambda e, s=s, b=b, n=n: e.tensor_tensor(out=self.xh[:sp, s, n * 512:(n + 1) * 512],
                                                                    in0=self.xh[:sp, s, n * 512:(n + 1) * 512],
                                                                    in1=self.pb[b][:sp, :], op=ALU.add),
                     reads=[self.pbr[b], self.xhr[s]], writes=[self.xhr[s]])
            if n == 0 and pro_b is not None:
                pro_b()
        yst = self.R1[:, 0:8192].bitcast(F32).rearrange("p (s d) -> p s d", d=D)
        ss = self.small[:sp, 56:56 + nsub]
        rs = self.small[:sp, 60:60 + nsub]
        for s in range(nsub):
            K.op("act", lambda e, s=s: e.activation(out=self.junk2[:sp, :], in_=self.xh[:sp, s, :], func=AF.Square,
                                                    accum_out=self.small[:sp, 56 + s:57 + s]),
                 reads=[self.xhr[s], self.smallr], writes=[self.junk2r, self.smallr])
        self.rstd_from_ss(ss, rs, 1.0 / D, sp)
        for s in range(nsub):
            K.op("dve", lambda e, s=s: e.scalar_tensor_tensor(out=yst[:sp, s, :], in0=self.xh[:sp, s, :],
                                                             scalar=self.small[:sp, 60 + s:61 + s], in1=self.gfin[:sp, :],
                                                             op0=ALU.mult, op1=ALU.mult),
                 reads=[self.xhr[s], self.smallr, self.gfinr], writes=self.R1r[4 * s:4 * s + 4])
            K.dma("sp", yres_fn(s), yst[:sp, s, :], reads=self.R1r[4 * s:4 * s + 4])

    def prompt_tile(self, ti):
        K = self.K
        I, O = self.I, self.O
        T, nsub, sp = 512, 4, 128
        if ti == 0:
            pass
        else:
            for s in range(nsub):
                xa, xr = self.xnext(s)
                K.op("dve", lambda e, s=s, xa=xa: e.tensor_copy(self.xh[:, s, :], xa), reads=list(xr), writes=[self.xhr[s]])
        self.dbg("xnT", self.actT[:, :, :], self.actr, [128, 8, 512])
        uslots = [(ti * 4 + s) % 5 for s in range(4)]
        last = None
        if ti == SEQ // 512 - 1:
            last = self.r1_dummy_ufp()
        self.in_proj(nsub, sp, T, uslots, last_u_fp32=last)
        self.dbg("kd", self.kd[:, :, :], self.kdr, [128, 4, 512])
        self.dbg("qi", self.qi[:, :, :], self.qir, [128, 4, 512])
        if last is not None:
            K.dma("sp", O["spp"][:, :], last[0][113:128, :], reads=list(last[1]))
        o2 = self.xn
        o2r = self.xnr
        for c in range(4):
            cs = slice(c * 128, (c + 1) * 128)
            b = self.bank()
            for h in range(4):
                self.mm(self.pb[b][:, h * 128:(h + 1) * 128], [(self.kd[:, h, cs], self.qi[:, h, cs])],
                        [self.kdr[h], self.qir[h]], self.pbr[b], signal=(h == 3))
            K.op("dve", lambda e, b=b, c=c: e.tensor_tensor(
                out=self.att[:, c, :, :], in0=self.pb[b][:, :].rearrange("p (h t) -> p h t", t=128),
                in1=self.maskT[:, :].unsqueeze(1).to_broadcast([128, 4, 128]), op=ALU.mult),
                 reads=[self.pbr[b], self.maskTr], writes=[self.attr[c]])
            for hp in range(2):
                b2 = self.bank()
                for h in (2 * hp, 2 * hp + 1):
                    self.mm(self.pb[b2][:, (h % 2) * 256:(h % 2 + 1) * 256],
                            [(self.kdt[:, c, h * 128:(h + 1) * 128], self.vt[:, c, h * 256:(h + 1) * 256])],
                            [self.kdtr[c], self.vtr[c]], self.pbr[b2], signal=(h % 2 == 1))
                for h in (2 * hp, 2 * hp + 1):
                    K.op("act", lambda e, h=h, c=c: e.activation(out=self.Sb[:, c, h, :], in_=self.S[:, h, :], func=AF.Copy,
                                                                 scale=self.eB[:, h, c:c + 1]),
                         reads=[self.Sr[h], self.eBr], writes=[self.Sbr[c * 4 + h]])
                    K.op("dve", lambda e, h=h, b2=b2, c=c: e.scalar_tensor_tensor(
                        out=self.S[:, h, :], in0=self.S[:, h, :], scalar=self.eB[:, h, c:c + 1],
                        in1=self.pb[b2][:, (h % 2) * 256:(h % 2 + 1) * 256], op0=ALU.mult, op1=ALU.add),
                         reads=[self.Sr[h], self.eBr, self.pbr[b2]], writes=[self.Sr[h]])
        dT, dTr = self.r1_bf(0)
        pT, pTr = self.r1_bf(1)

        def pool_cc(cc):
            g = cc // 2
            b = self.bank()
            for s in range(4):
                cur = uslots[s]
                prev = (cur + 4) % 5
                csl = slice(cc * 128, (cc + 1) * 128)
                if ti == 0 and s == 0:
                    pairs = [(self.ur[:, cur, csl], self.pmat[:, 2, g, :]), (self.ur[:, cur, csl], self.pmat[:, 3, g, :])]
                    rd = [self.urr[cur], self.pmatr]
                else:
                    pairs = [(self.ur[:, prev, csl], self.pmat[:, 1, g, :]), (self.ur[:, cur, csl], self.pmat[:, 0, g, :])]
                    rd = [self.urr[cur], self.urr[prev], self.pmatr]
                self.mm(self.pb[b][:, s * 128:(s + 1) * 128], pairs, rd, self.pbr[b], signal=(s == 3))
            K.op("act", lambda e, b=b, cc=cc: e.activation(out=dT[:, cc, :], in_=self.pb[b][:, :], func=AF.Copy),
                 reads=[self.pbr[b]], writes=[dTr[cc]])

        for cc in range(4):
            pool_cc(cc)
        for c in range(4):
            cs = slice(c * 128, (c + 1) * 128)
            sr = self.osr[c]
            obanks = []
            for hp in range(2):
                b3 = self.bank()
                obanks.append(b3)
                for h in (2 * hp, 2 * hp + 1):
                    self.mm(self.pb[b3][:, (h % 2) * 256:(h % 2 + 1) * 256],
                            [(self.att[:, c, h, :], self.vt[:, c, h * 256:(h + 1) * 256]),
                             (self.qi[:, h, cs], self.Sb[:, c, h, :])],
                            [self.attr[c], self.vtr[c], self.qir[h], self.Sbr[c * 4 + h]], self.pbr[b3], signal=(h % 2 == 1))
                for h in (2 * hp, 2 * hp + 1):
                    K.op("act", lambda e, h=h, b3=b3, c=c: e.activation(out=self.junk[:, :256], in_=self.pb[b3][:, (h % 2) * 256:(h % 2 + 1) * 256],
                                                                        func=AF.Square, accum_out=self.small[:, 24 + c * 4 + h:25 + c * 4 + h]),
                         reads=[self.pbr[b3], sr], writes=[self.junkr, sr])
            self.rstd_from_ss(self.small[:, 24 + c * 4:28 + c * 4], self.small[:, 40 + c * 4:44 + c * 4], 1.0 / 256, 128, sr=sr)
            for h in range(4):
                b3 = obanks[h // 2]
                K.op("dve", lambda e, h=h, b3=b3, c=c: e.scalar_tensor_tensor(
                    out=o2[:, c, h * 256:(h + 1) * 256], in0=self.pb[b3][:, (h % 2) * 256:(h % 2 + 1) * 256],
                    scalar=self.small[:, 40 + c * 4 + h:41 + c * 4 + h], in1=self.ogs[:, c, h * 256:(h + 1) * 256],
                    op0=ALU.mult, op1=ALU.mult),
                     reads=[self.pbr[b3], sr, self.ogsr[c]], writes=[o2r[c]])
        for cc in range(4, 8):
            pool_cc(cc)
        self.pool_mix(dT, dTr, pT, pTr, T)
        for c in range(4):
            self.trans_sub(o2, o2r, self.actT, self.actr, c, sp, "g_gla", gmod=2)
        self.branches_out(self.actT, self.actr, pT, pTr, nsub, sp, T)
        self.dbg("h1", self.xh[:, :, :], self.xhr, [128, 4, 1024])
        qx, qxr = self.r1_bf(0)
        PT, PTr = self.r1_bf(1)
        ox, oxr = self.r1_bf(3)
        for n in range(2):
            wt, wres = self.W(f"xq{n}")

            def ev(m, b, n=n):
                K.op("act", lambda e: e.activation(out=qx[:, n * 4 + m, :], in_=self.pb[b][:, :], func=AF.Copy, scale=1.0 / 16),
                     reads=[self.pbr[b]], writes=[qxr[n * 4 + m]])
            self.proj_fm(wt, wres, range(4), self.actT, self.actr, T, ev)
        Pns = [self.att[:, 2 * par:2 * par + 2, :, :].rearrange("p a h t -> p (a h t)").rearrange("p (h m) -> p h m", m=256)
               for par in range(2)]
        Pnrs = [[self.attr[0], self.attr[1]], [self.attr[2], self.attr[3]]]

        def scores(s):
            ss_ = slice(s * 128, (s + 1) * 128)
            banks = []
            for hp in range(2):
                b = self.bank()
                banks.append(b)
                for h in (2 * hp, 2 * hp + 1):
                    pairs = [(qx[:, 2 * h + dc, ss_], self.KTp[:, 2 * h + dc, :]) for dc in range(2)]
                    self.mm(self.pb[b][:, (h % 2) * 256:(h % 2 + 1) * 256], pairs,
                            [qxr[2 * h], qxr[2 * h + 1], self.KTpr], self.pbr[b], signal=(h % 2 == 1))
            return self.softmax_stages([(banks[h // 2], (h % 2) * 256) for h in range(4)], 128, Pns[s % 2], Pnrs[s % 2], par=s % 3,
                                       c_eng=POOL_SOFTMAX)

        def ptrans(s):
            ss_ = slice(s * 128, (s + 1) * 128)
            Pn, Pnr = Pns[s % 2], Pnrs[s % 2]
            b = self.bank()
            for h in range(4):
                for mt in range(2):
                    j = h * 2 + mt
                    K.op("pe", lambda e, b=b, h=h, mt=mt, j=j: e.transpose(self.pbf[b][:, j * 128:(j + 1) * 128],
                                                                          Pn[:, h, mt * 128:(mt + 1) * 128], self.ident[:, :]),
                         reads=list(Pnr) + [self.identr], writes=[self.pbr[b]], signal=(j == 7))
            K.op("act", lambda e, b=b, ss_=ss_: e.activation(out=PT[:, :, ss_], in_=self.pbf[b][:, :].rearrange("p (j t) -> p j t", t=128),
                                                             func=AF.Copy),
                 reads=[self.pbr[b]], writes=PTr)

        st = {}
        st[0] = scores(0); st[0][0](); st[0][1]()
        st[1] = scores(1); st[1][0]()
        st[2] = scores(2); st[2][0]()
        st[0][2](); st[1][1]()
        ptrans(0)
        st[3] = scores(3); st[3][0]()
        st[1][2](); st[2][1]()
        ptrans(1)
        st[2][2](); st[3][1]()
        ptrans(2)
        st[3][2]()
        ptrans(3)
        for h in range(4):
            for dc in range(2):
                b = self.bank()
                pairs = [(self.Vp[:, mt, h * 256 + dc * 128:h * 256 + (dc + 1) * 128], PT[:, h * 2 + mt, :]) for mt in range(2)]
                self.mm(self.pb[b][:, :], pairs, [self.Vpr, PTr[h * 2], PTr[h * 2 + 1]], self.pbr[b])
                K.op("act", lambda e, b=b, h=h, dc=dc: e.activation(out=ox[:, 2 * h + dc, :], in_=self.pb[b][:, :], func=AF.Copy),
                     reads=[self.pbr[b]], writes=[oxr[2 * h + dc]])
        self.resid_norm("xo", ox, oxr, nsub, sp, "g_mlp")
        self.dbg("h2", self.xh[:, :, :], self.xhr, [128, 4, 1024])
        pro_a = pro_b = None
        if ti + 1 == SEQ // 512 and self.do_sample:
            def pro_a():
                xa, xr = self.xnext(0)
                K.dma("sp", xa[:SB * ST, :], I["xs"][:, :], writes=xr)
                self.norm_sub(0, SB * ST, src=xa[:SB * ST, :], srcr=xr)

            def pro_b():
                self.trans_sub(self.xn, self.xnr, self.actT, self.actr, 0, SB * ST, "g_mix")
        if ti + 1 < SEQ // 512:
            def pro_a():
                for s in range(nsub):
                    xa, xr = self.xnext(s)
                    r0 = (ti + 1) * 512 + s * 128
                    K.dma("sp", xa, I["xp"][r0:r0 + 128, :], writes=xr)
                for s in range(nsub):
                    xa, xr = self.xnext(s)
                    self.norm_sub(s, sp, src=xa, srcr=xr)

            def pro_b():
                for s in range(nsub):
                    self.trans_sub(self.xn, self.xnr, self.actT, self.actr, s, sp, "g_mix")
        self.mlp_final(nsub, sp, T, None, lambda s: O["yp"][ti * 512 + s * 128:ti * 512 + (s + 1) * 128, :],
                       pro_a=pro_a, pro_b=pro_b)

    def r1_dummy_ufp(self):
        return self.ufp, [self.xnr[0], self.xnr[1]]

    def prompt_finish(self):
        K = self.K
        O = self.O
        for h in range(4):
            K.dma("sp", O["sgp"][h, :, :], self.S[:, h, :], reads=[self.Sr[h]])

    def sample_pass(self):
        K = self.K
        I, O = self.I, self.O
        T, nsub, sp = 64, 1, 64
        NB = SB
        xa, xr = self.xnext(0)
        K.op("dve", lambda e: e.tensor_copy(self.xh[:sp, 0, :], xa[:sp, :]), reads=list(xr), writes=[self.xhr[0]])
        ufp, ufpr = self.r1_dummy_ufp()
        self.in_proj(nsub, sp, T, [0], last_u_fp32=(ufp, ufpr))
        qblk = self.Sb[:, :, :, :].rearrange("p a b c -> p (a b c)").rearrange("p (h b t) -> p h b t", h=4, b=NB)
        qblkr = self.Sbr
        K.op("dve", lambda e: e.tensor_tensor(out=qblk, in0=self.qi[:, :, :T].unsqueeze(2).to_broadcast([128, 4, NB, T]),
                                             in1=self.bmq[:, :, :].unsqueeze(1).to_broadcast([128, 4, NB, T]), op=ALU.mult),
             reads=self.qir + [self.bmqr], writes=qblkr)
        kblk = self.R1[:sp, 0:8192].rearrange("p (b d) -> p b d", d=512)
        kblkr = self.R1r[0:16]
        K.op("dve", lambda e: e.tensor_tensor(out=kblk, in0=self.kdt[:sp, 0, :].unsqueeze(1).to_broadcast([sp, NB, 512]),
                                             in1=self.bmk[:sp, :].unsqueeze(2).to_broadcast([sp, NB, 512]), op=ALU.mult),
             reads=[self.kdtr[0], self.bmkr], writes=kblkr)
        b0 = self.bank()
        for h in range(4):
            self.mm(self.pb[b0][:sp, h * 64:(h + 1) * 64], [(self.kd[:, h, :T], self.qi[:, h, :T])],
                    [self.kdr[h], self.qir[h]], self.pbr[b0], signal=(h == 3))
        K.op("dve", lambda e: e.tensor_tensor(out=self.att[:sp, 0, :, :64],
                                             in0=self.pb[b0][:sp, :256].rearrange("p (h t) -> p h t", t=64),
                                             in1=self.smask[:, :].unsqueeze(1).to_broadcast([sp, 4, 64]), op=ALU.mult),
             reads=[self.pbr[b0], self.smaskr], writes=[self.attr[0]])
        ob = self.hold(4)
        for h in range(4):
            self.mm(self.pb[ob[h]][:sp, :256], [(self.att[:sp, 0, h, :64], self.vt[:sp, 0, h * 256:(h + 1) * 256])],
                    [self.attr[0], self.vtr[0]], self.pbr[ob[h]], start=True, stop=False)
        s0in = [self.xh[:, 1, :].rearrange("p (h v) -> p h v", v=256), self.xh[:, 2, :].rearrange("p (h v) -> p h v", v=256),
                self.Vp[:, :, :].rearrange("p a b -> p (a b)").bitcast(F32).rearrange("p (h v) -> p h v", v=256),
                self.S[:, :, :]]
        s0inr = [self.xhr[1], self.xhr[2], self.Vpr, self.Sr]
        NS0 = 4
        s0p = [self.vt[:, 1, :].rearrange("p (h v) -> p h v", v=256), self.vt[:, 2, :].rearrange("p (h v) -> p h v", v=256)]
        s0pr = [self.vtr[1], self.vtr[2]]
        snew = [self.xh[:, 3, :].rearrange("p (h v) -> p h v", v=256),
                self.KTp[:, :, :].rearrange("p a b -> p (a b)").bitcast(F32).rearrange("p (h v) -> p h v", v=256)]
        snewr = [self.xhr[3], self.KTpr]
        def s0w(i):
            r = s0inr[i % NS0]
            return list(r) if isinstance(r, (list, tuple)) else [r]

        for b in range(NS0 - 1):
            K.dma("sp", s0in[b], I["sgla"][b].rearrange("h k v -> k h v"), writes=s0w(b))
        cpr = Res("spscopy")
        K.dma("sp", O["sps"][:, 0:11, :], I["spool"][:, 4:15, :], writes=[cpr])
        for b in range(NB):
            K.dma("sp", O["sps"][b, 11:15, :], ufp[b * ST:(b + 1) * ST, :], reads=list(ufpr))
        for b in range(NB):
            bb = b % 2
            if b + NS0 - 1 < NB:
                K.dma("sp", s0in[(b + NS0 - 1) % NS0], I["sgla"][b + NS0 - 1].rearrange("h k v -> k h v"), writes=s0w(b + NS0 - 1))
            for h in range(4):
                K.op("act", lambda e, h=h, b=b, bb=bb: e.activation(out=s0p[bb][:, h, :], in_=s0in[b % NS0][:, h, :], func=AF.Copy,
                                                                    scale=self.eB[:, h, b:b + 1]),
                     reads=s0w(b) + [self.eBr], writes=[s0pr[bb]])
            for h in range(4):
                self.mm(self.pb[ob[h]][:sp, :256], [(qblk[:, h, b, :], s0p[bb][:, h, :])], qblkr + [s0pr[bb]], self.pbr[ob[h]],
                        start=False, stop=(b == NB - 1))
            for hp in range(2):
                b2 = self.bank()
                for h in (2 * hp, 2 * hp + 1):
                    self.mm(self.pb[b2][:, (h % 2) * 256:(h % 2 + 1) * 256],
                            [(kblk[:, b, h * 128:(h + 1) * 128], self.vt[:sp, 0, h * 256:(h + 1) * 256])],
                            kblkr + [self.vtr[0]], self.pbr[b2], signal=(h % 2 == 1))
                for h in (2 * hp, 2 * hp + 1):
                    K.op("dve", lambda e, h=h, b=b, bb=bb, b2=b2: e.scalar_tensor_tensor(
                        out=snew[bb][:, h, :], in0=s0in[b % NS0][:, h, :], scalar=self.eB[:, h, b:b + 1],
                        in1=self.pb[b2][:, (h % 2) * 256:(h % 2 + 1) * 256], op0=ALU.mult, op1=ALU.add),
                         reads=s0w(b) + [self.eBr, self.pbr[b2]], writes=[snewr[bb]])
            K.dma("pool", O["sgs"][b].rearrange("h k v -> k h v"), snew[bb], reads=[snewr[bb]])
        o2, o2r = self.xn, self.xnr
        for h in range(4):
            K.op("act", lambda e, h=h: e.activation(out=self.junk[:sp, :256], in_=self.pb[ob[h]][:sp, :256], func=AF.Square,
                                                    accum_out=self.small[:sp, 24 + h:25 + h]),
                 reads=[self.pbr[ob[h]], self.smallr], writes=[self.junkr, self.smallr])
        self.rstd_from_ss(self.small[:sp, 24:28], self.small[:sp, 40:44], 1.0 / 256, sp)
        for h in range(4):
            K.op("dve", lambda e, h=h: e.scalar_tensor_tensor(
                out=o2[:sp, 0, h * 256:(h + 1) * 256], in0=self.pb[ob[h]][:sp, :256], scalar=self.small[:sp, 40 + h:41 + h],
                in1=self.ogs[:sp, 0, h * 256:(h + 1) * 256], op0=ALU.mult, op1=ALU.mult),
                 reads=[self.pbr[ob[h]], self.smallr, self.ogsr[0]], writes=[o2r[0]])
        self.release(ob)
        self.trans_sub(o2, o2r, self.actT, self.actr, 0, sp, "g_gla", gmod=2)
        for kt in range(2):
            K.dma("pool", self.ur[:120, 1 + kt, :], I["spool"][kt * 8:(kt + 1) * 8].rearrange("b j d -> (b j) d"),
                  writes=[self.urr[1 + kt]])
        dT, dTr = self.r1_bf(0)
        pT, pTr = self.r1_bf(1)
        for cc in range(8):
            g = cc // 2
            csl = slice(cc * 128, (cc + 1) * 128)
            b = self.bank()
            pairs = [(self.ur[:120, 1, csl], self.smb[:, 0, g, :]), (self.ur[:120, 2, csl], self.smb[:, 1, g, :]),
                     (self.ur[:sp, 0, csl], self.smu[:, g, :])]
            self.mm(self.pb[b][:, :T], pairs, [self.urr[0], self.urr[1], self.urr[2], self.smbr, self.smur], self.pbr[b])
            K.op("act", lambda e, b=b, cc=cc: e.activation(out=dT[:, cc, :T], in_=self.pb[b][:, :T], func=AF.Copy),
                 reads=[self.pbr[b]], writes=[dTr[cc]])
        self.pool_mix(dT, dTr, pT, pTr, T)
        self.branches_out(self.actT, self.actr, pT, pTr, nsub, sp, T)
        qx, qxr = self.r1_bf(0)
        for n in range(2):
            wt, wres = self.W(f"xq{n}")

            def ev(m, b, n=n):
                if m is None:
                    K.op("act", lambda e: e.activation(out=qx[:, n * 4:n * 4 + 4, :T],
                                                       in_=self.pb[b][:, :4 * T].rearrange("p (m t) -> p m t", t=T), func=AF.Copy, scale=1.0 / 16),
                         reads=[self.pbr[b]], writes=qxr[n * 4:n * 4 + 4])
                    return
                K.op("act", lambda e: e.activation(out=qx[:, n * 4 + m, :T], in_=self.pb[b][:, :T], func=AF.Copy, scale=1.0 / 16),
                     reads=[self.pbr[b]], writes=[qxr[n * 4 + m]])
            self.proj_fm(wt, wres, range(4), self.actT, self.actr, T, ev)
        qxb = self.R1[:, 4096:12288].rearrange("p (j b t) -> p j b t", j=8, b=NB)
        qxbr = self.R1r[8:24]
        K.op("dve", lambda e: e.tensor_tensor(out=qxb, in0=qx[:, :, :T].unsqueeze(2).to_broadcast([128, 8, NB, T]),
                                             in1=self.bmq[:, :, :].unsqueeze(1).to_broadcast([128, 8, NB, T]), op=ALU.mult),
             reads=qxr + [self.bmqr], writes=qxbr)
        q3, q3r = self.r1_bf(3)
        PT = q3[:, :, 0:64]
        ox = q3[:, :, 64:128]
        kvb = [self.ogs[:, 1:3, :], self.ur[:, 3:5, :],
               self.xh[:, 1, :].bitcast(BF16).rearrange("p (a d) -> p a d", d=D),
               self.xh[:, 2, :].bitcast(BF16).rearrange("p (a d) -> p a d", d=D)]
        kvbr = [[self.ogsr[1], self.ogsr[2]], [self.urr[3], self.urr[4]], [self.xhr[1]], [self.xhr[2]]]
        NKV = 4
        kbT = [self.Sb[:, 0:2, :, :].rearrange("p a b c -> p (a b c)").rearrange("p (j m) -> p j m", m=256),
               self.Sb[:, 2:4, :, :].rearrange("p a b c -> p (a b c)").rearrange("p (j m) -> p j m", m=256)]
        kbTr = [self.Sbr[0:8], self.Sbr[8:16]]
        sbk = self.hold(4)

        def k_load_trans(b):
            bb = b % 2
            if USE_KVSCR:
                K.dma("pool", kvb[b % NKV], self.kvscr[0, b].rearrange("(mt p) d -> p mt d", p=128),
                      reads=[self.kvscr_res], writes=kvbr[b % NKV], sem_res=kvbr[b % NKV][0])
            else:
                K.dma("pool", kvb[b % NKV], I["ck"][b].rearrange("(mt p) d -> p mt d", p=128), writes=kvbr[b % NKV])
            for half in range(2):
                bt = self.bank()
                for jj in range(4):
                    j = half * 4 + jj
                    for mt in range(2):
                        K.op("pe", lambda e, bt=bt, jj=jj, mt=mt, j=j, b=b: e.transpose(
                            self.pbf[bt][:, jj * 256 + mt * 128:jj * 256 + (mt + 1) * 128],
                            kvb[b % NKV][:, mt, j * 128:(j + 1) * 128], self.ident[:, :]),
                             reads=kvbr[b % NKV] + [self.identr], writes=[self.pbr[bt]], signal=(jj == 3 and mt == 1))
                if half == 0:
                    K.op("act", lambda e, bt=bt, bb=bb: e.activation(out=kbT[bb][:, 0:4, :],
                                                                     in_=self.pbf[bt][:, :].rearrange("p (j m) -> p j m", m=256), func=AF.Copy),
                         reads=[self.pbr[bt]], writes=kbTr[bb])
                else:
                    K.op("dve", lambda e, bt=bt, bb=bb: e.tensor_copy(kbT[bb][:, 4:8, :],
                                                                      self.pbf[bt][:, :].rearrange("p (j m) -> p j m", m=256)),
                         reads=[self.pbr[bt]], writes=kbTr[bb])

        def k_scores(b):
            bb = b % 2
            for h in range(4):
                pairs = [(qxb[:, 2 * h + dc, b, :], kbT[bb][:, 2 * h + dc, :]) for dc in range(2)]
                self.mm(self.pb[sbk[h]][:sp, :256], pairs, qxbr + kbTr[bb], self.pbr[sbk[h]], start=(b == 0), stop=(b == NB - 1))

        k_load_trans(0)
        for b in range(NB):
            if b + 1 < NB:
                k_load_trans(b + 1)
            k_scores(b)
        Pn = self.att[:, 0:2, :, :].rearrange("p a h t -> p (a h t)").rearrange("p (h m) -> p h m", m=256)
        Pnr = [self.attr[0], self.attr[1]]
        self.softmax_rows([(sbk[h], 0) for h in range(4)], sp, Pn, Pnr)
        self.release(sbk)
        bt = self.bank()
        for h in range(4):
            for mt in range(2):
                j = h * 2 + mt
                K.op("pe", lambda e, bt=bt, h=h, mt=mt, j=j: e.transpose(self.pbf[bt][:, j * 64:(j + 1) * 64],
                                                                        Pn[:sp, h, mt * 128:(mt + 1) * 128], self.ident[:sp, :sp]),
                     reads=list(Pnr) + [self.identr], writes=[self.pbr[bt]], signal=(j == 7))
        K.op("act", lambda e, bt=bt: e.activation(out=PT, in_=self.pbf[bt][:, :512].rearrange("p (j t) -> p j t", t=64), func=AF.Copy),
             reads=[self.pbr[bt]], writes=q3r)
        bo = self.hold(1)[0]
        for b in range(NB):
            bb = b % 2
            if USE_KVSCR:
                K.dma("pool", kvb[b % NKV], self.kvscr[1, b].rearrange("(mt p) d -> p mt d", p=128),
                      reads=[self.kvscr_res], writes=kvbr[b % NKV], sem_res=kvbr[b % NKV][0])
            else:
                K.dma("pool", kvb[b % NKV], I["cv"][b].rearrange("(mt p) d -> p mt d", p=128), writes=kvbr[b % NKV])
            for j in range(8):
                h = j // 2
                pairs = [(kvb[b % NKV][:, mt, j * 128:(j + 1) * 128], PT[:, h * 2 + mt, b * ST:(b + 1) * ST]) for mt in range(2)]
                self.mm(self.pb[bo][:, j * 64 + b * ST:j * 64 + (b + 1) * ST], pairs, kvbr[b % NKV] + q3r, self.pbr[bo],
                        signal=(j == 7))
        K.op("act", lambda e: e.activation(out=ox, in_=self.pb[bo][:, :].rearrange("p (j t) -> p j t", t=64), func=AF.Copy),
             reads=[self.pbr[bo]], writes=q3r)
        self.release([bo])
        self.resid_norm("xo", ox, q3r, nsub, sp, "g_mlp")
        self.mlp_final(nsub, sp, T, None, lambda s: O["ys"][:, :])


def _extra_alloc(self):
    K = self.K
    self.eB, self.eBr = self.T_("eB", [128, 4, 16], F32)
    self.junk2 = self.vt[:, 3, :]
    self.junk2r = self.vtr[3]
    self.ufp = self.xn[:, 0:2, :].rearrange("p a d -> p (a d)").bitcast(F32)
    self.ufpr = None


_old_alloc = Prog.alloc


def _alloc(self):
    _old_alloc(self)
    _extra_alloc(self)


Prog.alloc = _alloc


def _cols_table(p):
    def colmaj(v, n):
        return np.ascontiguousarray(np.asarray(v, np.float32).reshape(n, 128).T)
    parts = [colmaj(p["norm_mix_g"][0], 8), colmaj(p["norm_x_g"][0], 8), colmaj(p["norm_mlp_g"][0], 8),
             colmaj(p["norm_mem_g"][0], 8), colmaj(p["gla_norm_g"][0], 2), colmaj(p["pool_scale"][0], 8),
             colmaj(p["b_gk"][0], 4)]
    return np.ascontiguousarray(np.concatenate(parts, axis=1))


def make_in_maps(inputs, cores):
    p = inputs
    consts = make_consts()
    shared = {
        "w_in": np.ascontiguousarray(p["w_in"][0]), "w_gk": np.ascontiguousarray(p["w_gk_up"][0]),
        "w_pm": np.ascontiguousarray(p["w_pool_mix"][0]),
        "w_a": np.ascontiguousarray(p["w_branch_a"][0]), "w_b": np.ascontiguousarray(p["w_branch_b"][0]),
        "w_o": np.ascontiguousarray(p["w_out"][0]), "w_xq": np.ascontiguousarray(p["w_xq"][0]),
        "w_xk": np.ascontiguousarray(p["w_xk"][0]), "w_xv": np.ascontiguousarray(p["w_xv"][0]),
        "w_xo": np.ascontiguousarray(p["w_xo"][0]), "w_up": np.ascontiguousarray(p["w_up"][0]),
        "w_dn": np.ascontiguousarray(p["w_down"][0]),
        "cols": _cols_table(p), "g_fin": np.ascontiguousarray(p["norm_final_g"]),
    }
    for k, v in consts.items():
        shared["c_" + k] = v
    maps = []
    for c in cores:
        m = dict(shared)
        m["xp"] = np.ascontiguousarray(p["x_prompt"][c])
        m["xs"] = np.ascontiguousarray(p["x_sample"][c * SB:(c + 1) * SB].reshape(SB * ST, D))
        m["mem"] = np.ascontiguousarray(p["mem_prompt"][c])
        m["sgla"] = np.ascontiguousarray(p["state_gla"][0, c * SB:(c + 1) * SB])
        m["spool"] = np.ascontiguousarray(p["state_pool"][0, c * SB:(c + 1) * SB])
        m["ck"] = np.ascontiguousarray(p["cache_mem_k"][0, c * SB:(c + 1) * SB].reshape(SB, MEM, D))
        m["cv"] = np.ascontiguousarray(p["cache_mem_v"][0, c * SB:(c + 1) * SB].reshape(SB, MEM, D))
        maps.append(m)
    return maps


_PROG_CACHE = {}


def get_prog(do_sample=True, dbg=()):
    key = (do_sample, tuple(dbg))
    if key not in _PROG_CACHE:
        _PROG_CACHE[key] = Prog(do_sample=do_sample, dbg=dbg)
    return _PROG_CACHE[key]


def kernel(**inputs):
    inputs = {k: np.asarray(v) for k, v in inputs.items()}
    prog = get_prog(True)
    cores = list(range(NCORE))
    maps = make_in_maps(inputs, cores)
    res = run_bass_kernel_spmd(prog.nc, maps, core_ids=cores)
    R = res.results
    yp = np.stack([R[c]["yp"] for c in cores]).astype(np.float32)
    ys = np.concatenate([R[c]["ys"].reshape(SB, ST, D) for c in cores]).astype(np.float32)
    mk = np.stack([R[c]["mk"].reshape(MEM, 4, 256) for c in cores])[None].astype(np.float32)
    mv = np.stack([R[c]["mv"].reshape(MEM, 4, 256) for c in cores])[None].astype(np.float32)
    sgp = np.stack([R[c]["sgp"] for c in cores])[None].astype(np.float32)
    sgs = np.concatenate([R[c]["sgs"] for c in cores])[None].astype(np.float32)
    spp = np.stack([R[c]["spp"] for c in cores])[None].astype(np.float32)
    sps = np.concatenate([R[c]["sps"] for c in cores])[None].astype(np.float32)
    return (yp, ys, mk, mv, sgp, sgs, spp, sps)
```

```python
import contextlib
import math
import numpy as np
import concourse.bass as bass
import concourse.mybir as mybir
from concourse.bass_utils import run_bass_kernel_spmd

F32 = mybir.dt.float32
BF16 = mybir.dt.bfloat16
AF = mybir.ActivationFunctionType
ALU = mybir.AluOpType
AX = mybir.AxisListType

COMPUTE = ("pe", "act", "dve", "pool")

D = 1024
SEQ = 2048
NCORE = 8
SB = 16
ST = 4
MEM = 256
DFF = 4096
INW = 6160
EPS = 1e-6
NSLOT = 5
USE_SCRATCH = True
USE_KVSCR = True
SCR_SPREAD = 3
HIDE_TAIL = True
WARM_DUMMIES = 8
POOL_SOFTMAX = "pool"


class Res:
    __slots__ = ("name", "last_w", "reads", "dsem", "dcount", "excl")

    def __init__(self, name, excl=False):
        self.name = name
        self.excl = excl
        self.last_w = None
        self.reads = {}
        self.dsem = None
        self.dcount = 0


class Sched:
    def __init__(self, nc, stack):
        self.nc = nc
        self.stack = stack
        self.streams = {e: [] for e in COMPUTE + ("sp",)}
        self.count = {e: 0 for e in COMPUTE}
        self.sem = {e: stack.enter_context(nc.semaphore(f"S_{e}")) for e in COMPUTE}
        self.waited = {e: {} for e in COMPUTE + ("sp",)}
        self.dma_res = []
        self.n_waits = 0
        self.n_ops = 0
        self.sb_bytes = 0

    def sb(self, name, shape, dt):
        n = 1
        for s in shape[1:]:
            n *= s
        self.sb_bytes += n * (2 if dt == BF16 else 4)
        return self.stack.enter_context(self.nc.sbuf_tensor("sb_" + name, list(shape), dt))

    def ps(self, name, shape, dt):
        return self.stack.enter_context(self.nc.psum_tensor("ps_" + name, list(shape), dt))

    def new_sem(self, name):
        return self.stack.enter_context(self.nc.semaphore(name))

    def _deps(self, eng, reads, writes):
        deps = {}

        def add(ev, raw):
            if ev is None:
                return
            sem, val, src = ev
            if (not raw) and src == eng and eng == "pe":
                return
            k = id(sem)
            if k not in deps or deps[k][1] < val:
                deps[k] = ev

        for r in reads:
            add(r.last_w, True)
            if r.excl:
                for ev in r.reads.values():
                    add(ev, False)
        for w in writes:
            add(w.last_w, False)
            for ev in w.reads.values():
                add(ev, False)
        return deps

    def _emit_waits(self, eng, deps):
        wd = self.waited[eng]
        out = []
        for k, (sem, val, src) in deps.items():
            if wd.get(k, 0) >= val:
                continue
            wd[k] = val
            out.append((sem, val))
        return out

    def _record(self, ev, reads, writes):
        k = id(ev[0])
        for r in reads:
            old = r.reads.get(k)
            if old is None or old[1] < ev[1]:
                r.reads[k] = ev
        for w in writes:
            w.last_w = ev
            w.reads = {}

    def op(self, eng, fn, reads=(), writes=(), signal=True):
        waits = self._emit_waits(eng, self._deps(eng, reads, writes))
        self.n_waits += len(waits)
        self.n_ops += 1
        if signal:
            self.count[eng] += 1
            val = self.count[eng]
        else:
            val = self.count[eng] + 1
        sem = self.sem[eng]
        ev = (sem, val, eng)

        def run(e, waits=waits, fn=fn, signal=signal, sem=sem):
            for s, v in waits:
                e.wait_ge(s, v)
            ins = fn(e)
            if signal:
                ins.then_inc(sem, 1)

        self.streams[eng].append(run)
        self._record(ev, reads, writes)
        return ev

    def dma(self, queue, out, in_, reads=(), writes=(), sem_res=None):
        if sem_res is None:
            sem_res = (list(writes) + list(reads))[0]
        qk = "sw" if queue == "pool" else "hw"
        if sem_res.dsem is None:
            sem_res.dsem = {}
        if qk not in sem_res.dsem:
            ent = [self.new_sem(f"D{qk}_{sem_res.name}"), 0]
            sem_res.dsem[qk] = ent
            self.dma_res.append(ent)
        ent = sem_res.dsem[qk]
        waits = self._emit_waits(queue, self._deps("dma", reads, writes))
        self.n_waits += len(waits)
        ent[1] += 16
        sem = ent[0]
        ev = (sem, ent[1], "dma")

        def run(e, waits=waits, out=out, in_=in_, sem=sem):
            for s, v in waits:
                e.wait_ge(s, v)
            e.dma_start(out=out, in_=in_).then_inc(sem, 16)

        self.streams[queue].append(run)
        self._record(ev, reads, writes)
        return ev

    def finish(self):
        waits = [(ent[0], ent[1]) for ent in self.dma_res]
        for e in COMPUTE:
            if self.count[e]:
                waits.append((self.sem[e], self.count[e]))

        def run(e, waits=waits):
            for s, v in waits:
                e.wait_ge(s, v)

        self.streams["sp"].append(run)

    def emit(self):
        st = self.streams
        with self.nc.Block() as block:
            @block.sync
            def _(e):
                for f in st["sp"]:
                    f(e)

            @block.tensor
            def _(e):
                for f in st["pe"]:
                    f(e)

            @block.scalar
            def _(e):
                for f in st["act"]:
                    f(e)

            @block.vector
            def _(e):
                for f in st["dve"]:
                    f(e)

            @block.gpsimd
            def _(e):
                for f in st["pool"]:
                    f(e)


POOL_WINDOWS = (2, 4, 8, 16)


def _bf16_round(a):
    a = np.asarray(a, np.float32)
    u = a.view(np.uint32).astype(np.uint64)
    r = ((u + 0x7FFF + ((u >> 16) & 1)) >> 16) << 16
    return r.astype(np.uint32).view(np.float32)


def make_consts():
    c = {}
    c["ident"] = np.eye(128, dtype=np.float32)
    s = np.arange(128)[:, None]
    t = np.arange(128)[None, :]
    c["maskT"] = (s <= t).astype(np.float32)
    s6 = np.arange(64)[:, None]
    t6 = np.arange(64)[None, :]
    c["smask"] = ((s6 // ST == t6 // ST) & (s6 <= t6)).astype(np.float32)
    r = np.ones((128, 512), np.float32)
    r[:, ::128] = 0.0
    c["rst512"] = r
    r = np.ones((128, 64), np.float32)
    r[:, ::ST] = 0.0
    c["rst64"] = r
    pm = np.zeros((128, 4, 4, 128), np.float32)
    for g, w in enumerate(POOL_WINDOWS):
        cur = ((s <= t) & (s >= t - w + 1)).astype(np.float32) / w - (s == t).astype(np.float32)
        prev = ((s - 128) >= (t - w + 1)).astype(np.float32) / w
        cnt = np.minimum(t + 1, w).astype(np.float32)
        m0 = ((s <= t) & (s >= t - w + 1)).astype(np.float32) / cnt - (s == t).astype(np.float32)
        hi = _bf16_round(m0)
        lo = _bf16_round(m0 - hi)
        pm[:, 0, g], pm[:, 1, g], pm[:, 2, g], pm[:, 3, g] = cur, prev, hi, lo
    c["pmat"] = pm
    mb = np.zeros((120, 2, 4, 64), np.float32)
    mu = np.zeros((64, 4, 64), np.float32)
    for g, w in enumerate(POOL_WINDOWS):
        for b in range(SB):
            for tt in range(ST):
                col = b * ST + tt
                lo_idx = 15 + tt - w + 1
                for j in range(15):
                    if j >= lo_idx:
                        mb[(b % 8) * 15 + j, b // 8, g, col] = 1.0 / w
                for t2 in range(ST):
                    v = 0.0
                    if t2 <= tt and (15 + t2) >= lo_idx:
                        v += 1.0 / w
                    if t2 == tt:
                        v -= 1.0
                    mu[b * ST + t2, g, col] = v
    c["smb"] = mb
    c["smu"] = mu
    bq = np.zeros((128, SB, 64), np.float32)
    for b in range(SB):
        bq[:, b, b * ST:(b + 1) * ST] = 1.0
    c["bmq"] = bq
    bk = np.zeros((64, SB), np.float32)
    for b in range(SB):
        bk[b * ST:(b + 1) * ST, b] = 1.0
    c["bmk"] = bk
    return c


CONST_SHAPES = {k: v.shape for k, v in make_consts().items()}

COLS = {}
_o = 0
for _n, _w in (("g_mix", 8), ("g_x", 8), ("g_mlp", 8), ("g_mem", 8), ("g_gla", 2), ("pscale", 8), ("bgk", 4)):
    COLS[_n] = (_o, _w)
    _o += _w
NCOL = _o


class StopBuild(Exception):
    pass


class Prog:
    def __init__(self, do_sample=True, dbg=(), stop=None):
        self.stop = stop
        self.dbg_names = dbg
        self.do_sample = do_sample
        self.nc = nc = bass.Bass("TRN2", target_bir_lowering=False)
        self.I = {}
        self.O = {}

        def inp(name, shape):
            self.I[name] = nc.dram_tensor(name, list(shape), F32, kind="ExternalInput").ap()

        def outp(name, shape):
            self.O[name] = nc.dram_tensor(name, list(shape), F32, kind="ExternalOutput").ap()

        inp("xp", [SEQ, D]); inp("xs", [SB * ST, D]); inp("mem", [MEM, D])
        inp("sgla", [SB, 4, 128, 256]); inp("spool", [SB, 15, D])
        inp("ck", [SB, MEM, D]); inp("cv", [SB, MEM, D])
        inp("w_in", [D, INW]); inp("w_gk", [16, 512]); inp("w_pm", [4, 256, 256])
        for n in ("w_a", "w_b", "w_o", "w_xq", "w_xk", "w_xv", "w_xo"):
            inp(n, [D, D])
        inp("w_up", [D, DFF]); inp("w_dn", [DFF, D])
        inp("cols", [128, NCOL]); inp("g_fin", [D])
        for k, shp in CONST_SHAPES.items():
            inp("c_" + k, shp)
        outp("yp", [SEQ, D]); outp("ys", [SB * ST, D]); outp("mk", [MEM, D]); outp("mv", [MEM, D])
        outp("sgp", [4, 128, 256]); outp("sgs", [SB, 4, 128, 256]); outp("spp", [15, D]); outp("sps", [SB, 15, D])
        self.dbg_out = {}

        with contextlib.ExitStack() as stack:
            self.K = K = Sched(nc, stack)
            self.alloc()
            for s_ in range(2):
                xa, xr = self.xnext(s_)
                K.dma("sp", xa, self.I["mem"][s_ * 128:(s_ + 1) * 128, :], writes=xr)
            self.plan_units()
            self.load_consts()
            try:
                self.stage("consts")
                for s_ in range(4):
                    K.dma("sp", self.xh[:, s_, :], self.I["xp"][s_ * 128:(s_ + 1) * 128, :], writes=[self.xhr[s_]])
                self.mem_kv()
                self.stage("memkv")
                for ti in range(SEQ // 512):
                    self.kv_active = ti >= 2
                    self.prompt_tile(ti)
                    self.stage(f"tile{ti}")
                self.kv_active = False
                while self.kv_todo:
                    w, b = self.kv_todo.pop(0)
                    K.dma("pool", self.kvscr[w, b], self.I["ck" if w == 0 else "cv"][b], writes=[self.kvscr_res])
                if self.kvscr_res.dsem is not None:
                    ent = self.kvscr_res.dsem["sw"]
                    self.kvscr_res.last_w = (ent[0], ent[1], "dma")
                self.prompt_finish()
                if do_sample:
                    self.sample_pass()
                assert self.wi == len(self.units), (self.wi, len(self.units))
            except StopBuild:
                print("[kernel] build stopped at", self.stop)
            K.finish()
            K.emit()
            print(f"[kernel] ops={K.n_ops} waits={K.n_waits} sbuf_bytes/partition={K.sb_bytes} "
                  f"dma_sems={len(K.dma_res)} counts={K.count}")

    def stage(self, name):
        if self.stop == name:
            raise StopBuild()

    def T_(self, name, shape, dt, nres=1):
        t = self.K.sb(name, shape, dt)
        if nres == 1:
            return t, Res(name)
        return t, [Res(f"{name}{i}") for i in range(nres)]

    def alloc(self):
        K = self.K
        self.pb = [K.ps(f"pb{i}", [128, 512], F32) for i in range(8)]
        self.pbf = [p.bitcast(BF16) for p in self.pb]
        self.pbr = [Res(f"pb{i}", excl=True) for i in range(8)]
        self.bi = 0
        self.held = set()
        self.pending_trans = None
        self.wt = [K.sb(f"wt{i}", [128, 8, 512], BF16) for i in range(NSLOT)]
        self.wr = [Res(f"wt{i}") for i in range(NSLOT)]
        self.xh, self.xhr = self.T_("xh", [128, 4, D], F32, 4)
        self.xn, self.xnr = self.T_("xn", [128, 4, D], BF16, 4)
        self.actT, self.actr = self.T_("actT", [128, 8, 512], BF16, 8)
        self.S, self.Sr = self.T_("S", [128, 4, 256], F32, 4)
        self.Sb, self.Sbr = self.T_("Sb", [128, 4, 4, 256], BF16, 16)
        self.ur, self.urr = self.T_("uring", [128, 5, D], BF16, 5)
        self.KTp, self.KTpr = self.T_("KTp", [128, 8, 256], BF16)
        self.Vp, self.Vpr = self.T_("Vp", [128, 2, D], BF16)
        self.junk, self.junkr = self.T_("junk", [128, 256], BF16)
        self.small, self.smallr = self.T_("small", [128, 96], F32)
        self.sm2r = [Res("sm_a"), Res("sm_b"), Res("sm_c")]
        self.nsr = [Res(f"ns{i}") for i in range(4)]
        self.osr = [Res(f"os{i}") for i in range(4)]
        self.ident, self.identr = self.T_("ident", [128, 128], BF16)
        self.maskT, self.maskTr = self.T_("maskT", [128, 128], F32)
        self.smask, self.smaskr = self.T_("smask", [64, 64], F32)
        self.rst512, self.rst512r = self.T_("rst512", [128, 512], BF16)
        self.rst64, self.rst64r = self.T_("rst64", [128, 64], F32)
        self.pmat, self.pmatr = self.T_("pmat", [128, 4, 4, 128], BF16)
        self.smb, self.smbr = self.T_("smb", [120, 2, 4, 64], BF16)
        self.smu, self.smur = self.T_("smu", [64, 4, 64], BF16)
        self.bmq, self.bmqr = self.T_("bmq", [128, SB, 64], BF16)
        self.bmk, self.bmkr = self.T_("bmk", [64, SB], BF16)
        self.cols, self.colsr = self.T_("cols", [128, NCOL], F32)
        self.ncols, self.ncolsr = self.T_("ncols", [128, 8], F32)
        self.gfin, self.gfinr = self.T_("gfin", [128, D], F32)
        self.wgz, self.wgzr = self.T_("wgz", [128, 8, 16], BF16)
        self.wgk, self.wgkr = self.T_("wgk", [16, 512], BF16)
        self.cres = Res("consts")
        self.R1 = K.sb("R1", [128, 16384], BF16)
        self.R1r = [Res(f"R1_{i}") for i in range(32)]
        self.qi, self.qir = self.T_("qi", [128, 4, 512], BF16, 4)
        self.kd, self.kdr = self.T_("kd", [128, 4, 512], BF16, 4)
        self.kdt, self.kdtr = self.T_("kdt", [128, 4, 512], BF16, 4)
        self.vt, self.vtr = self.T_("vt", [128, 4, D], BF16, 4)
        self.ogs, self.ogsr = self.T_("ogs", [128, 4, D], BF16, 4)
        self.ga, self.gar = self.T_("ga", [128, 8, 512], BF16, 8)
        self.gb, self.gbr = self.T_("gb", [128, 8, 512], BF16, 8)
        self.att, self.attr = self.T_("att", [128, 4, 4, 128], BF16, 4)
        self.tmp, self.tmpr = self.T_("tmp", [128, 4, 512], F32, 4)
        self.tp = 0

    def r1_f32(self, i):
        v = self.R1[:, i * 4096:(i + 1) * 4096].bitcast(F32)
        return v.rearrange("p (h t) -> p h t", t=512), self.R1r[i * 8:(i + 1) * 8]

    def r1_bf(self, i):
        v = self.R1[:, i * 4096:(i + 1) * 4096]
        return v.rearrange("p (c t) -> p c t", t=512), self.R1r[i * 8:(i + 1) * 8]

    def bank(self):
        while True:
            b = self.bi
            self.bi = (self.bi + 1) % 8
            if b not in self.held:
                return b

    def hold(self, n):
        bs = []
        for _ in range(n):
            b = self.bank()
            self.held.add(b)
            bs.append(b)
        return bs

    def release(self, bs):
        for b in bs:
            self.held.discard(b)

    def tmpbuf(self):
        i = self.tp
        self.tp = (self.tp + 1) % 4
        return self.tmp[:, i, :], self.tmpr[i]

    def col(self, name, j=0, n=1, P=128):
        o, w = COLS[name]
        return self.cols[:P, o + j:o + j + n]

    def load_consts(self):
        K = self.K
        I = self.I
        cr = self.cres
        K.dma("sp", self.cols[:], I["cols"][:, :], writes=[self.colsr], sem_res=cr)
        K.dma("sp", self.maskT[:], I["c_maskT"][:, :], writes=[self.maskTr], sem_res=cr)
        K.dma("sp", self.smask[:], I["c_smask"][:, :], writes=[self.smaskr], sem_res=cr)
        K.dma("sp", self.rst64[:], I["c_rst64"][:, :], writes=[self.rst64r], sem_res=cr)
        K.dma("sp", self.gfin[:], I["g_fin"].partition_broadcast(128), writes=[self.gfinr], sem_res=cr)
        K.dma("pool", self.ident[:], I["c_ident"][:, :], writes=[self.identr], sem_res=self.identr)
        self.prefetch(0)
        K.dma("pool", self.rst512[:], I["c_rst512"][:, :], writes=[self.rst512r], sem_res=cr)
        K.dma("pool", self.pmat[:], I["c_pmat"][:, :, :, :], writes=[self.pmatr], sem_res=cr)
        K.dma("pool", self.smb[:], I["c_smb"][:, :, :, :], writes=[self.smbr], sem_res=cr)
        K.dma("pool", self.smu[:], I["c_smu"][:, :, :], writes=[self.smur], sem_res=cr)
        K.dma("pool", self.bmq[:], I["c_bmq"][:, :, :], writes=[self.bmqr], sem_res=cr)
        K.dma("pool", self.bmk[:], I["c_bmk"][:, :], writes=[self.bmkr], sem_res=cr)
        K.dma("pool", self.wgk[:], I["w_gk"][:, :], writes=[self.wgkr], sem_res=cr)
        K.dma("pool", self.wgz[:], I["w_in"][:, 2048:2064].rearrange("(kc p) n -> p kc n", p=128),
              writes=[self.wgzr], sem_res=cr)
        for r in (self.colsr, self.maskTr, self.smaskr, self.rst64r, self.gfinr):
            r.last_w = (cr.dsem["hw"][0], cr.dsem["hw"][1], "dma")
        for r in (self.rst512r, self.pmatr, self.smbr, self.smur, self.bmqr, self.bmkr, self.wgkr, self.wgzr):
            r.last_w = (cr.dsem["sw"][0], cr.dsem["sw"][1], "dma")
        o, w = COLS["bgk"]
        K.op("dve", lambda e: e.tensor_scalar(out=self.ncols[:, 0:4], in0=self.cols[:, o:o + 4], scalar1=-1.0,
                                             scalar2=None, op0=ALU.mult), reads=[self.colsr], writes=[self.ncolsr])
        K.op("dve", lambda e: e.memset(self.ncols[:, 4:5], math.log(128.0 ** -0.5)), writes=[self.ncolsr])
        K.op("dve", lambda e: e.memset(self.ncols[:, 5:6], 1.0), writes=[self.ncolsr])
        K.op("dve", lambda e: e.memset(self.ncols[:, 6:7], EPS), writes=[self.ncolsr])
        for h in range(4):
            K.op("dve", lambda e, h=h: e.memset(self.S[:, h, :], 0.0), writes=[self.Sr[h]])
        K.op("dve", lambda e: e.memset(self.ur[:, 4, :], 0.0), writes=[self.urr[4]])

    def plan_units(self):
        I = self.I
        U = []

        def full(w, r0, c0, name, sidx=None, p=0):
            U.append((name, w[r0:r0 + 1024, c0:c0 + 512].rearrange("(kc p) n -> p kc n", p=128), 512, sidx, mode(sidx, p)))

        def mode(sidx, p):
            if sidx is None or not USE_SCRATCH:
                return 0
            tp = sidx % SCR_SPREAD
            return 0 if p < tp else (1 if p == tp else 2)

        def one_pass(first):
            k = 0
            for nm, c0 in (("v0", 1024), ("v1", 1536), ("og0", 2064), ("og1", 2576), ("u0", 3088), ("u1", 3600),
                           ("ga0", 4112), ("ga1", 4624), ("gb0", 5136), ("gb1", 5648), ("k", 512), ("q", 0)):
                full(I["w_in"], 0, c0, nm, k, first); k += 1
            U.append(("pm", I["w_pm"].rearrange("g (kc p) d -> p (g kc) d", p=128), 256, k, mode(k, first))); k += 1
            for nm, w, c0 in (("b0", "w_b", 0), ("a0", "w_a", 0), ("b1", "w_b", 512), ("a1", "w_a", 512),
                              ("o0", "w_o", 0), ("o1", "w_o", 512),
                              ("xq0", "w_xq", 0), ("xq1", "w_xq", 512), ("xo0", "w_xo", 0), ("xo1", "w_xo", 512)):
                full(I[w], 0, c0, nm, k, first); k += 1
            for i in range(8):
                full(I["w_up"], 0, i * 512, f"up{i}", k, first); k += 1
            for n in range(2):
                for j in range(4):
                    full(I["w_dn"], j * 1024, n * 512, f"dn{j}_{n}", k, first); k += 1
            return k

        for nm, w, c0 in (("xk0", "w_xk", 0), ("xk1", "w_xk", 512), ("xv0", "w_xv", 0), ("xv1", "w_xv", 512)):
            full(I[w], 0, c0, nm)
        npass = SEQ // 512 + (1 if self.do_sample else 0)
        for p in range(npass):
            nper = one_pass(p)
        self.units = U
        self.pend_st = []
        self.wi = 0
        self.wl = 0
        self.scr = self.nc.dram_tensor("wscratch", [nper, 128, 8 * 512], BF16, kind="Internal").ap()
        self.scrr = [Res(f"scr{i}") for i in range(nper)]
        self.kvscr = self.nc.dram_tensor("kvscratch", [2, SB, MEM, D], BF16, kind="Internal").ap()
        self.kvscr_res = Res("kvscr")
        self.kv_todo = [(w, b) for w in range(2) for b in range(SB)] if (self.do_sample and USE_KVSCR) else []
        self.kv_tick = 0
        self.kv_active = False

    def W(self, name):
        i = self.wi
        assert self.units[i][0] == name, (self.units[i][0], name)
        self.prefetch(i)
        self.kv_tick += 1
        if self.kv_active and self.kv_todo and self.kv_tick % 2 == 0:
            w, b = self.kv_todo.pop(0)
            src = self.I["ck" if w == 0 else "cv"][b]
            self.K.dma("pool", self.kvscr[w, b], src, writes=[self.kvscr_res])
        self.wi += 1
        s = i % NSLOT
        return self.wt[s], self.wr[s]

    def flush_one_store(self):
        j, s, sidx, nc_ = self.pend_st.pop(0)
        self.K.dma("pool", self.scr[sidx].rearrange("p (kc n) -> p kc n", n=512)[:, :, 0:nc_], self.wt[s][:, :, 0:nc_],
                   reads=[self.wr[s]], writes=[self.scrr[sidx]], sem_res=self.scrr[sidx])

    def prefetch(self, i):
        K = self.K
        lim = min(len(self.units), i + NSLOT - 1)
        while self.wl < lim:
            j = self.wl
            s = j % NSLOT
            nm, src, nc_, sidx, md = self.units[j]
            if md in (0, 1):
                K.dma("pool", self.wt[s][:, :, 0:nc_], src, writes=[self.wr[s]])
                if md == 1:
                    self.pend_st.append((j, s, sidx, nc_))
            else:
                K.dma("pool", self.wt[s][:, :, 0:nc_], self.scr[sidx].rearrange("p (kc n) -> p kc n", n=512)[:, :, 0:nc_],
                      reads=[self.scrr[sidx]], writes=[self.wr[s]])
            self.wl += 1
            while self.pend_st and self.pend_st[0][0] <= j - 2:
                self.flush_one_store()

    def mm(self, out, pairs, reads, bres, start=True, stop=True, signal=True):
        def fn(e, out=out, pairs=pairs, start=start, stop=stop):
            n = len(pairs)
            ins = None
            for i, (l, r) in enumerate(pairs):
                ins = e.matmul(out, l, r, start=(start and i == 0), stop=(stop and i == n - 1))
            return ins
        return self.K.op("pe", fn, reads=reads, writes=[bres], signal=signal)

    def dbg(self, name, ap, res, shape):
        if name not in self.dbg_names or name in self.dbg_out:
            return
        o = self.nc.dram_tensor("dbg_" + name, list(shape), F32, kind="ExternalOutput").ap()
        self.dbg_out[name] = o
        st = self.K.sb("dbgs_" + name, list(shape), F32)
        sr = Res("dbgs_" + name)
        rl = res if isinstance(res, (list, tuple)) else [res]
        self.K.op("dve", lambda e: e.tensor_copy(st[:], ap), reads=rl, writes=[sr])
        self.K.dma("sp", o, st[:], reads=[sr])

    def rstd_from_ss(self, ss_ap, out_ap, inv_n, P, sr=None):
        K = self.K
        sr = self.smallr if sr is None else sr
        K.op("act", lambda e: e.activation(out=out_ap, in_=ss_ap, func=AF.Ln, scale=inv_n, bias=self.ncols[:P, 6:7]),
             reads=[sr, self.ncolsr], writes=[sr])
        K.op("act", lambda e: e.activation(out=out_ap, in_=out_ap, func=AF.Exp, scale=-0.5), reads=[sr], writes=[sr])

    def xnext(self, s):
        g, gr = (self.ga, self.gar) if s < 2 else (self.gb, self.gbr)
        c0 = (s % 2) * 4
        return g[:, c0:c0 + 4, :].rearrange("p c t -> p (c t)").bitcast(F32), gr[c0:c0 + 4]

    def norm_sub(self, s, sp, src=None, srcr=None):
        K = self.K
        sr = self.nsr[s]
        if src is None:
            src, srcr = self.xh[:sp, s, :], [self.xhr[s]]
        K.op("act", lambda e: e.activation(out=self.xn[:sp, s, :], in_=src, func=AF.Square,
                                           accum_out=self.small[:sp, s:s + 1]),
             reads=list(srcr) + [sr], writes=[self.xnr[s], sr])
        self.rstd_from_ss(self.small[:sp, s:s + 1], self.small[:sp, 4 + s:5 + s], 1.0 / D, sp, sr=sr)
        K.op("dve", lambda e: e.tensor_scalar(out=self.xn[:sp, s, :], in0=src,
                                             scalar1=self.small[:sp, 4 + s:5 + s], scalar2=None, op0=ALU.mult),
             reads=list(srcr) + [sr], writes=[self.xnr[s]])

    def trans_sub(self, src, srcr, dst, dstr, s, sp, gname, gmod=8):
        K = self.K
        b = self.bank()
        for c in range(8):
            K.op("pe", lambda e, b=b, c=c: e.transpose(self.pbf[b][:, c * sp:(c + 1) * sp], src[:sp, s, c * 128:(c + 1) * 128],
                                                      self.ident[:sp, :sp]),
                 reads=[srcr[s], self.identr], writes=[self.pbr[b]], signal=(c == 7))
        o, w = COLS[gname]
        if gmod == 8:
            outv = dst[:, :, s * sp:(s + 1) * sp]
            inv = self.pbf[b][:, :8 * sp].rearrange("p (c t) -> p c t", t=sp)
            gv = self.cols[:, o:o + 8].unsqueeze(2).to_broadcast([128, 8, sp])
        else:
            outv = dst[:, :, s * sp:(s + 1) * sp].rearrange("p (a b) t -> p a b t", b=gmod)
            inv = self.pbf[b][:, :8 * sp].rearrange("p (a b t) -> p a b t", b=gmod, t=sp)
            gv = self.cols[:, o:o + gmod].unsqueeze(1).unsqueeze(3).to_broadcast([128, 8 // gmod, gmod, sp])
        K.op("dve", lambda e: e.tensor_tensor(out=outv, in0=inv, in1=gv, op=ALU.mult),
             reads=[self.pbr[b], self.colsr], writes=list(dstr))

    def norm_to_T(self, gname, nsub, sp, T):
        for s in range(nsub):
            self.norm_sub(s, sp)
        for s in range(nsub):
            self.trans_sub(self.xn, self.xnr, self.actT, self.actr, s, sp, gname)

    def to_T(self, src, srcr, dst, dstr, nsub, sp, T, gname, gmod=8):
        K = self.K
        for c in range(8):
            b = self.bank()
            for s in range(nsub):
                K.op("pe", lambda e, b=b, s=s, c=c: e.transpose(self.pbf[b][:, s * sp:(s + 1) * sp],
                                                                src[:sp, s, c * 128:(c + 1) * 128],
                                                                self.ident[:sp, :sp]),
                     reads=[srcr[s], self.identr], writes=[self.pbr[b]], signal=(s == nsub - 1))
            K.op("act", lambda e, b=b, c=c: e.activation(out=dst[:, c, :T], in_=self.pbf[b][:, :T], func=AF.Copy,
                                                         scale=self.col(gname, c % gmod)),
                 reads=[self.pbr[b], self.colsr], writes=[dstr[c]])

    def proj_fm(self, wt, wres, ms, srcT, srcr, T, evac):
        ms = list(ms)
        if T * len(ms) <= 512 and len(ms) == 4:
            b = self.bank()
            for m in ms:
                pairs = [(wt[:, kc, m * 128:(m + 1) * 128], srcT[:, kc, :T]) for kc in range(8)]
                self.mm(self.pb[b][:, m * T:(m + 1) * T], pairs, [wres] + list(srcr), self.pbr[b], signal=(m == ms[-1]))
            evac(None, b)
            return
        if self.pending_trans is not None and T == 512:
            pend, self.pending_trans = self.pending_trans, None
            TA = T - 128
            head = ms[:3]
            hb = []
            for m in head:
                b = self.bank()
                hb.append(b)
                pairs = [(wt[:, kc, m * 128:(m + 1) * 128], srcT[:, kc, :TA]) for kc in range(8)]
                self.mm(self.pb[b][:, :TA], pairs, [wres] + list(srcr), self.pbr[b])
            pend()
            for m, b in zip(head, hb):
                pairs = [(wt[:, kc, m * 128:(m + 1) * 128], srcT[:, kc, TA:T]) for kc in range(8)]
                self.mm(self.pb[b][:, TA:T], pairs, [wres] + list(srcr), self.pbr[b])
                evac(m, b)
            ms = ms[3:]
        for m in ms:
            b = self.bank()
            pairs = [(wt[:, kc, m * 128:(m + 1) * 128], srcT[:, kc, :T]) for kc in range(8)]
            self.mm(self.pb[b][:, :T], pairs, [wres] + list(srcr), self.pbr[b])
            evac(m, b)

    def proj_tm(self, wt, wres, srcT, srcr, nsub, sp, evac):
        for s in range(nsub):
            b = self.bank()
            pairs = [(srcT[:, kc, s * sp:(s + 1) * sp], wt[:, kc, :]) for kc in range(8)]
            self.mm(self.pb[b][:sp, :], pairs, [wres] + list(srcr), self.pbr[b])
            evac(s, b)

    def sigmoid_to(self, b, T, out_ap, out_res, P=128, ncol=None, fold=None):
        K = self.K
        ncol = T if ncol is None else ncol
        t, tr = self.tmpbuf()
        out_res_l = out_res if isinstance(out_res, (list, tuple)) else [out_res]
        if fold is not None:
            tv = t[:P, :ncol].rearrange("p (m t) -> p m t", t=fold)
            K.op("act", lambda e: e.activation(out=t[:P, :ncol], in_=self.pb[b][:P, :ncol], func=AF.Exp, scale=-1.0),
                 reads=[self.pbr[b]], writes=[tr])
            K.op("act", lambda e: e.activation(out=t[:P, :ncol], in_=t[:P, :ncol], func=AF.Ln, bias=self.ncols[:P, 5:6]),
                 reads=[tr, self.ncolsr], writes=[tr])
            K.op("act", lambda e: e.activation(out=out_ap, in_=tv, func=AF.Exp, scale=-1.0),
                 reads=[tr], writes=list(out_res_l))
            return t, tr
        K.op("act", lambda e: e.activation(out=t[:P, :ncol], in_=self.pb[b][:P, :ncol], func=AF.Exp, scale=-1.0),
             reads=[self.pbr[b]], writes=[tr])
        K.op("act", lambda e: e.activation(out=t[:P, :ncol], in_=t[:P, :ncol], func=AF.Ln, bias=self.ncols[:P, 5:6]),
             reads=[tr, self.ncolsr], writes=[tr])
        K.op("act", lambda e: e.activation(out=out_ap, in_=t[:P, :ncol], func=AF.Exp, scale=-1.0),
             reads=[tr], writes=[out_res])
        return t, tr

    def mem_kv(self):
        K = self.K
        I, O = self.I, self.O
        for s in range(2):
            xa, xr = self.xnext(s)
            self.norm_sub(s, 128, src=xa, srcr=xr)
        memT = self.kd[:, :, :].rearrange("p h t -> p (h t)").rearrange("p (c t) -> p c t", t=256)
        memTr = self.kdr
        for s in range(2):
            self.trans_sub(self.xn, self.xnr, memT, memTr, s, 128, "g_mem")
        self.norm_to_T("g_mix", 4, 128, 512)
        self.stage("memkv_norm")
        stg, stgr = self.r1_f32(0)
        stg2, stg2r = self.r1_f32(1)
        for which, (st, str_), outn in (("xk", (stg, stgr), "mk"), ("xv", (stg2, stg2r), "mv")):
            if which == "xv":
                self.stage("memkv_xk")
            for n in range(2):
                wt, wres = self.W(f"{which}{n}")
                for mt in range(2):
                    b = self.bank()
                    pairs = [(memT[:, kc, mt * 128:(mt + 1) * 128], wt[:, kc, :]) for kc in range(8)]
                    self.mm(self.pb[b][:, :], pairs, [wres] + memTr, self.pbr[b])
                    g = mt * 2 + n
                    K.op("act", lambda e, b=b, g=g, st=st: e.activation(out=st[:, g, :], in_=self.pb[b][:, :], func=AF.Copy),
                         reads=[self.pbr[b]], writes=[str_[g * 2], str_[g * 2 + 1]])
                    if which == "xv":
                        K.op("dve", lambda e, b=b, mt=mt, n=n: e.tensor_copy(self.Vp[:, mt, n * 512:(n + 1) * 512], self.pb[b][:, :]),
                             reads=[self.pbr[b]], writes=[self.Vpr])
                    K.dma("sp", O[outn][mt * 128:(mt + 1) * 128, n * 512:(n + 1) * 512], st[:, g, :],
                          reads=[str_[g * 2], str_[g * 2 + 1]])
                if which == "xk":
                    for m in range(4):
                        b = self.bank()
                        pairs = [(wt[:, kc, m * 128:(m + 1) * 128], memT[:, kc, :256]) for kc in range(8)]
                        self.mm(self.pb[b][:, :256], pairs, [wres] + memTr, self.pbr[b])
                        K.op("dve", lambda e, b=b, m=m, n=n: e.tensor_copy(self.KTp[:, n * 4 + m, :], self.pb[b][:, :256]),
                             reads=[self.pbr[b]], writes=[self.KTpr])

    def in_proj(self, nsub, sp, T, uslots, last_u_fp32=None):
        K = self.K
        gzT = self.att[:16, 0, :, :].rearrange("p a b -> p (a b)")
        gzr = self.attr[0]
        b = self.bank()
        pairs = [(self.wgz[:, kc, :], self.actT[:, kc, :T]) for kc in range(8)]
        self.mm(self.pb[b][:16, :T], pairs, [self.wgzr] + self.actr, self.pbr[b])
        K.op("act", lambda e, b=b: e.activation(out=gzT[:, :T], in_=self.pb[b][:16, :T], func=AF.Copy),
             reads=[self.pbr[b]], writes=[gzr])
        T1, T1r = self.r1_f32(0)
        Bc, Bcr = self.r1_f32(1)
        E1, E1r = self.r1_f32(2)
        E2, E2r = self.r1_f32(3)
        CL = 128 if T == 512 else ST
        nch = T // CL

        def decay_chain():
            for h in range(4):
                b = self.bank()
                self.mm(self.pb[b][:, :T], [(self.wgk[:, h * 128:(h + 1) * 128], gzT[:, :T])], [self.wgkr, gzr], self.pbr[b])
                K.op("act", lambda e, b=b, h=h: e.activation(out=T1[:, h, :T], in_=self.pb[b][:, :T], func=AF.Exp,
                                                             scale=-1.0, bias=self.ncols[:, h:h + 1]),
                     reads=[self.pbr[b], self.ncolsr], writes=T1r[2 * h:2 * h + 2])
                K.op("act", lambda e, h=h: e.activation(out=T1[:, h, :T], in_=T1[:, h, :T], func=AF.Ln,
                                                        bias=self.ncols[:, 5:6]),
                     reads=T1r[2 * h:2 * h + 2] + [self.ncolsr], writes=T1r[2 * h:2 * h + 2])
                rst = self.rst512 if T == 512 else self.rst64
                rstr = self.rst512r if T == 512 else self.rst64r
                K.op("dve", lambda e, h=h, rst=rst: e.tensor_tensor_scan(out=Bc[:, h, :T], data0=rst[:, :T], data1=T1[:, h, :T],
                                                                         initial=0.0, op0=ALU.mult, op1=ALU.add),
                     reads=T1r[2 * h:2 * h + 2] + [rstr], writes=Bcr[2 * h:2 * h + 2])
            CL = 128 if T == 512 else ST
            nch = T // CL
            Bv = Bc[:, :, :T].rearrange("p h (c t) -> p h c t", t=CL)
            Dv = T1[:, :, :T].rearrange("p h (c t) -> p h c t", t=CL)
            Bend = Bv[:, :, :, CL - 1:CL]
            K.op("dve", lambda e: e.tensor_tensor(out=Dv, in0=Bv, in1=Bend.to_broadcast([128, 4, nch, CL]), op=ALU.subtract),
                 reads=Bcr, writes=T1r)
            eB = self.eB
            K.op("act", lambda e: e.activation(out=eB[:, :, :nch], in_=Bv[:, :, :, CL - 1], func=AF.Exp, scale=-1.0 / 16),
                 reads=Bcr, writes=[self.eBr])
            K.op("act", lambda e: e.activation(out=E1[:, :, :T], in_=T1[:, :, :T], func=AF.Exp, scale=1.0 / 16),
                 reads=T1r, writes=E1r)
            K.op("act", lambda e: e.activation(out=E2[:, :, :T], in_=T1[:, :, :T], func=AF.Exp, scale=-1.0 / 16,
                                               bias=self.ncols[:, 4:5]),
                 reads=T1r + [self.ncolsr], writes=E2r)
        for n in range(2):
            wt, wres = self.W(f"v{n}")

            def ev(s, b, n=n):
                K.op("act", lambda e: e.activation(out=self.vt[:sp, s, n * 512:(n + 1) * 512], in_=self.pb[b][:sp, :],
                                                   func=AF.Copy), reads=[self.pbr[b]], writes=[self.vtr[s]])
            self.proj_tm(wt, wres, self.actT, self.actr, nsub, sp, ev)
        decay_chain()
        for n in range(2):
            wt, wres = self.W(f"og{n}")

            def ev(s, b, n=n):
                t, tr = self.tmpbuf()
                self.sigmoid_to(b, 512, t[:sp, :], tr, P=sp)
                K.op("dve", lambda e: e.tensor_tensor(out=self.ogs[:sp, s, n * 512:(n + 1) * 512], in0=self.pb[b][:sp, :],
                                                     in1=t[:sp, :], op=ALU.mult),
                     reads=[self.pbr[b], tr], writes=[self.ogsr[s]])
            self.proj_tm(wt, wres, self.actT, self.actr, nsub, sp, ev)
        for n in range(2):
            wt, wres = self.W(f"u{n}")

            def ev(s, b, n=n):
                sl = uslots[s]
                K.op("act", lambda e: e.activation(out=self.ur[:sp, sl, n * 512:(n + 1) * 512], in_=self.pb[b][:sp, :],
                                                   func=AF.Copy), reads=[self.pbr[b]], writes=[self.urr[sl]])
                if last_u_fp32 is not None and s == nsub - 1:
                    st, str_ = last_u_fp32
                    K.op("dve", lambda e: e.tensor_copy(st[:sp, n * 512:(n + 1) * 512], self.pb[b][:sp, :]),
                         reads=[self.pbr[b]], writes=list(str_))
            self.proj_tm(wt, wres, self.actT, self.actr, nsub, sp, ev)
        for gt, gr, nm in ((self.ga, self.gar, "ga"), (self.gb, self.gbr, "gb")):
            for n in range(2):
                wt, wres = self.W(f"{nm}{n}")

                def ev(m, b, n=n, gt=gt, gr=gr):
                    if m is None:
                        self.sigmoid_to(b, T, gt[:, n * 4:n * 4 + 4, :T], gr[n * 4:n * 4 + 4], ncol=4 * T, fold=T)
                        return
                    self.sigmoid_to(b, T, gt[:, n * 4 + m, :T], gr[n * 4 + m])
                self.proj_fm(wt, wres, range(4), self.actT, self.actr, T, ev)
        wt, wres = self.W("k")

        def evk(h, b):
            if h is None:
                K.op("dve", lambda e: e.tensor_tensor(out=self.kd[:, :, :T], in0=self.pb[b][:, :4 * T].rearrange("p (m t) -> p m t", t=T),
                                                     in1=E1[:, :, :T], op=ALU.mult),
                     reads=[self.pbr[b]] + E1r, writes=self.kdr)
                return
            K.op("dve", lambda e: e.tensor_tensor(out=self.kd[:, h, :T], in0=self.pb[b][:, :T], in1=E1[:, h, :T], op=ALU.mult),
                 reads=[self.pbr[b]] + E1r[2 * h:2 * h + 2], writes=[self.kdr[h]])
        self.proj_fm(wt, wres, range(4), self.actT, self.actr, T, evk)
        wt, wres = self.W("q")

        def evq(h, b):
            if h is None:
                K.op("dve", lambda e: e.tensor_tensor(out=self.qi[:, :, :T], in0=self.pb[b][:, :4 * T].rearrange("p (m t) -> p m t", t=T),
                                                     in1=E2[:, :, :T], op=ALU.mult),
                     reads=[self.pbr[b]] + E2r, writes=self.qir)
                return
            K.op("dve", lambda e: e.tensor_tensor(out=self.qi[:, h, :T], in0=self.pb[b][:, :T], in1=E2[:, h, :T], op=ALU.mult),
                 reads=[self.pbr[b]] + E2r[2 * h:2 * h + 2], writes=[self.qir[h]])
        self.proj_fm(wt, wres, range(4), self.actT, self.actr, T, evq)
        for c in range(nch if T == 512 else 1):
            cl = 128 if T == 512 else 64
            b = self.bank()
            for h in range(4):
                K.op("pe", lambda e, b=b, h=h, c=c, cl=cl: e.transpose(self.pbf[b][:cl, h * 128:(h + 1) * 128],
                                                                      self.kd[:, h, c * cl:(c + 1) * cl], self.ident[:, :]),
                     reads=[self.kdr[h], self.identr], writes=[self.pbr[b]], signal=(h == 3))
            K.op("act", lambda e, b=b, c=c, cl=cl: e.activation(out=self.kdt[:cl, c, :], in_=self.pbf[b][:cl, :512], func=AF.Copy),
                 reads=[self.pbr[b]], writes=[self.kdtr[c]])

    def branches_out(self, o2T, o2Tr, pooledT, pooledTr, nsub, sp, T):
        K = self.K
        mT, mTr = self.r1_bf(2)
        wts = {}
        for m in range(8):
            if m % 4 == 0:
                wts["b"] = self.W(f"b{m // 4}")
                wts["a"] = self.W(f"a{m // 4}")
            bb = self.bank()
            wt, wres = wts["b"]
            self.mm(self.pb[bb][:, :T], [(wt[:, kc, (m % 4) * 128:(m % 4 + 1) * 128], pooledT[:, kc, :T]) for kc in range(8)],
                    [wres] + list(pooledTr), self.pbr[bb])
            ba = self.bank()
            wt, wres = wts["a"]
            self.mm(self.pb[ba][:, :T], [(wt[:, kc, (m % 4) * 128:(m % 4 + 1) * 128], o2T[:, kc, :T]) for kc in range(8)],
                    [wres] + list(o2Tr), self.pbr[ba])
            t1, t1r = self.tmpbuf()
            t2, t2r = self.tmpbuf()
            K.op("dve", lambda e, bb=bb, m=m, t1=t1: e.tensor_tensor(out=t1[:, :T], in0=self.pb[bb][:, :T], in1=self.gb[:, m, :T], op=ALU.mult),
                 reads=[self.pbr[bb], self.gbr[m]], writes=[t1r])
            K.op("dve", lambda e, ba=ba, m=m, t2=t2: e.tensor_tensor(out=t2[:, :T], in0=self.pb[ba][:, :T], in1=self.ga[:, m, :T], op=ALU.mult),
                 reads=[self.pbr[ba], self.gar[m]], writes=[t2r])
            K.op("dve", lambda e, m=m, t1=t1, t2=t2: e.tensor_tensor(out=mT[:, m, :T], in0=t1[:, :T], in1=t2[:, :T], op=ALU.add),
                 reads=[t1r, t2r], writes=[mTr[m]])
        self.resid_norm("o", mT, mTr, nsub, sp, "g_x")

    def pool_mix(self, dT, dTr, pT, pTr, T):
        K = self.K
        wt, wres = self.W("pm")
        for g in range(4):
            for dch in range(2):
                b = self.bank()
                pairs = [(wt[:, g * 2 + kc, dch * 128:(dch + 1) * 128], dT[:, 2 * g + kc, :T]) for kc in range(2)]
                self.mm(self.pb[b][:, :T], pairs, [wres, dTr[2 * g], dTr[2 * g + 1]], self.pbr[b])
                K.op("act", lambda e, b=b, g=g, dch=dch: e.activation(out=pT[:, 2 * g + dch, :T], in_=self.pb[b][:, :T], func=AF.Copy,
                                                                    scale=self.col("pscale", 2 * g + dch)),
                     reads=[self.pbr[b], self.colsr], writes=[pTr[2 * g + dch]])

    def resid_norm(self, wname, srcT, srcr, nsub, sp, gname):
        K = self.K
        ws = [self.W(f"{wname}0"), self.W(f"{wname}1")]
        for s in range(nsub):
            for n in range(2):
                wt, wres = ws[n]
                b = self.bank()
                pairs = [(srcT[:, kc, s * sp:(s + 1) * sp], wt[:, kc, :]) for kc in range(8)]
                self.mm(self.pb[b][:sp, :], pairs, [wres] + list(srcr), self.pbr[b])
                K.op("dve", lambda e, s=s, n=n, b=b: e.tensor_tensor(out=self.xh[:sp, s, n * 512:(n + 1) * 512],
                                                                    in0=self.xh[:sp, s, n * 512:(n + 1) * 512],
                                                                    in1=self.pb[b][:sp, :], op=ALU.add),
                     reads=[self.pbr[b], self.xhr[s]], writes=[self.xhr[s]])
            if s >= 1:
                self.trans_sub(self.xn, self.xnr, self.actT, self.actr, s - 1, sp, gname)
            self.norm_sub(s, sp)
        if nsub == 4 and sp == 128 and HIDE_TAIL:
            self.pending_trans = lambda: self.trans_sub(self.xn, self.xnr, self.actT, self.actr, nsub - 1, sp, gname)
        else:
            self.trans_sub(self.xn, self.xnr, self.actT, self.actr, nsub - 1, sp, gname)

    def out_resid(self, wname, srcT, srcr, nsub, sp):
        K = self.K
        for n in range(2):
            wt, wres = self.W(f"{wname}{n}")

            def ev(s, b, n=n):
                K.op("dve", lambda e: e.tensor_tensor(out=self.xh[:sp, s, n * 512:(n + 1) * 512],
                                                     in0=self.xh[:sp, s, n * 512:(n + 1) * 512],
                                                     in1=self.pb[b][:sp, :], op=ALU.add),
                     reads=[self.pbr[b], self.xhr[s]], writes=[self.xhr[s]])
            self.proj_tm(wt, wres, srcT, srcr, nsub, sp, ev)

    def softmax_stages(self, hb, P, Pn, Pnr, par=0, c_eng="dve"):
        K = self.K
        sm = self.sm2r[par]
        cb = (8, 64, 80)[par]
        mx = self.small[:P, cb:cb + 4]
        nmx = self.small[:P, cb + 4:cb + 8]
        ssum = self.small[:P, cb + 8:cb + 12]
        rs = self.small[:P, cb + 12:cb + 16]
        bufs = {}

        def stage_a():
            h = 0
            while h < 4:
                b, co = hb[h]
                if h + 1 < 4 and hb[h + 1] == (b, co + 256):
                    K.op("dve", lambda e, b=b, h=h, co=co: e.reduce_max(
                        out=self.small[:P, cb + 4 + h:cb + 4 + h + 2], in_=self.pb[b][:P, co:co + 512].rearrange("p (h m) -> p h m", m=256),
                        axis=AX.X, negate=True),
                         reads=[self.pbr[b]], writes=[sm])
                    h += 2
                else:
                    K.op("dve", lambda e, b=b, h=h, co=co: e.reduce_max(out=self.small[:P, cb + 4 + h:cb + 4 + h + 1], in_=self.pb[b][:P, co:co + 256],
                                                                        axis=AX.X, negate=True),
                         reads=[self.pbr[b]], writes=[sm])
                    h += 1

        def stage_b():
            bufs["pf"] = [self.tmpbuf(), self.tmpbuf()]
            for h, (b, co) in enumerate(hb):
                pf, pfr = bufs["pf"][h // 2]
                K.op("act", lambda e, b=b, h=h, pf=pf, co=co: e.activation(out=pf[:P, (h % 2) * 256:(h % 2 + 1) * 256],
                                                                           in_=self.pb[b][:P, co:co + 256],
                                                                           func=AF.Exp, scale=1.0, bias=self.small[:P, cb + 4 + h:cb + 5 + h],
                                                                           accum_out=self.small[:P, cb + 8 + h:cb + 9 + h]),
                     reads=[self.pbr[b], sm], writes=[pfr, sm])

        def stage_c():
            K.op("dve", lambda e: e.reciprocal(out=rs, in_=ssum), reads=[sm], writes=[sm])
            for hp, (pf, pfr) in enumerate(bufs["pf"]):
                K.op(c_eng, lambda e, hp=hp, pf=pf: e.tensor_tensor(
                    out=Pn[:P, 2 * hp:2 * hp + 2, :], in0=pf[:P, :].rearrange("p (h m) -> p h m", m=256),
                    in1=self.small[:P, cb + 12 + 2 * hp:cb + 14 + 2 * hp].unsqueeze(2).to_broadcast([P, 2, 256]), op=ALU.mult),
                     reads=[pfr, sm], writes=list(Pnr))
        return stage_a, stage_b, stage_c

    def softmax_rows(self, hb, P, Pn, Pnr, par=0):
        fa, fb, fc = self.softmax_stages(hb, P, Pn, Pnr, par)
        fa()
        fb()
        fc()

    def mlp_final(self, nsub, sp, T, ydst, yres_fn, pro_a=None, pro_b=None):
        K = self.K
        hid = self.R1[:, :].rearrange("p (c t) -> p c t", t=512)
        hidr = self.R1r
        for i in range(8):
            wt, wres = self.W(f"up{i}")

            def ev(m, b, i=i):
                t, tr = self.tmpbuf()
                if m is None:
                    K.op("act", lambda e: e.activation(out=t[:, :4 * T], in_=self.pb[b][:, :4 * T], func=AF.Relu),
                         reads=[self.pbr[b]], writes=[tr])
                    K.op("dve", lambda e: e.scalar_tensor_tensor(out=hid[:, 4 * i:4 * i + 4, :T],
                                                                in0=self.pb[b][:, :4 * T].rearrange("p (m t) -> p m t", t=T), scalar=0.0,
                                                                in1=t[:, :4 * T].rearrange("p (m t) -> p m t", t=T), op0=ALU.max, op1=ALU.mult),
                         reads=[self.pbr[b], tr], writes=hidr[4 * i:4 * i + 4])
                    return
                K.op("act", lambda e: e.activation(out=t[:, :T], in_=self.pb[b][:, :T], func=AF.Relu),
                     reads=[self.pbr[b]], writes=[tr])
                K.op("dve", lambda e: e.scalar_tensor_tensor(out=hid[:, 4 * i + m, :T], in0=self.pb[b][:, :T], scalar=0.0,
                                                            in1=t[:, :T], op0=ALU.max, op1=ALU.mult),
                     reads=[self.pbr[b], tr], writes=[hidr[4 * i + m]])
            self.proj_fm(wt, wres, range(4), self.actT, self.actr, T, ev)
        if pro_a is not None:
            pro_a()
        for n in range(2):
            bs = [self.bank() for _ in range(nsub)]
            for j in range(4):
                wt, wres = self.W(f"dn{j}_{n}")
                for s in range(nsub):
                    pairs = [(hid[:, 8 * j + kc, s * sp:(s + 1) * sp], wt[:, kc, :]) for kc in range(8)]
                    self.mm(self.pb[bs[s]][:sp, :], pairs, [wres] + hidr[8 * j:8 * j + 8], self.pbr[bs[s]],
                            start=(j == 0), stop=(j == 3))
            for s in range(nsub):
                b = bs[s]
                K.op("dve", lambda e, s=s, b=b, n=n: e.tensor_tensor(out=self.xh[:sp, s, n * 512:(n + 1) * 512],
                                                                    in0=self.xh[:sp, s, n * 512:(n + 1) * 512],
                                                                    in1=self.pb[b][:sp, :], op=ALU.add),
                     reads=[self.pbr[b], self.xhr[s]], writes=[self.xhr[s]])
            if n == 0 and pro_b is not None:
                pro_b()
        yst = self.R1[:, 0:8192].bitcast(F32).rearrange("p (s d) -> p s d", d=D)
        ss = self.small[:sp, 56:56 + nsub]
        rs = self.small[:sp, 60:60 + nsub]
        for s in range(nsub):
            K.op("act", lambda e, s=s: e.activation(out=self.junk2[:sp, :], in_=self.xh[:sp, s, :], func=AF.Square,
                                                    accum_out=self.small[:sp, 56 + s:57 + s]),
                 reads=[self.xhr[s], self.smallr], writes=[self.junk2r, self.smallr])
        self.rstd_from_ss(ss, rs, 1.0 / D, sp)
        for s in range(nsub):
            K.op("dve", lambda e, s=s: e.scalar_tensor_tensor(out=yst[:sp, s, :], in0=self.xh[:sp, s, :],
                                                             scalar=self.small[:sp, 60 + s:61 + s], in1=self.gfin[:sp, :],
                                                             op0=ALU.mult, op1=ALU.mult),
                 reads=[self.xhr[s], self.smallr, self.gfinr], writes=self.R1r[4 * s:4 * s + 4])
            K.dma("sp", yres_fn(s), yst[:sp, s, :], reads=self.R1r[4 * s:4 * s + 4])

    def prompt_tile(self, ti):
        K = self.K
        I, O = self.I, self.O
        T, nsub, sp = 512, 4, 128
        if ti == 0:
            pass
        else:
            for s in range(nsub):
                xa, xr = self.xnext(s)
                K.op("dve", lambda e, s=s, xa=xa: e.tensor_copy(self.xh[:, s, :], xa), reads=list(xr), writes=[self.xhr[s]])
        self.dbg("xnT", self.actT[:, :, :], self.actr, [128, 8, 512])
        uslots = [(ti * 4 + s) % 5 for s in range(4)]
        last = None
        if ti == SEQ // 512 - 1:
            last = self.r1_dummy_ufp()
        self.in_proj(nsub, sp, T, uslots, last_u_fp32=last)
        self.dbg("kd", self.kd[:, :, :], self.kdr, [128, 4, 512])
        self.dbg("qi", self.qi[:, :, :], self.qir, [128, 4, 512])
        if last is not None:
            K.dma("sp", O["spp"][:, :], last[0][113:128, :], reads=list(last[1]))
        o2 = self.xn
        o2r = self.xnr
        for c in range(4):
            cs = slice(c * 128, (c + 1) * 128)
            b = self.bank()
            for h in range(4):
                self.mm(self.pb[b][:, h * 128:(h + 1) * 128], [(self.kd[:, h, cs], self.qi[:, h, cs])],
                        [self.kdr[h], self.qir[h]], self.pbr[b], signal=(h == 3))
            K.op("dve", lambda e, b=b, c=c: e.tensor_tensor(
                out=self.att[:, c, :, :], in0=self.pb[b][:, :].rearrange("p (h t) -> p h t", t=128),
                in1=self.maskT[:, :].unsqueeze(1).to_broadcast([128, 4, 128]), op=ALU.mult),
                 reads=[self.pbr[b], self.maskTr], writes=[self.attr[c]])
            for hp in range(2):
                b2 = self.bank()
                for h in (2 * hp, 2 * hp + 1):
                    self.mm(self.pb[b2][:, (h % 2) * 256:(h % 2 + 1) * 256],
                            [(self.kdt[:, c, h * 128:(h + 1) * 128], self.vt[:, c, h * 256:(h + 1) * 256])],
                            [self.kdtr[c], self.vtr[c]], self.pbr[b2], signal=(h % 2 == 1))
                for h in (2 * hp, 2 * hp + 1):
                    K.op("act", lambda e, h=h, c=c: e.activation(out=self.Sb[:, c, h, :], in_=self.S[:, h, :], func=AF.Copy,
                                                                 scale=self.eB[:, h, c:c + 1]),
                         reads=[self.Sr[h], self.eBr], writes=[self.Sbr[c * 4 + h]])
                    K.op("dve", lambda e, h=h, b2=b2, c=c: e.scalar_tensor_tensor(
                        out=self.S[:, h, :], in0=self.S[:, h, :], scalar=self.eB[:, h, c:c + 1],
                        in1=self.pb[b2][:, (h % 2) * 256:(h % 2 + 1) * 256], op0=ALU.mult, op1=ALU.add),
                         reads=[self.Sr[h], self.eBr, self.pbr[b2]], writes=[self.Sr[h]])
        dT, dTr = self.r1_bf(0)
        pT, pTr = self.r1_bf(1)

        def pool_cc(cc):
            g = cc // 2
            b = self.bank()
            for s in range(4):
                cur = uslots[s]
                prev = (cur + 4) % 5
                csl = slice(cc * 128, (cc + 1) * 128)
                if ti == 0 and s == 0:
                    pairs = [(self.ur[:, cur, csl], self.pmat[:, 2, g, :]), (self.ur[:, cur, csl], self.pmat[:, 3, g, :])]
                    rd = [self.urr[cur], self.pmatr]
                else:
                    pairs = [(self.ur[:, prev, csl], self.pmat[:, 1, g, :]), (self.ur[:, cur, csl], self.pmat[:, 0, g, :])]
                    rd = [self.urr[cur], self.urr[prev], self.pmatr]
                self.mm(self.pb[b][:, s * 128:(s + 1) * 128], pairs, rd, self.pbr[b], signal=(s == 3))
            K.op("act", lambda e, b=b, cc=cc: e.activation(out=dT[:, cc, :], in_=self.pb[b][:, :], func=AF.Copy),
                 reads=[self.pbr[b]], writes=[dTr[cc]])

        for cc in range(4):
            pool_cc(cc)
        for c in range(4):
            cs = slice(c * 128, (c + 1) * 128)
            sr = self.osr[c]
            obanks = []
            for hp in range(2):
                b3 = self.bank()
                obanks.append(b3)
                for h in (2 * hp, 2 * hp + 1):
                    self.mm(self.pb[b3][:, (h % 2) * 256:(h % 2 + 1) * 256],
                            [(self.att[:, c, h, :], self.vt[:, c, h * 256:(h + 1) * 256]),
                             (self.qi[:, h, cs], self.Sb[:, c, h, :])],
                            [self.attr[c], self.vtr[c], self.qir[h], self.Sbr[c * 4 + h]], self.pbr[b3], signal=(h % 2 == 1))
                for h in (2 * hp, 2 * hp + 1):
                    K.op("act", lambda e, h=h, b3=b3, c=c: e.activation(out=self.junk[:, :256], in_=self.pb[b3][:, (h % 2) * 256:(h % 2 + 1) * 256],
                                                                        func=AF.Square, accum_out=self.small[:, 24 + c * 4 + h:25 + c * 4 + h]),
                         reads=[self.pbr[b3], sr], writes=[self.junkr, sr])
            self.rstd_from_ss(self.small[:, 24 + c * 4:28 + c * 4], self.small[:, 40 + c * 4:44 + c * 4], 1.0 / 256, 128, sr=sr)
            for h in range(4):
                b3 = obanks[h // 2]
                K.op("dve", lambda e, h=h, b3=b3, c=c: e.scalar_tensor_tensor(
                    out=o2[:, c, h * 256:(h + 1) * 256], in0=self.pb[b3][:, (h % 2) * 256:(h % 2 + 1) * 256],
                    scalar=self.small[:, 40 + c * 4 + h:41 + c * 4 + h], in1=self.ogs[:, c, h * 256:(h + 1) * 256],
                    op0=ALU.mult, op1=ALU.mult),
                     reads=[self.pbr[b3], sr, self.ogsr[c]], writes=[o2r[c]])
        for cc in range(4, 8):
            pool_cc(cc)
        self.pool_mix(dT, dTr, pT, pTr, T)
        for c in range(4):
            self.trans_sub(o2, o2r, self.actT, self.actr, c, sp, "g_gla", gmod=2)
        self.branches_out(self.actT, self.actr, pT, pTr, nsub, sp, T)
        self.dbg("h1", self.xh[:, :, :], self.xhr, [128, 4, 1024])
        qx, qxr = self.r1_bf(0)
        PT, PTr = self.r1_bf(1)
        ox, oxr = self.r1_bf(3)
        for n in range(2):
            wt, wres = self.W(f"xq{n}")

            def ev(m, b, n=n):
                K.op("act", lambda e: e.activation(out=qx[:, n * 4 + m, :], in_=self.pb[b][:, :], func=AF.Copy, scale=1.0 / 16),
                     reads=[self.pbr[b]], writes=[qxr[n * 4 + m]])
            self.proj_fm(wt, wres, range(4), self.actT, self.actr, T, ev)
        Pns = [self.att[:, 2 * par:2 * par + 2, :, :].rearrange("p a h t -> p (a h t)").rearrange("p (h m) -> p h m", m=256)
               for par in range(2)]
        Pnrs = [[self.attr[0], self.attr[1]], [self.attr[2], self.attr[3]]]

        def scores(s):
            ss_ = slice(s * 128, (s + 1) * 128)
            banks = []
            for hp in range(2):
                b = self.bank()
                banks.append(b)
                for h in (2 * hp, 2 * hp + 1):
                    pairs = [(qx[:, 2 * h + dc, ss_], self.KTp[:, 2 * h + dc, :]) for dc in range(2)]
                    self.mm(self.pb[b][:, (h % 2) * 256:(h % 2 + 1) * 256], pairs,
                            [qxr[2 * h], qxr[2 * h + 1], self.KTpr], self.pbr[b], signal=(h % 2 == 1))
            return self.softmax_stages([(banks[h // 2], (h % 2) * 256) for h in range(4)], 128, Pns[s % 2], Pnrs[s % 2], par=s % 3,
                                       c_eng=POOL_SOFTMAX)

        def ptrans(s):
            ss_ = slice(s * 128, (s + 1) * 128)
            Pn, Pnr = Pns[s % 2], Pnrs[s % 2]
            b = self.bank()
            if WARM_DUMMIES:
                self.mm(self.pb[b][:, :], [(self.ident[:, :], qx[:, 0, :])] * WARM_DUMMIES, [self.identr, qxr[0]], self.pbr[b],
                        signal=False)
            for h in range(4):
                for mt in range(2):
                    j = h * 2 + mt
                    K.op("pe", lambda e, b=b, h=h, mt=mt, j=j: e.transpose(self.pbf[b][:, j * 128:(j + 1) * 128],
                                                                          Pn[:, h, mt * 128:(mt + 1) * 128], self.ident[:, :]),
                         reads=list(Pnr) + [self.identr], writes=[self.pbr[b]], signal=(j == 7))
            K.op("act", lambda e, b=b, ss_=ss_: e.activation(out=PT[:, :, ss_], in_=self.pbf[b][:, :].rearrange("p (j t) -> p j t", t=128),
                                                             func=AF.Copy),
                 reads=[self.pbr[b]], writes=PTr)

        st = {}
        st[0] = scores(0); st[0][0](); st[0][1]()
        st[1] = scores(1); st[1][0]()
        st[2] = scores(2); st[2][0]()
        st[0][2](); st[1][1]()
        ptrans(0)
        st[3] = scores(3); st[3][0]()
        st[1][2](); st[2][1]()
        ptrans(1)
        st[2][2](); st[3][1]()
        ptrans(2)
        st[3][2]()
        ptrans(3)
        for h in range(4):
            for dc in range(2):
                b = self.bank()
                pairs = [(self.Vp[:, mt, h * 256 + dc * 128:h * 256 + (dc + 1) * 128], PT[:, h * 2 + mt, :]) for mt in range(2)]
                self.mm(self.pb[b][:, :], pairs, [self.Vpr, PTr[h * 2], PTr[h * 2 + 1]], self.pbr[b])
                K.op("act", lambda e, b=b, h=h, dc=dc: e.activation(out=ox[:, 2 * h + dc, :], in_=self.pb[b][:, :], func=AF.Copy),
                     reads=[self.pbr[b]], writes=[oxr[2 * h + dc]])
        self.resid_norm("xo", ox, oxr, nsub, sp, "g_mlp")
        self.dbg("h2", self.xh[:, :, :], self.xhr, [128, 4, 1024])
        pro_a = pro_b = None
        if ti + 1 == SEQ // 512 and self.do_sample:
            def pro_a():
                xa, xr = self.xnext(0)
                K.dma("sp", xa[:SB * ST, :], I["xs"][:, :], writes=xr)
                self.norm_sub(0, SB * ST, src=xa[:SB * ST, :], srcr=xr)

            def pro_b():
                self.trans_sub(self.xn, self.xnr, self.actT, self.actr, 0, SB * ST, "g_mix")
        if ti + 1 < SEQ // 512:
            def pro_a():
                for s in range(nsub):
                    xa, xr = self.xnext(s)
                    r0 = (ti + 1) * 512 + s * 128
                    K.dma("sp", xa, I["xp"][r0:r0 + 128, :], writes=xr)
                for s in range(nsub):
                    xa, xr = self.xnext(s)
                    self.norm_sub(s, sp, src=xa, srcr=xr)

            def pro_b():
                for s in range(nsub):
                    self.trans_sub(self.xn, self.xnr, self.actT, self.actr, s, sp, "g_mix")
        self.mlp_final(nsub, sp, T, None, lambda s: O["yp"][ti * 512 + s * 128:ti * 512 + (s + 1) * 128, :],
                       pro_a=pro_a, pro_b=pro_b)

    def r1_dummy_ufp(self):
        return self.ufp, [self.xnr[0], self.xnr[1]]

    def prompt_finish(self):
        K = self.K
        O = self.O
        for h in range(4):
            K.dma("sp", O["sgp"][h, :, :], self.S[:, h, :], reads=[self.Sr[h]])

    def sample_pass(self):
        K = self.K
        I, O = self.I, self.O
        T, nsub, sp = 64, 1, 64
        NB = SB
        xa, xr = self.xnext(0)
        K.op("dve", lambda e: e.tensor_copy(self.xh[:sp, 0, :], xa[:sp, :]), reads=list(xr), writes=[self.xhr[0]])
        ufp, ufpr = self.r1_dummy_ufp()
        self.in_proj(nsub, sp, T, [0], last_u_fp32=(ufp, ufpr))
        qblk = self.Sb[:, :, :, :].rearrange("p a b c -> p (a b c)").rearrange("p (h b t) -> p h b t", h=4, b=NB)
        qblkr = self.Sbr
        K.op("dve", lambda e: e.tensor_tensor(out=qblk, in0=self.qi[:, :, :T].unsqueeze(2).to_broadcast([128, 4, NB, T]),
                                             in1=self.bmq[:, :, :].unsqueeze(1).to_broadcast([128, 4, NB, T]), op=ALU.mult),
             reads=self.qir + [self.bmqr], writes=qblkr)
        kblk = self.R1[:sp, 0:8192].rearrange("p (b d) -> p b d", d=512)
        kblkr = self.R1r[0:16]
        K.op("dve", lambda e: e.tensor_tensor(out=kblk, in0=self.kdt[:sp, 0, :].unsqueeze(1).to_broadcast([sp, NB, 512]),
                                             in1=self.bmk[:sp, :].unsqueeze(2).to_broadcast([sp, NB, 512]), op=ALU.mult),
             reads=[self.kdtr[0], self.bmkr], writes=kblkr)
        b0 = self.bank()
        for h in range(4):
            self.mm(self.pb[b0][:sp, h * 64:(h + 1) * 64], [(self.kd[:, h, :T], self.qi[:, h, :T])],
                    [self.kdr[h], self.qir[h]], self.pbr[b0], signal=(h == 3))
        K.op("dve", lambda e: e.tensor_tensor(out=self.att[:sp, 0, :, :64],
                                             in0=self.pb[b0][:sp, :256].rearrange("p (h t) -> p h t", t=64),
                                             in1=self.smask[:, :].unsqueeze(1).to_broadcast([sp, 4, 64]), op=ALU.mult),
             reads=[self.pbr[b0], self.smaskr], writes=[self.attr[0]])
        ob = self.hold(4)
        for h in range(4):
            self.mm(self.pb[ob[h]][:sp, :256], [(self.att[:sp, 0, h, :64], self.vt[:sp, 0, h * 256:(h + 1) * 256])],
                    [self.attr[0], self.vtr[0]], self.pbr[ob[h]], start=True, stop=False)
        s0in = [self.xh[:, 1, :].rearrange("p (h v) -> p h v", v=256), self.xh[:, 2, :].rearrange("p (h v) -> p h v", v=256),
                self.Vp[:, :, :].rearrange("p a b -> p (a b)").bitcast(F32).rearrange("p (h v) -> p h v", v=256),
                self.S[:, :, :]]
        s0inr = [self.xhr[1], self.xhr[2], self.Vpr, self.Sr]
        NS0 = 4
        s0p = [self.vt[:, 1, :].rearrange("p (h v) -> p h v", v=256), self.vt[:, 2, :].rearrange("p (h v) -> p h v", v=256)]
        s0pr = [self.vtr[1], self.vtr[2]]
        snew = [self.xh[:, 3, :].rearrange("p (h v) -> p h v", v=256),
                self.KTp[:, :, :].rearrange("p a b -> p (a b)").bitcast(F32).rearrange("p (h v) -> p h v", v=256)]
        snewr = [self.xhr[3], self.KTpr]
        def s0w(i):
            r = s0inr[i % NS0]
            return list(r) if isinstance(r, (list, tuple)) else [r]

        for b in range(NS0 - 1):
            K.dma("sp", s0in[b], I["sgla"][b].rearrange("h k v -> k h v"), writes=s0w(b))
        cpr = Res("spscopy")
        K.dma("sp", O["sps"][:, 0:11, :], I["spool"][:, 4:15, :], writes=[cpr])
        for b in range(NB):
            K.dma("sp", O["sps"][b, 11:15, :], ufp[b * ST:(b + 1) * ST, :], reads=list(ufpr))
        for b in range(NB):
            bb = b % 2
            if b + NS0 - 1 < NB:
                K.dma("sp", s0in[(b + NS0 - 1) % NS0], I["sgla"][b + NS0 - 1].rearrange("h k v -> k h v"), writes=s0w(b + NS0 - 1))
            for h in range(4):
                K.op("act", lambda e, h=h, b=b, bb=bb: e.activation(out=s0p[bb][:, h, :], in_=s0in[b % NS0][:, h, :], func=AF.Copy,
                                                                    scale=self.eB[:, h, b:b + 1]),
                     reads=s0w(b) + [self.eBr], writes=[s0pr[bb]])
            for h in range(4):
                self.mm(self.pb[ob[h]][:sp, :256], [(qblk[:, h, b, :], s0p[bb][:, h, :])], qblkr + [s0pr[bb]], self.pbr[ob[h]],
                        start=False, stop=(b == NB - 1))
            for hp in range(2):
                b2 = self.bank()
                for h in (2 * hp, 2 * hp + 1):
                    self.mm(self.pb[b2][:, (h % 2) * 256:(h % 2 + 1) * 256],
                            [(kblk[:, b, h * 128:(h + 1) * 128], self.vt[:sp, 0, h * 256:(h + 1) * 256])],
                            kblkr + [self.vtr[0]], self.pbr[b2], signal=(h % 2 == 1))
                for h in (2 * hp, 2 * hp + 1):
                    K.op("dve", lambda e, h=h, b=b, bb=bb, b2=b2: e.scalar_tensor_tensor(
                        out=snew[bb][:, h, :], in0=s0in[b % NS0][:, h, :], scalar=self.eB[:, h, b:b + 1],
                        in1=self.pb[b2][:, (h % 2) * 256:(h % 2 + 1) * 256], op0=ALU.mult, op1=ALU.add),
                         reads=s0w(b) + [self.eBr, self.pbr[b2]], writes=[snewr[bb]])
            K.dma("pool", O["sgs"][b].rearrange("h k v -> k h v"), snew[bb], reads=[snewr[bb]])
        o2, o2r = self.xn, self.xnr
        for h in range(4):
            K.op("act", lambda e, h=h: e.activation(out=self.junk[:sp, :256], in_=self.pb[ob[h]][:sp, :256], func=AF.Square,
                                                    accum_out=self.small[:sp, 24 + h:25 + h]),
                 reads=[self.pbr[ob[h]], self.smallr], writes=[self.junkr, self.smallr])
        self.rstd_from_ss(self.small[:sp, 24:28], self.small[:sp, 40:44], 1.0 / 256, sp)
        for h in range(4):
            K.op("dve", lambda e, h=h: e.scalar_tensor_tensor(
                out=o2[:sp, 0, h * 256:(h + 1) * 256], in0=self.pb[ob[h]][:sp, :256], scalar=self.small[:sp, 40 + h:41 + h],
                in1=self.ogs[:sp, 0, h * 256:(h + 1) * 256], op0=ALU.mult, op1=ALU.mult),
                 reads=[self.pbr[ob[h]], self.smallr, self.ogsr[0]], writes=[o2r[0]])
        self.release(ob)
        self.trans_sub(o2, o2r, self.actT, self.actr, 0, sp, "g_gla", gmod=2)
        for kt in range(2):
            K.dma("pool", self.ur[:120, 1 + kt, :], I["spool"][kt * 8:(kt + 1) * 8].rearrange("b j d -> (b j) d"),
                  writes=[self.urr[1 + kt]])
        dT, dTr = self.r1_bf(0)
        pT, pTr = self.r1_bf(1)
        for cc in range(8):
            g = cc // 2
            csl = slice(cc * 128, (cc + 1) * 128)
            b = self.bank()
            pairs = [(self.ur[:120, 1, csl], self.smb[:, 0, g, :]), (self.ur[:120, 2, csl], self.smb[:, 1, g, :]),
                     (self.ur[:sp, 0, csl], self.smu[:, g, :])]
            self.mm(self.pb[b][:, :T], pairs, [self.urr[0], self.urr[1], self.urr[2], self.smbr, self.smur], self.pbr[b])
            K.op("act", lambda e, b=b, cc=cc: e.activation(out=dT[:, cc, :T], in_=self.pb[b][:, :T], func=AF.Copy),
                 reads=[self.pbr[b]], writes=[dTr[cc]])
        self.pool_mix(dT, dTr, pT, pTr, T)
        self.branches_out(self.actT, self.actr, pT, pTr, nsub, sp, T)
        qx, qxr = self.r1_bf(0)
        for n in range(2):
            wt, wres = self.W(f"xq{n}")

            def ev(m, b, n=n):
                if m is None:
                    K.op("act", lambda e: e.activation(out=qx[:, n * 4:n * 4 + 4, :T],
                                                       in_=self.pb[b][:, :4 * T].rearrange("p (m t) -> p m t", t=T), func=AF.Copy, scale=1.0 / 16),
                         reads=[self.pbr[b]], writes=qxr[n * 4:n * 4 + 4])
                    return
                K.op("act", lambda e: e.activation(out=qx[:, n * 4 + m, :T], in_=self.pb[b][:, :T], func=AF.Copy, scale=1.0 / 16),
                     reads=[self.pbr[b]], writes=[qxr[n * 4 + m]])
            self.proj_fm(wt, wres, range(4), self.actT, self.actr, T, ev)
        qxb = self.R1[:, 4096:12288].rearrange("p (j b t) -> p j b t", j=8, b=NB)
        qxbr = self.R1r[8:24]
        K.op("dve", lambda e: e.tensor_tensor(out=qxb, in0=qx[:, :, :T].unsqueeze(2).to_broadcast([128, 8, NB, T]),
                                             in1=self.bmq[:, :, :].unsqueeze(1).to_broadcast([128, 8, NB, T]), op=ALU.mult),
             reads=qxr + [self.bmqr], writes=qxbr)
        q3, q3r = self.r1_bf(3)
        PT = q3[:, :, 0:64]
        ox = q3[:, :, 64:128]
        kvb = [self.ogs[:, 1:3, :], self.ur[:, 3:5, :],
               self.xh[:, 1, :].bitcast(BF16).rearrange("p (a d) -> p a d", d=D),
               self.xh[:, 2, :].bitcast(BF16).rearrange("p (a d) -> p a d", d=D)]
        kvbr = [[self.ogsr[1], self.ogsr[2]], [self.urr[3], self.urr[4]], [self.xhr[1]], [self.xhr[2]]]
        NKV = 4
        kbT = [self.Sb[:, 0:2, :, :].rearrange("p a b c -> p (a b c)").rearrange("p (j m) -> p j m", m=256),
               self.Sb[:, 2:4, :, :].rearrange("p a b c -> p (a b c)").rearrange("p (j m) -> p j m", m=256)]
        kbTr = [self.Sbr[0:8], self.Sbr[8:16]]
        sbk = self.hold(4)

        def k_load_trans(b):
            bb = b % 2
            if USE_KVSCR:
                K.dma("pool", kvb[b % NKV], self.kvscr[0, b].rearrange("(mt p) d -> p mt d", p=128),
                      reads=[self.kvscr_res], writes=kvbr[b % NKV], sem_res=kvbr[b % NKV][0])
            else:
                K.dma("pool", kvb[b % NKV], I["ck"][b].rearrange("(mt p) d -> p mt d", p=128), writes=kvbr[b % NKV])
            for half in range(2):
                bt = self.bank()
                for jj in range(4):
                    j = half * 4 + jj
                    for mt in range(2):
                        K.op("pe", lambda e, bt=bt, jj=jj, mt=mt, j=j, b=b: e.transpose(
                            self.pbf[bt][:, jj * 256 + mt * 128:jj * 256 + (mt + 1) * 128],
                            kvb[b % NKV][:, mt, j * 128:(j + 1) * 128], self.ident[:, :]),
                             reads=kvbr[b % NKV] + [self.identr], writes=[self.pbr[bt]], signal=(jj == 3 and mt == 1))
                if half == 0:
                    K.op("act", lambda e, bt=bt, bb=bb: e.activation(out=kbT[bb][:, 0:4, :],
                                                                     in_=self.pbf[bt][:, :].rearrange("p (j m) -> p j m", m=256), func=AF.Copy),
                         reads=[self.pbr[bt]], writes=kbTr[bb])
                else:
                    K.op("dve", lambda e, bt=bt, bb=bb: e.tensor_copy(kbT[bb][:, 4:8, :],
                                                                      self.pbf[bt][:, :].rearrange("p (j m) -> p j m", m=256)),
                         reads=[self.pbr[bt]], writes=kbTr[bb])

        def k_scores(b):
            bb = b % 2
            for h in range(4):
                pairs = [(qxb[:, 2 * h + dc, b, :], kbT[bb][:, 2 * h + dc, :]) for dc in range(2)]
                self.mm(self.pb[sbk[h]][:sp, :256], pairs, qxbr + kbTr[bb], self.pbr[sbk[h]], start=(b == 0), stop=(b == NB - 1))

        k_load_trans(0)
        for b in range(NB):
            if b + 1 < NB:
                k_load_trans(b + 1)
            k_scores(b)
        Pn = self.att[:, 0:2, :, :].rearrange("p a h t -> p (a h t)").rearrange("p (h m) -> p h m", m=256)
        Pnr = [self.attr[0], self.attr[1]]
        self.softmax_rows([(sbk[h], 0) for h in range(4)], sp, Pn, Pnr)
        self.release(sbk)
        bt = self.bank()
        for h in range(4):
            for mt in range(2):
                j = h * 2 + mt
                K.op("pe", lambda e, bt=bt, h=h, mt=mt, j=j: e.transpose(self.pbf[bt][:, j * 64:(j + 1) * 64],
                                                                        Pn[:sp, h, mt * 128:(mt + 1) * 128], self.ident[:sp, :sp]),
                     reads=list(Pnr) + [self.identr], writes=[self.pbr[bt]], signal=(j == 7))
        K.op("act", lambda e, bt=bt: e.activation(out=PT, in_=self.pbf[bt][:, :512].rearrange("p (j t) -> p j t", t=64), func=AF.Copy),
             reads=[self.pbr[bt]], writes=q3r)
        bo = self.hold(1)[0]
        for b in range(NB):
            bb = b % 2
            if USE_KVSCR:
                K.dma("pool", kvb[b % NKV], self.kvscr[1, b].rearrange("(mt p) d -> p mt d", p=128),
                      reads=[self.kvscr_res], writes=kvbr[b % NKV], sem_res=kvbr[b % NKV][0])
            else:
                K.dma("pool", kvb[b % NKV], I["cv"][b].rearrange("(mt p) d -> p mt d", p=128), writes=kvbr[b % NKV])
            for j in range(8):
                h = j // 2
                pairs = [(kvb[b % NKV][:, mt, j * 128:(j + 1) * 128], PT[:, h * 2 + mt, b * ST:(b + 1) * ST]) for mt in range(2)]
                self.mm(self.pb[bo][:, j * 64 + b * ST:j * 64 + (b + 1) * ST], pairs, kvbr[b % NKV] + q3r, self.pbr[bo],
                        signal=(j == 7))
        K.op("act", lambda e: e.activation(out=ox, in_=self.pb[bo][:, :].rearrange("p (j t) -> p j t", t=64), func=AF.Copy),
             reads=[self.pbr[bo]], writes=q3r)
        self.release([bo])
        self.resid_norm("xo", ox, q3r, nsub, sp, "g_mlp")
        self.mlp_final(nsub, sp, T, None, lambda s: O["ys"][:, :])


def _extra_alloc(self):
    K = self.K
    self.eB, self.eBr = self.T_("eB", [128, 4, 16], F32)
    self.junk2 = self.vt[:, 3, :]
    self.junk2r = self.vtr[3]
    self.ufp = self.xn[:, 0:2, :].rearrange("p a d -> p (a d)").bitcast(F32)
    self.ufpr = None


_old_alloc = Prog.alloc


def _alloc(self):
    _old_alloc(self)
    _extra_alloc(self)


Prog.alloc = _alloc


def _cols_table(p):
    def colmaj(v, n):
        return np.ascontiguousarray(np.asarray(v, np.float32).reshape(n, 128).T)
    parts = [colmaj(p["norm_mix_g"][0], 8), colmaj(p["norm_x_g"][0], 8), colmaj(p["norm_mlp_g"][0], 8),
             colmaj(p["norm_mem_g"][0], 8), colmaj(p["gla_norm_g"][0], 2), colmaj(p["pool_scale"][0], 8),
             colmaj(p["b_gk"][0], 4)]
    return np.ascontiguousarray(np.concatenate(parts, axis=1))


def make_in_maps(inputs, cores):
    p = inputs
    consts = make_consts()
    shared = {
        "w_in": np.ascontiguousarray(p["w_in"][0]), "w_gk": np.ascontiguousarray(p["w_gk_up"][0]),
        "w_pm": np.ascontiguousarray(p["w_pool_mix"][0]),
        "w_a": np.ascontiguousarray(p["w_branch_a"][0]), "w_b": np.ascontiguousarray(p["w_branch_b"][0]),
        "w_o": np.ascontiguousarray(p["w_out"][0]), "w_xq": np.ascontiguousarray(p["w_xq"][0]),
        "w_xk": np.ascontiguousarray(p["w_xk"][0]), "w_xv": np.ascontiguousarray(p["w_xv"][0]),
        "w_xo": np.ascontiguousarray(p["w_xo"][0]), "w_up": np.ascontiguousarray(p["w_up"][0]),
        "w_dn": np.ascontiguousarray(p["w_down"][0]),
        "cols": _cols_table(p), "g_fin": np.ascontiguousarray(p["norm_final_g"]),
    }
    for k, v in consts.items():
        shared["c_" + k] = v
    maps = []
    for c in cores:
        m = dict(shared)
        m["xp"] = np.ascontiguousarray(p["x_prompt"][c])
        m["xs"] = np.ascontiguousarray(p["x_sample"][c * SB:(c + 1) * SB].reshape(SB * ST, D))
        m["mem"] = np.ascontiguousarray(p["mem_prompt"][c])
        m["sgla"] = np.ascontiguousarray(p["state_gla"][0, c * SB:(c + 1) * SB])
        m["spool"] = np.ascontiguousarray(p["state_pool"][0, c * SB:(c + 1) * SB])
        m["ck"] = np.ascontiguousarray(p["cache_mem_k"][0, c * SB:(c + 1) * SB].reshape(SB, MEM, D))
        m["cv"] = np.ascontiguousarray(p["cache_mem_v"][0, c * SB:(c + 1) * SB].reshape(SB, MEM, D))
        maps.append(m)
    return maps


_PROG_CACHE = {}


def get_prog(do_sample=True, dbg=()):
    key = (do_sample, tuple(dbg))
    if key not in _PROG_CACHE:
        _PROG_CACHE[key] = Prog(do_sample=do_sample, dbg=dbg)
    return _PROG_CACHE[key]


def kernel(**inputs):
    inputs = {k: np.asarray(v) for k, v in inputs.items()}
    prog = get_prog(True)
    cores = list(range(NCORE))
    maps = make_in_maps(inputs, cores)
    res = run_bass_kernel_spmd(prog.nc, maps, core_ids=cores)
    R = res.results
    yp = np.stack([R[c]["yp"] for c in cores]).astype(np.float32)
    ys = np.concatenate([R[c]["ys"].reshape(SB, ST, D) for c in cores]).astype(np.float32)
    mk = np.stack([R[c]["mk"].reshape(MEM, 4, 256) for c in cores])[None].astype(np.float32)
    mv = np.stack([R[c]["mv"].reshape(MEM, 4, 256) for c in cores])[None].astype(np.float32)
    sgp = np.stack([R[c]["sgp"] for c in cores])[None].astype(np.float32)
    sgs = np.concatenate([R[c]["sgs"] for c in cores])[None].astype(np.float32)
    spp = np.stack([R[c]["spp"] for c in cores])[None].astype(np.float32)
    sps = np.concatenate([R[c]["sps"] for c in cores])[None].astype(np.float32)
    return (yp, ys, mk, mv, sgp, sgs, spp, sps)
```
